# Optimizing a Trainium2 kernel written in Bass

```python
import math
import jax, jax.numpy as jnp
from jax import lax
import numpy as np

D_MODEL = 2048
BATCH = 4
SEQ = 4096
DEPTH = 4

CHUNK = 64
Q_BLOCK = 128
EPS = 1e-6
GROUP_WIDTH = D_MODEL // 4

MLA_HEADS = 4
MLA_NOPE = 128
MLA_ROPE = 64
MLA_V = GROUP_WIDTH // MLA_HEADS
MLA_Q_RANK = D_MODEL // 4
MLA_KV_RANK = D_MODEL // 8
ROPE_THETA = 10000.0
SSM_HEAD_DIM = 64
SSM_HEADS = GROUP_WIDTH // SSM_HEAD_DIM
SSM_GROUPS = 2
SSM_STATE = 128
SSM_CONV = 4
SSM_CHUNK = CHUNK
SSM_CONV_DIM = GROUP_WIDTH + 2 * SSM_GROUPS * SSM_STATE
SB_HEADS = 4
SB_HEAD_DIM = GROUP_WIDTH // SB_HEADS
CA_HEADS = 4
CA_HEAD_DIM = GROUP_WIDTH // CA_HEADS
CA_PAST_CHUNKS = 8
CA_BAND = (CA_PAST_CHUNKS + 1) * CHUNK
CA_REL_PAST = 256
CA_REL_SIZE = CA_REL_PAST + CHUNK
N_EXPERTS = 32
TOP_K = 4
D_EXPERT = 768
SWIGLU_ALPHA = 1.702
SWIGLU_LIMIT = 7.0
MOE_BLOCK = 256

A_COLS = MLA_Q_RANK + MLA_KV_RANK + MLA_ROPE
B_COLS = GROUP_WIDTH + SSM_CONV_DIM + SSM_HEADS
C_COLS = 3 * GROUP_WIDTH
D_COLS = 3 * GROUP_WIDTH
IN_COLS = A_COLS + B_COLS + C_COLS + D_COLS

kernel_name = 'hymba_style_streaming_hybrid_moe'


def rms_norm(x, g):
    xf = x.astype(jnp.float32)
    y = xf * lax.rsqrt(jnp.mean(xf * xf, axis=-1, keepdims=True) + EPS)
    return (y * g.astype(jnp.float32)).astype(x.dtype)


def modulate(h, shift, scale):
    return h * (1 + scale[:, None, :]) + shift[:, None, :]


def rope_tables(seq):
    inv = 1.0 / (ROPE_THETA ** (jnp.arange(0, MLA_ROPE, 2, dtype=jnp.float32) / MLA_ROPE))
    ang = jnp.arange(seq, dtype=jnp.float32)[:, None] * inv[None, :]
    return jnp.cos(ang), jnp.sin(ang)


def apply_rope(x, cos, sin):
    x1, x2 = jnp.split(x.astype(jnp.float32), 2, axis=-1)
    return jnp.concatenate([x1 * cos - x2 * sin, x1 * sin + x2 * cos], axis=-1).astype(x.dtype)


def to_qblocks(t, nq):
    return jnp.moveaxis(t.reshape(t.shape[0], nq, Q_BLOCK, *t.shape[2:]), 1, 0)


def mla_attention(q_nope, q_rope, k_nope, k_rope, v):
    b, s, h, _ = q_nope.shape
    nq = s // Q_BLOCK
    scale = (MLA_NOPE + MLA_ROPE) ** -0.5
    key_chunk = jnp.arange(s) // CHUNK

    def one_block(args):
        qn, qr, blk = args
        sc = jnp.einsum('bqhd,bkhd->bhqk', qn, k_nope) + jnp.einsum('bqhr,bkr->bhqk', qr, k_rope)
        sc = sc.astype(jnp.float32) * scale
        q_chunk = (blk * Q_BLOCK + jnp.arange(Q_BLOCK)) // CHUNK
        mask = key_chunk[None, :] <= q_chunk[:, None]
        p = jax.nn.softmax(jnp.where(mask, sc, -jnp.inf), axis=-1).astype(v.dtype)
        return jnp.einsum('bhqk,bkhd->bqhd', p, v)

    out = lax.map(one_block, (to_qblocks(q_nope, nq), to_qblocks(q_rope, nq), jnp.arange(nq)))
    return jnp.moveaxis(out, 0, 1).reshape(b, s, h, -1)


def ssd_scan(x, dt, a, bm, cm):
    b, s, h, p = x.shape
    g, n = bm.shape[2], bm.shape[3]
    r = h // g
    nc = s // SSM_CHUNK
    q = SSM_CHUNK
    xq = x.astype(jnp.float32).reshape(b, nc, q, g, r, p)
    dtq = dt.reshape(b, nc, q, g, r)
    bq = bm.astype(jnp.float32).reshape(b, nc, q, g, n)
    cq = cm.astype(jnp.float32).reshape(b, nc, q, g, n)
    da_cs = jnp.cumsum(dtq * a.reshape(g, r), axis=2)
    cs_h = jnp.moveaxis(da_cs, 2, -1)
    tril = jnp.tril(jnp.ones((q, q), dtype=bool))
    diff = cs_h[..., :, None] - cs_h[..., None, :]
    decay = jnp.where(tril, jnp.exp(jnp.where(tril, diff, 0.0)), 0.0)
    cb = jnp.einsum('bctgn,bcsgn->bcgts', cq, bq)
    weights = cb[:, :, :, None] * decay * jnp.moveaxis(dtq, 2, -1)[..., None, :]
    y_diag = jnp.einsum('bcgrts,bcsgrp->bctgrp', weights, xq)
    decay_to_end = jnp.exp(da_cs[:, :, -1:] - da_cs)
    states = jnp.einsum('bcsgn,bcsgr,bcsgrp->bcgrpn', bq, decay_to_end * dtq, xq)
    chunk_decay = jnp.exp(da_cs[:, :, -1])

    def step(state, inp):
        st, dec = inp
        return state * dec[..., None, None] + st, state

    h0 = jnp.zeros((b, g, r, p, n), jnp.float32)
    _, h_in = lax.scan(step, h0, (jnp.moveaxis(states, 1, 0), jnp.moveaxis(chunk_decay, 1, 0)))
    h_in = jnp.moveaxis(h_in, 0, 1)
    y_off = jnp.einsum('bctgn,bcgrpn,bctgr->bctgrp', cq, h_in, jnp.exp(da_cs))
    return (y_diag + y_off).reshape(b, s, h, p)


def mamba2_mixer(z, xbc, dt_raw, conv_w, conv_b, dt_bias, a_log, d_skip, norm_g):
    b, s, _ = z.shape
    xbc = lax.conv_general_dilated(
        xbc, conv_w.astype(xbc.dtype)[:, None, :], window_strides=(1,),
        padding=((SSM_CONV - 1, 0),), dimension_numbers=('NWC', 'WIO', 'NWC'),
        feature_group_count=SSM_CONV_DIM) + conv_b
    xbc = jax.nn.silu(xbc)
    xs, bm, cm = jnp.split(xbc, [GROUP_WIDTH, GROUP_WIDTH + SSM_GROUPS * SSM_STATE], axis=-1)
    x = xs.reshape(b, s, SSM_HEADS, SSM_HEAD_DIM)
    bm = bm.reshape(b, s, SSM_GROUPS, SSM_STATE)
    cm = cm.reshape(b, s, SSM_GROUPS, SSM_STATE)
    dt = jax.nn.softplus(dt_raw.astype(jnp.float32) + dt_bias.astype(jnp.float32))
    a = -jnp.exp(a_log.astype(jnp.float32))
    y = ssd_scan(x, dt, a, bm, cm) + x.astype(jnp.float32) * d_skip.astype(jnp.float32)[:, None]
    y = y.reshape(b, s, GROUP_WIDTH) * jax.nn.silu(z.astype(jnp.float32))
    yg = y.reshape(b, s, SSM_GROUPS, GROUP_WIDTH // SSM_GROUPS)
    yg = yg * lax.rsqrt(jnp.mean(yg * yg, axis=-1, keepdims=True) + EPS)
    return (yg.reshape(b, s, GROUP_WIDTH) * norm_g.astype(jnp.float32)).astype(z.dtype)


def stick_breaking_attention(q, k, v):
    b, s, h, d = q.shape
    nq = s // Q_BLOCK
    scale = d ** -0.5
    key_pos = jnp.arange(s)

    def one_block(args):
        qb, blk = args
        zt = jnp.einsum('bqhd,bkhd->bhqk', qb, k).astype(jnp.float32) * scale
        q_pos = blk * Q_BLOCK + jnp.arange(Q_BLOCK)
        mask = key_pos[None, :] < q_pos[:, None]
        log_beta = jax.nn.log_sigmoid(zt)
        log_keep = jnp.where(mask, jax.nn.log_sigmoid(-zt), 0.0)
        later = lax.cumsum(log_keep, axis=3, reverse=True) - log_keep
        att = jnp.where(mask, jnp.exp(log_beta + later), 0.0).astype(v.dtype)
        return jnp.einsum('bhqk,bkhd->bqhd', att, v)

    out = lax.map(one_block, (to_qblocks(q, nq), jnp.arange(nq)))
    return jnp.moveaxis(out, 0, 1).reshape(b, s, h, d)


def chunked_relpos_attention(q, k, v, rel_bias):
    b, s, h, d = q.shape
    nc = s // CHUNK
    qc = q.reshape(b, nc, CHUNK, h, d)
    pad = ((0, 0), (CA_PAST_CHUNKS, 0), (0, 0), (0, 0), (0, 0))
    kp = jnp.pad(k.reshape(b, nc, CHUNK, h, d), pad)
    vp = jnp.pad(v.reshape(b, nc, CHUNK, h, d), pad)
    kb = jnp.concatenate([kp[:, j:j + nc] for j in range(CA_PAST_CHUNKS + 1)], axis=2)
    vb = jnp.concatenate([vp[:, j:j + nc] for j in range(CA_PAST_CHUNKS + 1)], axis=2)
    sc = jnp.einsum('bcqhd,bckhd->bchqk', qc, kb).astype(jnp.float32) * d ** -0.5
    dist = CA_PAST_CHUNKS * CHUNK + jnp.arange(CHUNK)[:, None] - jnp.arange(CA_BAND)[None, :]
    idx = jnp.clip(dist, -(CHUNK - 1), CA_REL_PAST) + (CHUNK - 1)
    sc = sc + rel_bias[:, idx].astype(jnp.float32)[None, None]
    key_chunk = jnp.arange(nc)[:, None] - CA_PAST_CHUNKS + jnp.arange(CA_BAND)[None, :] // CHUNK
    valid = (key_chunk >= 0)[None, :, None, None, :]
    p = jax.nn.softmax(jnp.where(valid, sc, -jnp.inf), axis=-1).astype(v.dtype)
    return jnp.einsum('bchqk,bckhd->bcqhd', p, vb).reshape(b, s, h, d)


def clamped_swiglu(gate, up):
    gate = jnp.minimum(gate, SWIGLU_LIMIT)
    up = jnp.clip(up, -SWIGLU_LIMIT, SWIGLU_LIMIT)
    return gate * jax.nn.sigmoid(SWIGLU_ALPHA * gate) * (up + 1.0)


def moe_ffn(h, router_w, router_b, w1, b1, w2, b2):
    b, s, d = h.shape
    n_tok = b * s
    t = h.reshape(n_tok, d)
    logits = (t @ router_w).astype(jnp.float32) + router_b.astype(jnp.float32)
    top_v, top_e = lax.top_k(logits, TOP_K)
    gates = jax.nn.softmax(top_v, axis=-1)
    flat_e = top_e.reshape(-1)
    flat_g = gates.reshape(-1)
    n_assign = n_tok * TOP_K
    order = jnp.argsort(flat_e)
    sorted_e = flat_e[order]
    counts = jnp.bincount(flat_e, length=N_EXPERTS)
    padded = (counts + MOE_BLOCK - 1) // MOE_BLOCK * MOE_BLOCK
    start = jnp.cumsum(counts) - counts
    padded_end = jnp.cumsum(padded)
    padded_start = padded_end - padded
    dest = padded_start[sorted_e] + jnp.arange(n_assign) - start[sorted_e]
    n_blocks = -(-(n_assign + N_EXPERTS * (MOE_BLOCK - 1)) // MOE_BLOCK)
    n_rows = n_blocks * MOE_BLOCK
    row_tok = jnp.zeros((n_rows,), jnp.int32).at[dest].set((order // TOP_K).astype(jnp.int32))
    row_gate = jnp.zeros((n_rows,), jnp.float32).at[dest].set(flat_g[order])
    block_e = jnp.minimum(jnp.searchsorted(padded_end, jnp.arange(n_blocks) * MOE_BLOCK, side='right'),
                          N_EXPERTS - 1)

    def run_block(args):
        tok, e = args
        xb = t[tok]
        hu = (xb @ w1[e] + b1[e]).astype(jnp.float32)
        act = clamped_swiglu(hu[:, :D_EXPERT], hu[:, D_EXPERT:]).astype(t.dtype)
        return act @ w2[e] + b2[e]

    y = lax.map(run_block, (row_tok.reshape(n_blocks, MOE_BLOCK), block_e))
    y = y.reshape(n_rows, d) * row_gate[:, None].astype(y.dtype)
    return jax.ops.segment_sum(y, row_tok, num_segments=n_tok).reshape(b, s, d)


def hybrid_layer(x, cond, cos, sin, attn_norm, ffn_norm, mod_w, mod_b, w_in,
                 mla_q_norm, mla_w_q_up, mla_kv_norm, mla_w_kv_up,
                 ssm_conv_w, ssm_conv_b, ssm_dt_bias, ssm_a_log, ssm_d, ssm_norm,
                 ca_rel_bias, mix_out_norm, w_out,
                 router_w, router_b, moe_w1, moe_b1, moe_w2, moe_b2):
    b, s, _ = x.shape
    mod = cond @ mod_w + mod_b
    shift_m, scale_m, gate_m, shift_f, scale_f, gate_f = jnp.split(mod, 6, axis=-1)

    h = modulate(rms_norm(x, attn_norm), shift_m, scale_m)
    proj = h @ w_in
    p_a, p_b, p_c, p_d = jnp.split(proj, [A_COLS, A_COLS + B_COLS, A_COLS + B_COLS + C_COLS], axis=-1)

    q_lat, kv_lat, k_rope = jnp.split(p_a, [MLA_Q_RANK, MLA_Q_RANK + MLA_KV_RANK], axis=-1)
    q = (rms_norm(q_lat, mla_q_norm) @ mla_w_q_up).reshape(b, s, MLA_HEADS, MLA_NOPE + MLA_ROPE)
    kv = (rms_norm(kv_lat, mla_kv_norm) @ mla_w_kv_up).reshape(b, s, MLA_HEADS, MLA_NOPE + MLA_V)
    q_rope = apply_rope(q[..., MLA_NOPE:], cos[:, None, :], sin[:, None, :])
    k_rope = apply_rope(k_rope, cos, sin)
    out_a = mla_attention(q[..., :MLA_NOPE], q_rope, kv[..., :MLA_NOPE], k_rope, kv[..., MLA_NOPE:])
    out_a = rms_norm(out_a.reshape(b, s, GROUP_WIDTH), mix_out_norm[0])

    z, xbc, dt_raw = jnp.split(p_b, [GROUP_WIDTH, GROUP_WIDTH + SSM_CONV_DIM], axis=-1)
    out_b = mamba2_mixer(z, xbc, dt_raw, ssm_conv_w, ssm_conv_b, ssm_dt_bias, ssm_a_log, ssm_d, ssm_norm)

    q_c, k_c, v_c = [u.reshape(b, s, SB_HEADS, SB_HEAD_DIM) for u in jnp.split(p_c, 3, axis=-1)]
    out_c = rms_norm(stick_breaking_attention(q_c, k_c, v_c).reshape(b, s, GROUP_WIDTH), mix_out_norm[1])

    q_d, k_d, v_d = [u.reshape(b, s, CA_HEADS, CA_HEAD_DIM) for u in jnp.split(p_d, 3, axis=-1)]
    out_d = chunked_relpos_attention(q_d, k_d, v_d, ca_rel_bias).reshape(b, s, GROUP_WIDTH)
    out_d = rms_norm(out_d, mix_out_norm[2])

    mixed = jnp.concatenate([out_a, out_b, out_c, out_d], axis=-1) @ w_out
    x = x + gate_m[:, None, :] * mixed

    h = modulate(rms_norm(x, ffn_norm), shift_f, scale_f)
    x = x + gate_f[:, None, :] * moe_ffn(h, router_w, router_b, moe_w1, moe_b1, moe_w2, moe_b2)
    return x


def setup_inputs(seed: int = 0) -> dict:
    key = jax.random.key(seed)
    ks = jax.random.split(key, 32)
    counter = iter(range(32))

    def nk():
        return ks[next(counter)]

    def nrm(shape, scale):
        return jax.random.normal(nk(), shape, jnp.float32) * scale

    def gain(shape):
        return 1.0 + nrm(shape, 0.02)

    L = DEPTH
    x = nrm((BATCH, SEQ, D_MODEL), 1.0)
    c = nrm((BATCH, D_MODEL), 1.0)
    attn_norm = gain((L, D_MODEL))
    ffn_norm = gain((L, D_MODEL))
    mod_w = nrm((L, D_MODEL, 6 * D_MODEL), 0.5 * D_MODEL ** -0.5)
    mod_b = nrm((L, 6 * D_MODEL), 0.02)
    w_in = nrm((L, D_MODEL, IN_COLS), D_MODEL ** -0.5)
    mla_q_norm = gain((L, MLA_Q_RANK))
    mla_w_q_up = nrm((L, MLA_Q_RANK, MLA_HEADS * (MLA_NOPE + MLA_ROPE)), MLA_Q_RANK ** -0.5)
    mla_kv_norm = gain((L, MLA_KV_RANK))
    mla_w_kv_up = nrm((L, MLA_KV_RANK, MLA_HEADS * (MLA_NOPE + MLA_V)), MLA_KV_RANK ** -0.5)
    ssm_conv_w = nrm((L, SSM_CONV, SSM_CONV_DIM), SSM_CONV ** -0.5)
    ssm_conv_b = nrm((L, SSM_CONV_DIM), 0.02)
    u = jax.random.uniform(nk(), (L, SSM_HEADS), jnp.float32)
    dt0 = jnp.exp(u * (math.log(0.1) - math.log(0.001)) + math.log(0.001))
    ssm_dt_bias = dt0 + jnp.log(-jnp.expm1(-dt0))
    ssm_a_log = jnp.log(jax.random.uniform(nk(), (L, SSM_HEADS), jnp.float32, minval=1.0, maxval=16.0))
    ssm_d = gain((L, SSM_HEADS))
    ssm_norm = gain((L, GROUP_WIDTH))
    ca_rel_bias = nrm((L, CA_HEADS, CA_REL_SIZE), 0.1)
    mix_out_norm = gain((L, 3, GROUP_WIDTH))
    w_out = nrm((L, D_MODEL, D_MODEL), D_MODEL ** -0.5)
    router_w = nrm((L, D_MODEL, N_EXPERTS), D_MODEL ** -0.5)
    router_b = nrm((L, N_EXPERTS), 0.01)
    moe_w1 = nrm((L, N_EXPERTS, D_MODEL, 2 * D_EXPERT), D_MODEL ** -0.5)
    moe_b1 = nrm((L, N_EXPERTS, 2 * D_EXPERT), 0.02)
    moe_w2 = nrm((L, N_EXPERTS, D_EXPERT, D_MODEL), D_EXPERT ** -0.5)
    moe_b2 = nrm((L, N_EXPERTS, D_MODEL), 0.02)
    final_norm = gain((D_MODEL,))
    return {'x': x, 'c': c, 'attn_norm': attn_norm, 'ffn_norm': ffn_norm, 'mod_w': mod_w, 'mod_b': mod_b,
            'w_in': w_in, 'mla_q_norm': mla_q_norm, 'mla_w_q_up': mla_w_q_up, 'mla_kv_norm': mla_kv_norm,
            'mla_w_kv_up': mla_w_kv_up, 'ssm_conv_w': ssm_conv_w, 'ssm_conv_b': ssm_conv_b,
            'ssm_dt_bias': ssm_dt_bias, 'ssm_a_log': ssm_a_log, 'ssm_d': ssm_d, 'ssm_norm': ssm_norm,
            'ca_rel_bias': ca_rel_bias, 'mix_out_norm': mix_out_norm, 'w_out': w_out,
            'router_w': router_w, 'router_b': router_b, 'moe_w1': moe_w1, 'moe_b1': moe_b1,
            'moe_w2': moe_w2, 'moe_b2': moe_b2, 'final_norm': final_norm}


def reference(x, c, attn_norm, ffn_norm, mod_w, mod_b, w_in, mla_q_norm, mla_w_q_up, mla_kv_norm,
              mla_w_kv_up, ssm_conv_w, ssm_conv_b, ssm_dt_bias, ssm_a_log, ssm_d, ssm_norm,
              ca_rel_bias, mix_out_norm, w_out, router_w, router_b, moe_w1, moe_b1, moe_w2, moe_b2,
              final_norm):
    cond = jax.nn.silu(c)
    cos, sin = rope_tables(x.shape[1])
    for i in range(DEPTH):
        x = hybrid_layer(x, cond, cos, sin, attn_norm[i], ffn_norm[i], mod_w[i], mod_b[i], w_in[i],
                         mla_q_norm[i], mla_w_q_up[i], mla_kv_norm[i], mla_w_kv_up[i],
                         ssm_conv_w[i], ssm_conv_b[i], ssm_dt_bias[i], ssm_a_log[i], ssm_d[i], ssm_norm[i],
                         ca_rel_bias[i], mix_out_norm[i], w_out[i],
                         router_w[i], router_b[i], moe_w1[i], moe_b1[i], moe_w2[i], moe_b2[i])
    return rms_norm(x, final_norm)
```

```python
from contextlib import ExitStack
import os
import numpy as np
import concourse.bass as bass
import concourse.mybir as mybir
from concourse.bass_utils import run_bass_kernel_spmd

F32 = mybir.dt.float32
BF16 = mybir.dt.bfloat16
AF = mybir.ActivationFunctionType
ALU = mybir.AluOpType

SEM_LIMIT = 30000
NDMA_SLOTS = 6

D = 2048
S = 4096
NKC = 16
TB = 512
NTB = S // TB
EPS = 1e-6
NEXP = 32
DEXP = 768
NCORES = 4


class _Res:
    __slots__ = ("lw", "rd")

    def __init__(self):
        self.lw = None
        self.rd = {}


class Prog:
    ENGS = ("pe", "act", "dve", "pool", "sp")

    def __init__(self, nc, stack):
        self.nc = nc
        self.stack = stack
        self.streams = {e: [] for e in self.ENGS}
        self.seen = {e: {} for e in self.ENGS}
        self.clk = {}
        self.res = {}
        self.nsem = 0
        self.ninstr = 0
        self.pending = {}
        self.allclk = []
        for e in ("pe", "act", "dve", "pool"):
            self.clk[e] = self._newclk(e)
        self.dslots = {q: [self._newclk(f"d{q}{i}") for i in range(NDMA_SLOTS)] for q in ("sp", "pool", "act")}
        self.dnext = {q: 0 for q in ("sp", "pool", "act")}

    def _newclk(self, name):
        self.nsem += 1
        c = [self.stack.enter_context(self.nc.semaphore(f"{name}_{self.nsem}")), 0]
        self.allclk.append(c)
        return c

    def _r(self, key):
        r = self.res.get(key)
        if r is None:
            r = self.res[key] = _Res()
        return r

    def _deps(self, eng, reads, writes, acc, is_dma):
        need = {}

        def add(d, kind):
            if d is None:
                return
            clk, tick, deng = d
            if (not is_dma) and deng == eng and kind != "raw":
                return
            if self.seen[eng].get(clk, 0) >= tick:
                return
            if need.get(clk, 0) < tick:
                need[clk] = tick

        for k in reads:
            add(self._r(k).lw, "raw")
        for k in writes:
            r = self._r(k)
            if not (acc and r.lw is not None and r.lw[2] == eng):
                add(r.lw, "waw")
            for d in r.rd.values():
                add(d, "war")
        return need

    def _commit(self, reads, writes, done):
        for k in reads:
            self._r(k).rd[done[0]] = done
        for k in writes:
            r = self._r(k)
            r.lw = done
            r.rd = {}

    def _emit_waits(self, eng, need):
        for clk, tick in need.items():
            self.seen[eng][clk] = tick
            self.streams[eng].append(("w", clk, tick))

    def op(self, eng, fn, reads=(), writes=(), inc=True, acc=False):
        need = self._deps(eng, reads, writes, acc, False)
        self._emit_waits(eng, need)
        c = self.clk[eng]
        if c[1] >= SEM_LIMIT and not self.pending.get(eng, False):
            c = self.clk[eng] = self._newclk(eng)
        self.pending[eng] = not inc
        if inc:
            c[1] += 1
            tick = c[1]
        else:
            tick = c[1] + 1
        done = (c[0], tick, eng)
        self.streams[eng].append(("i", fn, c[0] if inc else None))
        self._commit(reads, writes, done)
        self.ninstr += 1
        return done

    def dma(self, q, fn, reads=(), writes=()):
        need = self._deps(q, reads, writes, False, True)
        i = self.dnext[q]
        self.dnext[q] = (i + 1) % NDMA_SLOTS
        slot = self.dslots[q][i]
        if slot[1] >= SEM_LIMIT:
            slot = self.dslots[q][i] = self._newclk(f"d{q}{i}")
        if slot[1] > 0 and self.seen[q].get(slot[0], 0) < slot[1]:
            if need.get(slot[0], 0) < slot[1]:
                need[slot[0]] = slot[1]
        self._emit_waits(q, need)
        slot[1] += 16
        done = (slot[0], slot[1], "dma")
        self.streams[q].append(("d", fn, slot[0]))
        self._commit(reads, writes, done)
        self.ninstr += 1
        return done

    def barrier(self):
        for e in self.ENGS:
            need = {}
            for c in self.allclk:
                if c[1] > 0 and self.seen[e].get(c[0], 0) < c[1]:
                    need[c[0]] = c[1]
            self._emit_waits(e, need)
        self.res = {}

    def finish(self):
        nc = self.nc
        engmap = {"pe": "tensor", "act": "scalar", "dve": "vector", "pool": "gpsimd", "sp": "sync"}
        with nc.Block() as block:
            for e in self.ENGS:
                items = self.streams[e]

                def body(engobj, items=items):
                    for it in items:
                        if it[0] == "w":
                            engobj.wait_ge(it[1], it[2])
                        elif it[0] == "i":
                            ins = it[1](engobj)
                            if it[2] is not None:
                                ins.then_inc(it[2], 1)
                        else:
                            it[1](engobj).then_inc(it[2], 16)

                getattr(block, engmap[e])(body)


IN_A, IN_B, IN_C, IN_D = 0, 832, 2376, 3912
FT_CH = {}
_c = 0
for _nm, _n in (("qlat", 4), ("kvlat", 2), ("kr", 1), ("krs", 1), ("z", 4), ("xs", 4), ("bm", 2), ("cm", 2),
                ("qc", 4), ("kc", 4), ("qd", 4), ("kd", 4)):
    FT_CH[_nm] = _c
    _c += _n
NFT = _c


class K:
    pass


def build(depth, n_exp=NEXP, dbg_cat=False, dbg_out=False, chain=False):
    nc = bass.Bass("TRN2", target_bir_lowering=False)
    k = K()
    k.nc = nc

    def din(name, shape, dt=F32):
        return nc.dram_tensor(name, list(shape), dt, kind="ExternalInput").ap()

    def dscr(name, shape, dt):
        return nc.dram_tensor(name, list(shape), dt, kind="Internal").ap()

    L = depth
    x_in = din("x", [S, D])
    c_in = din("c", [16, 128])
    attn_norm = din("attn_norm", [L, 16, 128])
    ffn_norm = din("ffn_norm", [L, 16, 128])
    mod_w = din("mod_w", [L, D, 6 * D])
    mod_b = din("mod_b", [L, 96, 128])
    w_in = din("w_in", [L, D, 5448])
    mla_q_norm = din("mla_q_norm", [L, 4, 128])
    wq = din("mla_w_q_up", [L, 512, 768])
    mla_kv_norm = din("mla_kv_norm", [L, 2, 128])
    wkv = din("mla_w_kv_up", [L, 256, 1024])
    conv_w = din("ssm_conv_w", [L, 4 * 8, 128])
    conv_b = din("ssm_conv_b", [L, 8, 128])
    dt_bias = din("ssm_dt_bias", [L, 8, 1])
    a_log = din("ssm_a_log", [L, 8, 1])
    ssm_d = din("ssm_d", [L, 8])
    ssm_norm = din("ssm_norm", [L, 4, 128])
    ssm_norm8 = din("ssm_norm8", [L, 8, 64])
    ca_bias = din("ca_biasT", [L, 4, 5, 128, 128])
    mix_norm = din("mix_out_norm", [L, 12, 128])
    w_out = din("w_out", [L, D, D])
    router_w = din("router_w", [L, D, NEXP])
    router_b = din("router_b", [L, 1, NEXP])
    w1 = din("moe_w1", [L, NEXP, D, 2 * DEXP])
    b1 = din("moe_b1", [L, NEXP * 12, 128])
    w2 = din("moe_w2", [L, NEXP, DEXP, D])
    b2 = din("moe_b2", [L, NEXP, D])
    final_norm = din("final_norm", [16, 128])
    cst = din("cst", [128, 8, 128])
    sel8 = din("sel8", [8, 8 * 128])
    sel32 = din("sel32", [32, 32 * 128])
    rope_cos = din("rope_cos", [128, S])
    rope_sin = din("rope_sin", [128, S])
    y_out = nc.dram_tensor("y", [S, D], F32, kind="ExternalOutput").ap()
    xo_out = nc.dram_tensor("xo", [S, D], F32, kind="ExternalOutput").ap() if chain else None

    xT = dscr("xT", [D, S], F32)
    FT = dscr("FT", [NFT * 128, S], BF16)
    DTR = dscr("DTR", [8, S], F32)
    VCs = dscr("VC", [S, 512], BF16)
    VDs = dscr("VD", [S, 512], BF16)
    catT = din("catT_in", [D, S], BF16) if dbg_cat else dscr("catT", [D, S], BF16)
    k.dbg_cat = dbg_cat
    k.dbg_out = dbg_out
    cat_dump = nc.dram_tensor("cat_dump", [D, S], BF16, kind="ExternalOutput").ap() if (dbg_out and not dbg_cat) else None
    h2T = dscr("h2T", [D, S], BF16)
    GTs = dscr("GT", [NEXP, S], F32)
    FT2 = dscr("FT2", [11 * 128, S], BF16)
    VMs = dscr("VM", [S, 512], BF16)
    XSs = dscr("XSs", [S, 512], BF16)
    CSs = dscr("CSs", [8, S], F32)
    CSCs = dscr("CSCs", [128, 256], F32)
    DTCs = dscr("DTCs", [128, 256], F32)

    with ExitStack() as gst:
        P = Prog(nc, gst)

        ARENA_BYTES = 196608
        ARENA = gst.enter_context(nc.sbuf_tensor("ARENA", [128, ARENA_BYTES // 2], BF16))
        aoff = [0]

        class Tile:
            def __init__(self, ap, name, shape):
                self.ap, self.name, self.shape = ap, name, tuple(shape)

            def __getitem__(self, key):
                return self.ap[key]

        def sb(st, name, shape, dt):
            esz = 4 if dt == F32 else 2
            nel = 1
            for d_ in shape[1:]:
                nel *= d_
            nbytes = (nel * esz + 63) // 64 * 64
            off = aoff[0]
            assert off + nbytes <= ARENA_BYTES, (name, off, nbytes)
            aoff[0] = off + nbytes
            if st is not gst:
                st.callback(lambda off=off: aoff.__setitem__(0, off))
            v = ARENA[0:shape[0], off // 2:off // 2 + nel * esz // 2]
            if dt == F32:
                v = v.bitcast(F32)
            if len(shape) == 3:
                v = v.rearrange("p (a b) -> p a b", a=shape[1])
            return Tile(v, name, shape)

        PS = [gst.enter_context(nc.psum_tensor(f"ps{i}", [128, 512], F32)) for i in range(8)]
        CF = sb(gst, "CF", [128, 8, 128], F32)
        CB = sb(gst, "CB", [128, 8, 128], BF16)
        SEL = sb(gst, "SEL", [8, 8 * 128], F32)
        P.dma("sp", lambda e: e.dma_start(out=CF[:], in_=cst[:, :, :]), writes=["CF"])
        P.dma("sp", lambda e: e.dma_start(out=SEL[:], in_=sel8[:, :]), writes=["SEL"])
        P.op("dve", lambda e: e.tensor_copy(out=CB[:], in_=CF[:]), reads=["CF"], writes=["CB"])
        IDF, ONESF = CF[:, 0, :], CF[:, 1, :]
        IDB, ONESB, TRIB, ZB = CB[:, 0, :], CB[:, 1, :], CB[:, 5, :], CB[:, 6, :]
        M_LT, M_LE, M_MLA = CF[:, 2, :], CF[:, 3, :], CF[:, 4, :]
        COLS = sb(gst, "COLS", [128, 400], F32)
        colmap = {}
        _o = 0
        for nm, n in (("an", 16), ("fn", 16), ("modb", 96), ("mod", 96), ("A1", 16), ("A2", 16), ("qn", 4), ("kvn", 2),
                      ("cw", 32), ("cb", 8), ("sn", 4), ("mn", 12), ("cond", 16), ("fin", 16), ("b1", 12)):
            colmap[nm] = (_o, n)
            _o += n
        assert _o <= 400

        def col(nm, j=0, n=1):
            o = colmap[nm][0] + j
            return COLS[:, o:o + n]

        cnt = [0]

        def uid(s):
            cnt[0] += 1
            return f"{s}{cnt[0]}"

        def colize(st, src_ap, n, nm, j0=0):
            t = sb(st, uid("cz"), [128, 128], F32)
            key = uid("czk")
            P.dma("sp", lambda e: e.dma_start(out=t[:n, :], in_=src_ap), writes=[key])
            P.op("pe", lambda e: e.matmul(PS[7][:, :n], lhsT=t[:n, :], rhs=IDF[:n, :n], start=True, stop=True),
                 reads=[key, "CF"], writes=["ps7"])
            P.op("dve", lambda e: e.tensor_copy(out=col(nm, j0, n), in_=PS[7][:, :n]), reads=["ps7"], writes=["COLS"])

        def rstd_from_ps(ps_t, out_t, dim, tmp_t, p=128):
            P.op("dve", lambda e: e.tensor_scalar(out=tmp_t[:p, :], in0=ps_t[:p, :], scalar1=1.0 / dim, scalar2=EPS, op0=ALU.mult, op1=ALU.add),
                 reads=[nm_(ps_t)], writes=[nm_(tmp_t)])
            P.op("act", lambda e: e.activation(out=tmp_t[:p, :], in_=tmp_t[:p, :], func=AF.Sqrt), reads=[nm_(tmp_t)], writes=[nm_(tmp_t)])
            P.op("dve", lambda e: e.reciprocal(out=out_t[:p, :], in_=tmp_t[:p, :]), reads=[nm_(tmp_t)], writes=[nm_(out_t)])

        def nm_(t):
            if isinstance(t, str):
                return t
            if isinstance(t, Tile):
                return t.name
            n_ = t.tensor.name if hasattr(t, "tensor") else t.name
            assert n_ != "ARENA", "arena AP passed to nm_"
            return n_

        with ExitStack() as st:
            XI = [sb(st, f"XI{i}", [128, D], F32) for i in range(2)]
            XO = [sb(st, f"XO{i}", [128, 4, 128], F32) for i in range(2)]
            n = 0
            for tt in range(S // 128):
                xi = XI[tt % 2]
                P.dma("sp", lambda e, xi=xi, tt=tt: e.dma_start(out=xi[:], in_=x_in[tt * 128:(tt + 1) * 128, :]), writes=[nm_(xi)])
                for g in range(4):
                    ps = PS[n % 4]
                    xo = XO[n % 2]
                    for j in range(4):
                        kc = g * 4 + j
                        P.op("pe", lambda e, ps=ps, xi=xi, j=j, kc=kc: e.matmul(ps[:, j * 128:(j + 1) * 128], lhsT=xi[:, kc * 128:(kc + 1) * 128], rhs=IDF, start=True, stop=True),
                             reads=[nm_(xi), "CF"], writes=[nm_(ps)], inc=(j == 3))
                    P.op("act", lambda e, ps=ps, xo=xo: e.activation(out=xo[:].rearrange("p a b -> p (a b)"), in_=ps[:, :], func=AF.Copy), reads=[nm_(ps)], writes=[nm_(xo)])
                    P.dma("sp", lambda e, xo=xo, g=g, tt=tt: e.dma_start(
                        out=xT[g * 512:(g + 1) * 512, tt * 128:(tt + 1) * 128].rearrange("(a p) t -> p a t", p=128), in_=xo[:]),
                        reads=[nm_(xo)], writes=["xT"])
                    n += 1
            colize(st, c_in[:, :], 16, "cond")
            P.op("act", lambda e: e.activation(out=col("cond", 0, 16), in_=col("cond", 0, 16), func=AF.Silu), reads=["COLS"], writes=["COLS"])
            colize(st, final_norm[:, :], 16, "fin")
        P.barrier()

        def load_xT_blk(XT, tb, src=xT, q="sp"):
            P.dma(q, lambda e: e.dma_start(out=XT[:], in_=src[:, tb * TB:(tb + 1) * TB].rearrange("(kc p) t -> p kc t", p=128)),
                  reads=[nm_(src)], writes=[nm_(XT)])

        def norm_mod(st_tiles, XT, HT, Acol, Scol, tb, HF=None):
            SQ, tmp, rstd, t2 = st_tiles
            P.op("act", lambda e: e.activation(out=SQ[:].rearrange("p a b -> p (a b)"), in_=XT[:].rearrange("p a b -> p (a b)"), func=AF.Square),
                 reads=[nm_(XT)], writes=[nm_(SQ)])
            for kc in range(NKC):
                P.op("pe", lambda e, kc=kc: e.matmul(PS[6][:, :], lhsT=ONESB, rhs=SQ[:, kc, :], start=(kc == 0), stop=(kc == NKC - 1)),
                     reads=[nm_(SQ), "CB"], writes=["ps6"], inc=(kc == NKC - 1), acc=(kc > 0))
            rstd_from_ps(PS[6], rstd, D, tmp)
            for kc in range(NKC):
                P.op("dve", lambda e, kc=kc: e.scalar_tensor_tensor(out=t2[:], in0=XT[:, kc, :], scalar=Acol(kc), in1=rstd[:], op0=ALU.mult, op1=ALU.mult),
                     reads=[nm_(XT), nm_(rstd), "COLS"], writes=[nm_(t2)])
                if HF is None:
                    P.op("dve", lambda e, kc=kc: e.tensor_scalar(out=HT[:, kc, :], in0=t2[:], scalar1=Scol(kc), scalar2=None, op0=ALU.add),
                         reads=[nm_(t2), "COLS"], writes=[nm_(HT)])
                else:
                    P.op("dve", lambda e, kc=kc: e.tensor_scalar(out=HF[:, kc, :], in0=t2[:], scalar1=Scol(kc), scalar2=None, op0=ALU.add),
                         reads=[nm_(t2), "COLS"], writes=[nm_(HF)])
                    P.op("act", lambda e, kc=kc: e.activation(out=HT[:, kc, :], in_=HF[:, kc, :], func=AF.Copy), reads=[nm_(HF)], writes=[nm_(HT)])

        def group_norm_store(OA, nch, gcolf, dst_rows, tb, SQs, tmp, rstd, stg, dim):
            P.op("act", lambda e: e.activation(out=SQs[:, :nch, :].rearrange("p a b -> p (a b)"), in_=OA[:, :nch, :].rearrange("p a b -> p (a b)"), func=AF.Square),
                 reads=[nm_(OA)], writes=[nm_(SQs)])
            for c in range(nch):
                P.op("pe", lambda e, c=c: e.matmul(PS[6][:, :], lhsT=ONESB, rhs=SQs[:, c, :], start=(c == 0), stop=(c == nch - 1)),
                     reads=[nm_(SQs), "CB"], writes=["ps6"], inc=(c == nch - 1), acc=(c > 0))
            rstd_from_ps(PS[6], rstd, dim, tmp)
            for c in range(nch):
                P.op("dve", lambda e, c=c: e.scalar_tensor_tensor(out=stg[:, c, :], in0=OA[:, c, :], scalar=gcolf(c), in1=rstd[:], op0=ALU.mult, op1=ALU.mult),
                     reads=[nm_(OA), nm_(rstd), "COLS"], writes=[nm_(stg)])
            P.dma("sp", lambda e: e.dma_start(out=catT[dst_rows:dst_rows + nch * 128, tb * TB:(tb + 1) * TB].rearrange("(a p) t -> p a t", p=128), in_=stg[:, :nch, :]),
                  reads=[nm_(stg)], writes=["catT"])

        benv = dict(locals())

        def do_layer(l):
            with ExitStack() as st:
                colize(st, attn_norm[l], 16, "an")
                colize(st, ffn_norm[l], 16, "fn")
                colize(st, mod_b[l], 96, "modb")
                colize(st, mla_q_norm[l], 4, "qn")
                colize(st, mla_kv_norm[l], 2, "kvn")
                colize(st, conv_w[l], 32, "cw")
                colize(st, conv_b[l], 8, "cb")
                colize(st, ssm_norm[l], 4, "sn")
                colize(st, mix_norm[l], 12, "mn")
                MW = [sb(st, f"MW{i}", [128, NKC, 512], F32) for i in range(2)]
                for nb in range(24):
                    mw = MW[nb % 2]
                    P.dma("sp" if nb % 2 == 0 else "act", lambda e, mw=mw, nb=nb: e.dma_start(out=mw[:], in_=mod_w[l][:, nb * 512:(nb + 1) * 512].rearrange("(kc p) n -> p kc n", p=128)),
                          writes=[nm_(mw)])
                    for j in range(4):
                        for kc in range(NKC):
                            P.op("pe", lambda e, mw=mw, j=j, kc=kc: e.matmul(PS[7][:, j:j + 1], lhsT=mw[:, kc, j * 128:(j + 1) * 128], rhs=col("cond", kc, 1), start=(kc == 0), stop=(kc == NKC - 1)),
                                 reads=[nm_(mw), "COLS"], writes=["ps7"], inc=(kc == NKC - 1), acc=(kc > 0))
                    P.op("dve", lambda e, nb=nb: e.tensor_tensor(out=col("mod", nb * 4, 4), in0=PS[7][:, 0:4], in1=col("modb", nb * 4, 4), op=ALU.add),
                         reads=["ps7", "COLS"], writes=["COLS"])
                P.op("dve", lambda e: e.scalar_tensor_tensor(out=col("A1", 0, 16), in0=col("mod", 16, 16), scalar=1.0, in1=col("an", 0, 16), op0=ALU.add, op1=ALU.mult),
                     reads=["COLS"], writes=["COLS"])
                P.op("dve", lambda e: e.scalar_tensor_tensor(out=col("A2", 0, 16), in0=col("mod", 64, 16), scalar=1.0, in1=col("fn", 0, 16), op0=ALU.add, op1=ALU.mult),
                     reads=["COLS"], writes=["COLS"])
            P.barrier()

            with ExitStack() as st:
                XT = sb(st, "XT", [128, NKC, TB], F32)
                HT = sb(st, "HT", [128, NKC, TB], BF16)
                SQ = sb(st, "SQ", [128, NKC, TB], BF16)
                tmp = sb(st, "tmp", [128, TB], F32)
                rstd = sb(st, "rstd", [128, TB], F32)
                t2 = sb(st, "t2", [128, TB], F32)
                WB = [sb(st, f"WB{i}", [128, NKC, 512], BF16) for i in range(2)]
                STG = [sb(st, f"STG{i}", [128, TB], BF16) for i in range(3)]
                STF = sb(st, "STF", [128, TB], F32)
                blocks = []
                blocks.append(([(IN_A, 512)], [("ft", FT_CH["qlat"] + i) for i in range(4)]))
                blocks.append(([(IN_A + 512, 256), (IN_A + 768, 64), (IN_A + 768, 64), (IN_A + 800, 32), (IN_A + 768, 32), (IN_A + 800, 32), (IN_A + 768, 32)],
                               [("ft", FT_CH["kvlat"]), ("ft", FT_CH["kvlat"] + 1), ("ft", FT_CH["kr"]), None]))
                blocks[-1] = (blocks[-1][0], [("ft", FT_CH["kvlat"]), ("ft", FT_CH["kvlat"] + 1), ("ft", FT_CH["kr"]), ("ft64", FT_CH["krs"])])
                blocks.append(([(IN_B, 512)], [("ft", FT_CH["z"] + i) for i in range(4)]))
                blocks.append(([(IN_B + 512, 512)], [("ft", FT_CH["xs"] + i) for i in range(4)]))
                blocks.append(([(IN_B + 1024, 512)], [("ft", FT_CH["bm"]), ("ft", FT_CH["bm"] + 1), ("ft", FT_CH["cm"]), ("ft", FT_CH["cm"] + 1)]))
                blocks.append(([(IN_B + 1536, 8)], [("dtr", 0)]))
                blocks.append(([(IN_C, 512)], [("ft", FT_CH["qc"] + i) for i in range(4)]))
                blocks.append(([(IN_C + 512, 512)], [("ft", FT_CH["kc"] + i) for i in range(4)]))
                blocks.append(([(IN_D, 512)], [("ft", FT_CH["qd"] + i) for i in range(4)]))
                blocks.append(([(IN_D + 512, 512)], [("ft", FT_CH["kd"] + i) for i in range(4)]))
                blocks.append(([(IN_C + 1024, 512)], [("v", VCs)]))
                blocks.append(([(IN_D + 1024, 512)], [("v", VDs)]))
                wl = w_in[l].rearrange("(kc p) n -> p kc n", p=128)
                nw = 0
                ne = 0
                for tb in range(NTB):
                    load_xT_blk(XT, tb)
                    norm_mod((SQ, tmp, rstd, t2), XT, HT, lambda kc: col("A1", kc), lambda kc: col("mod", kc), tb)
                    for segs, dests in blocks:
                        wb = WB[nw % 2]
                        nw += 1
                        o = 0
                        for (c0, n_) in segs:
                            P.dma("pool", lambda e, wb=wb, o=o, c0=c0, n_=n_: e.dma_start(out=wb[:, :, o:o + n_], in_=wl[:, :, c0:c0 + n_]), writes=[nm_(wb)])
                            o += n_
                        if dests[0][0] == "v":
                            for tt in range(4):
                                ps = PS[ne % 4]
                                stg = STG[ne % 3]
                                ne += 1
                                for kc in range(NKC):
                                    P.op("pe", lambda e, ps=ps, wb=wb, kc=kc, tt=tt: e.matmul(ps[:, :], lhsT=HT[:, kc, tt * 128:(tt + 1) * 128], rhs=wb[:, kc, :], start=(kc == 0), stop=(kc == NKC - 1)),
                                         reads=[nm_(wb), "HT"], writes=[nm_(ps)], inc=(kc == NKC - 1), acc=(kc > 0))
                                P.op("act", lambda e, ps=ps, stg=stg: e.activation(out=stg[:], in_=ps[:, :], func=AF.Copy), reads=[nm_(ps)], writes=[nm_(stg)])
                                dstv = dests[0][1]
                                r0 = tb * TB + tt * 128
                                P.dma("sp", lambda e, stg=stg, dstv=dstv, r0=r0: e.dma_start(out=dstv[r0:r0 + 128, :], in_=stg[:]), reads=[nm_(stg)], writes=[nm_(dstv)])
                            continue
                        for j, dst in enumerate(dests):
                            m = 128
                            if dst[0] == "dtr":
                                m = 8
                            ps = PS[ne % 4]
                            stg = STG[ne % 3]
                            ne += 1
                            for kc in range(NKC):
                                P.op("pe", lambda e, ps=ps, wb=wb, kc=kc, j=j, m=m: e.matmul(ps[:m, :], lhsT=wb[:, kc, j * 128:j * 128 + m], rhs=HT[:, kc, :], start=(kc == 0), stop=(kc == NKC - 1)),
                                     reads=[nm_(wb), "HT"], writes=[nm_(ps)], inc=(kc == NKC - 1), acc=(kc > 0))
                            if dst[0] == "dtr":
                                P.op("act", lambda e, ps=ps: e.activation(out=STF[:8, :], in_=ps[:8, :], func=AF.Copy), reads=[nm_(ps)], writes=["STF"])
                                P.dma("sp", lambda e, tb=tb: e.dma_start(out=DTR[:, tb * TB:(tb + 1) * TB], in_=STF[:8, :]), reads=["STF"], writes=["DTR"])
                            else:
                                P.op("act", lambda e, ps=ps, stg=stg: e.activation(out=stg[:], in_=ps[:, :], func=AF.Copy), reads=[nm_(ps)], writes=[nm_(stg)])
                                ch = dst[1]
                                if dst[0] == "ft64":
                                    pass
                                P.dma("sp", lambda e, stg=stg, ch=ch, tb=tb: e.dma_start(out=FT[ch * 128:(ch + 1) * 128, tb * TB:(tb + 1) * TB], in_=stg[:]), reads=[nm_(stg)], writes=["FT"])
            P.barrier()
            k.l = l
            if not dbg_cat:
                mixers(P, k, {**benv, **locals()})
            P.barrier()
            if cat_dump is not None and l == 0:
                P.dma("sp", lambda e: e.dma_start(out=cat_dump[:, :], in_=catT[:, :]), reads=["catT"], writes=["cat_dump"])
                P.barrier()
            tail(P, k, {**benv, **locals()}, n_exp)
            P.barrier()


        for l_ in range(L):
            do_layer(l_)

        if dbg_out:
            for nm2, src, shp, dt_ in (("d_h2T", h2T, [D, S], BF16), ("d_GT", GTs, [NEXP, S], F32), ("d_xT", xT, [D, S], F32)):
                dd = nc.dram_tensor(nm2, shp, dt_, kind="ExternalOutput").ap()
                P.dma("sp", lambda e, dd=dd, src=src: e.dma_start(out=dd[:, :], in_=src[:, :]), reads=[], writes=[nm2])
            P.barrier()
        final(P, k, locals())
        P.finish()
    k.P = P
    return nc, k


def mixers(P, k, env):
    nc = k.nc
    l = k.l
    PS, FT, col, IDF, ONESB, ONESF = env["PS"], env["FT"], env["col"], env["IDF"], env["ONESB"], env["ONESF"]
    IDB, TRIB, ZB, M_LT, M_LE, M_MLA = env["IDB"], env["TRIB"], env["ZB"], env["M_LT"], env["M_LE"], env["M_MLA"]
    sb, nm_, rstd_from_ps, group_norm_store = env["sb"], env["nm_"], env["rstd_from_ps"], env["group_norm_store"]
    wq, wkv, VCs, VDs, DTR, ca_bias, catT = env["wq"], env["wkv"], env["VCs"], env["VDs"], env["DTR"], env["ca_bias"], env["catT"]
    rope_cos, rope_sin, FT2, VMs, SEL = env["rope_cos"], env["rope_sin"], env["FT2"], env["VMs"], env["SEL"]
    dt_bias, a_log, ssm_d, CF = env["dt_bias"], env["a_log"], env["ssm_d"], env["CF"]

    def ftload(dst, h, ch, src=FT, q="sp"):
        P.dma(q, lambda e: e.dma_start(out=dst[:, h, :], in_=src[ch * 128:(ch + 1) * 128, :]), reads=[nm_(src)], writes=[nm_(dst)])

    def mm(out, lhsT, rhs, start, stop, reads, writes, inc=None):
        P.op("pe", lambda e: e.matmul(out, lhsT=lhsT, rhs=rhs, start=start, stop=stop), reads=reads, writes=writes, inc=(stop if inc is None else inc), acc=not start)

    def mla_prep():
      with ExitStack() as st:
        LT = sb(st, "aLT", [128, 6, TB], BF16)
        KRt = sb(st, "aKR", [128, 2, TB], BF16)
        COS = sb(st, "aCOS", [128, TB], F32)
        SIN = sb(st, "aSIN", [128, TB], F32)
        SQ = sb(st, "aSQ", [128, 6, TB], BF16)
        NRM = sb(st, "aNRM", [128, 6, TB], BF16)
        tmp = sb(st, "atmp", [128, TB], F32)
        rstd = sb(st, "arstd", [128, TB], F32)
        t1 = sb(st, "at1", [128, TB], F32)
        t2 = sb(st, "at2", [128, TB], F32)
        WQ = sb(st, "aWQ", [128, 4, 1280], BF16)
        WKV = sb(st, "aWKV", [128, 2, 1536], BF16)
        STG = [sb(st, f"aSTG{i}", [128, TB], BF16) for i in range(3)]
        wql = wq[l].rearrange("(kc p) n -> p kc n", p=128)
        wkl = wkv[l].rearrange("(kc p) n -> p kc n", p=128)
        P.dma("pool", lambda e: e.dma_start(out=WQ[:, :, 0:768], in_=wql[:, :, :]), writes=["aWQ"])
        P.dma("pool", lambda e: e.dma_start(out=WKV[:, :, 0:1024], in_=wkl[:, :, :]), writes=["aWKV"])
        for h in range(4):
            P.dma("pool", lambda e, h=h: e.dma_start(out=WQ[:, :, 768 + 64 * h:768 + 64 * h + 64], in_=wql[:, :, 192 * h + 128:192 * h + 192]), writes=["aWQ"])
            P.dma("pool", lambda e, h=h: e.dma_start(out=WQ[:, :, 1024 + 64 * h:1024 + 64 * h + 32], in_=wql[:, :, 192 * h + 160:192 * h + 192]), writes=["aWQ"])
            P.dma("pool", lambda e, h=h: e.dma_start(out=WQ[:, :, 1024 + 64 * h + 32:1024 + 64 * h + 64], in_=wql[:, :, 192 * h + 128:192 * h + 160]), writes=["aWQ"])
            P.dma("pool", lambda e, h=h: e.dma_start(out=WKV[:, :, 1024 + 128 * h:1024 + 128 * h + 128], in_=wkl[:, :, 256 * h + 128:256 * h + 256]), writes=["aWKV"])
        ne = 0
        for tb in range(NTB):
            sl = slice(tb * TB, (tb + 1) * TB)
            for c in range(6):
                P.dma("sp", lambda e, c=c, sl=sl: e.dma_start(out=LT[:, c, :], in_=FT[(FT_CH["qlat"] + c) * 128:(FT_CH["qlat"] + c + 1) * 128, sl]), reads=["FT"], writes=["aLT"])
            for c in range(2):
                P.dma("sp", lambda e, c=c, sl=sl: e.dma_start(out=KRt[:, c, :], in_=FT[(FT_CH["kr"] + c) * 128:(FT_CH["kr"] + c + 1) * 128, sl]), reads=["FT"], writes=["aKR"])
            P.dma("act", lambda e, sl=sl: e.dma_start(out=COS[:], in_=rope_cos[:, sl]), writes=["aCOS"])
            P.dma("act", lambda e, sl=sl: e.dma_start(out=SIN[:], in_=rope_sin[:, sl]), writes=["aSIN"])
            P.op("act", lambda e: e.activation(out=SQ[:].rearrange("p a b -> p (a b)"), in_=LT[:].rearrange("p a b -> p (a b)"), func=AF.Square), reads=["aLT"], writes=["aSQ"])
            for (c0, n, gname, dim) in ((0, 4, "qn", 512), (4, 2, "kvn", 256)):
                for c in range(n):
                    mm(PS[6][:, :], ONESB, SQ[:, c0 + c, :], c == 0, c == n - 1, ["aSQ", "CB"], ["ps6"])
                rstd_from_ps(PS[6], rstd, dim, tmp)
                for c in range(n):
                    P.op("dve", lambda e, c=c, c0=c0, gname=gname: e.scalar_tensor_tensor(out=NRM[:, c0 + c, :], in0=LT[:, c0 + c, :], scalar=col(gname, c), in1=rstd[:], op0=ALU.mult, op1=ALU.mult),
                         reads=["aLT", "arstd", "COLS"], writes=["aNRM"])
            for h in range(8):
                ps = PS[ne % 4]
                stg = STG[ne % 3]
                ne += 1
                if h < 4:
                    for kc in range(4):
                        mm(ps[:, :], WQ[:, kc, 192 * h:192 * h + 128], NRM[:, kc, :], kc == 0, kc == 3, ["aWQ", "aNRM"], [nm_(ps)])
                    row = h
                else:
                    hh = h - 4
                    for kc in range(2):
                        mm(ps[:, :], WKV[:, kc, 256 * hh:256 * hh + 128], NRM[:, 4 + kc, :], kc == 0, kc == 1, ["aWKV", "aNRM"], [nm_(ps)])
                    row = 6 + hh
                P.op("act", lambda e, ps=ps, stg=stg: e.activation(out=stg[:], in_=ps[:, :], func=AF.Copy), reads=[nm_(ps)], writes=[nm_(stg)])
                P.dma("sp", lambda e, stg=stg, row=row, sl=sl: e.dma_start(out=FT2[row * 128:(row + 1) * 128, sl], in_=stg[:]), reads=[nm_(stg)], writes=["FT2"])
            for ab in range(2):
                psa, pss = PS[4], PS[5]
                for kc in range(4):
                    mm(psa[:, :], WQ[:, kc, 768 + 128 * ab:768 + 128 * ab + 128], NRM[:, kc, :], kc == 0, kc == 3, ["aWQ", "aNRM"], ["ps4"])
                for kc in range(4):
                    mm(pss[:, :], WQ[:, kc, 1024 + 128 * ab:1024 + 128 * ab + 128], NRM[:, kc, :], kc == 0, kc == 3, ["aWQ", "aNRM"], ["ps5"])
                stg = STG[ne % 3]
                ne += 1
                P.op("dve", lambda e: e.tensor_tensor(out=t1[:], in0=psa[:, :], in1=COS[:], op=ALU.mult), reads=["ps4", "aCOS"], writes=["at1"])
                P.op("dve", lambda e: e.tensor_tensor(out=t2[:], in0=pss[:, :], in1=SIN[:], op=ALU.mult), reads=["ps5", "aSIN"], writes=["at2"])
                P.op("dve", lambda e, stg=stg: e.tensor_tensor(out=stg[:], in0=t1[:], in1=t2[:], op=ALU.add), reads=["at1", "at2"], writes=[nm_(stg)])
                P.dma("sp", lambda e, stg=stg, ab=ab, sl=sl: e.dma_start(out=FT2[(4 + ab) * 128:(5 + ab) * 128, sl], in_=stg[:]), reads=[nm_(stg)], writes=["FT2"])
            stg = STG[ne % 3]
            ne += 1
            P.op("dve", lambda e: e.tensor_tensor(out=t1[:], in0=KRt[:, 0, :], in1=COS[:], op=ALU.mult), reads=["aKR", "aCOS"], writes=["at1"])
            P.op("dve", lambda e: e.tensor_tensor(out=t2[:], in0=KRt[:, 1, :], in1=SIN[:], op=ALU.mult), reads=["aKR", "aSIN"], writes=["at2"])
            P.op("dve", lambda e, stg=stg: e.tensor_tensor(out=stg[:], in0=t1[:], in1=t2[:], op=ALU.add), reads=["at1", "at2"], writes=[nm_(stg)])
            P.dma("sp", lambda e, stg=stg, sl=sl: e.dma_start(out=FT2[10 * 128:11 * 128, sl], in_=stg[:]), reads=[nm_(stg)], writes=["FT2"])
            for tt in range(4):
                ps = PS[ne % 4]
                stg = STG[ne % 3]
                ne += 1
                for kc in range(2):
                    mm(ps[:, :], NRM[:, 4 + kc, tt * 128:(tt + 1) * 128], WKV[:, kc, 1024:1536], kc == 0, kc == 1, ["aWKV", "aNRM"], [nm_(ps)])
                P.op("act", lambda e, ps=ps, stg=stg: e.activation(out=stg[:], in_=ps[:, :], func=AF.Copy), reads=[nm_(ps)], writes=[nm_(stg)])
                r0 = tb * TB + tt * 128
                P.dma("sp", lambda e, stg=stg, r0=r0: e.dma_start(out=VMs[r0:r0 + 128, :], in_=stg[:]), reads=[nm_(stg)], writes=["VM"])
    mla_prep()
    P.barrier()

    def attn_phase(kind):
        with ExitStack() as st:
            Q = sb(st, "tQ", [128, 4, S], BF16)
            Kt = sb(st, "tK", [128, 4, S], BF16)
            V = sb(st, "tV", [128, 32, 512], BF16)
            OA = sb(st, "tOA", [128, 4, TB], F32)
            SQs = sb(st, "tSQs", [128, 4, TB], BF16)
            tmp = sb(st, "ttmp", [128, TB], F32)
            rstd = sb(st, "trstd", [128, TB], F32)
            stg = sb(st, "tstg", [128, 4, TB], BF16)
            ATT = [sb(st, f"tATT{i}", [128, TB], BF16) for i in range(3)]
            RD = sb(st, "tRD", [128, TB], F32)
            if kind == "mla":
                QR = sb(st, "tQR", [128, 2, S], BF16)
                KRD = sb(st, "tKRD", [128, 1, S], BF16)
                for h in range(4):
                    ftload(Q, h, h, FT2)
                    ftload(Kt, h, 6 + h, FT2, "act")
                ftload(QR, 0, 4, FT2)
                ftload(QR, 1, 5, FT2)
                ftload(KRD, 0, 10, FT2)
                vsrc, scale, mrow, dst = VMs, 192.0 ** -0.5, 0, 0
            elif kind == "sb":
                for h in range(4):
                    ftload(Q, h, FT_CH["qc"] + h)
                    ftload(Kt, h, FT_CH["kc"] + h, FT, "act")
                vsrc, scale, mrow, dst = VCs, 128.0 ** -0.5, 4, 1024
                CAC = sb(st, "tCAC", [128, TB], F32)
                E1 = sb(st, "tE1", [128, TB], F32)
                SP = sb(st, "tSP", [128, TB], F32)
                NLK = sb(st, "tNLK", [128, TB], BF16)
                T1 = sb(st, "tT1", [128, TB], F32)
            else:
                for h in range(4):
                    ftload(Q, h, FT_CH["qd"] + h)
                    ftload(Kt, h, FT_CH["kd"] + h, FT, "act")
                vsrc, scale, mrow, dst = VDs, 128.0 ** -0.5, 8, 1536
                BT = sb(st, "tBT", [128, 20, 128], F32)
                P.dma("sp", lambda e: e.dma_start(out=BT[:], in_=ca_bias[l].rearrange("h j k q -> k (h j) q")), writes=["tBT"])
                P.op("dve", lambda e: e.tensor_scalar(out=BT[:].rearrange("p a b -> p (a b)"), in0=BT[:].rearrange("p a b -> p (a b)"), scalar1=float(128.0 ** 0.5), scalar2=None, op0=ALU.mult), reads=["tBT"], writes=["tBT"])
            P.dma("sp", lambda e: e.dma_start(out=V[:], in_=vsrc.rearrange("(b p) c -> p b c", p=128)), reads=[nm_(vsrc)], writes=["tV"])
            n = 0
            for QB in range(NTB):
                for h in range(4):
                    if kind == "ca":
                        for qi in range(4):
                            qt = QB * 4 + qi
                            blks = [jb for jb in range(5) if qt - 4 + jb >= 0]
                            for idx, jb in enumerate(blks):
                                kb = qt - 4 + jb
                                ps = PS[n % 2]
                                att = ATT[n % 3]
                                n += 1
                                mm(ps[:, :128], Kt[:, h, kb * 128:(kb + 1) * 128], Q[:, h, qt * 128:(qt + 1) * 128], True, False, ["tK", "tQ"], [nm_(ps)])
                                mm(ps[:, :128], IDF, BT[:, h * 5 + jb, :], False, True, ["tBT", "CF"], [nm_(ps)])
                                P.op("act", lambda e, ps=ps, att=att: e.activation(out=att[:, :128], in_=ps[:, :128], func=AF.Exp, scale=float(scale)), reads=[nm_(ps)], writes=[nm_(att)])
                                last = idx == len(blks) - 1
                                mm(PS[2][:, qi * 128:(qi + 1) * 128], V[:, kb, h * 128:(h + 1) * 128], att[:, :128], idx == 0, last, ["tV", nm_(att)], ["ps2"], inc=False)
                                mm(PS[3][:, qi * 128:(qi + 1) * 128], ONESB, att[:, :128], idx == 0, last, ["CB", nm_(att)], ["ps3"], inc=True)
                    elif kind == "mla":
                        nkb = 4 * QB + 4
                        for kb in range(nkb):
                            j = kb - 4 * QB
                            q0 = 128 * max(j, 0)
                            nn = TB - q0
                            qs = slice(QB * TB + q0, (QB + 1) * TB)
                            ps = PS[n % 2]
                            att = ATT[n % 3]
                            n += 1
                            r0 = 64 * (h % 2)
                            mm(ps[:, :nn], Kt[:, h, kb * 128:(kb + 1) * 128], Q[:, h, qs], True, False, ["tK", "tQ"], [nm_(ps)])
                            mm(ps[:, :nn], KRD[r0:r0 + 64, 0, kb * 128:(kb + 1) * 128], QR[r0:r0 + 64, h // 2, qs], False, True, ["tKRD", "tQR"], [nm_(ps)])
                            P.op("act", lambda e, ps=ps, att=att, nn=nn: e.activation(out=att[:, :nn], in_=ps[:, :nn], func=AF.Exp, scale=float(scale)), reads=[nm_(ps)], writes=[nm_(att)])
                            if j >= 0:
                                P.op("dve", lambda e, att=att: e.tensor_tensor(out=att[:, :128], in0=att[:, :128], in1=M_MLA, op=ALU.mult), reads=[nm_(att), "CF"], writes=[nm_(att)])
                            last = kb == nkb - 1
                            mm(PS[2][:, q0:], V[:, kb, h * 128:(h + 1) * 128], att[:, :nn], kb == 0, last, ["tV", nm_(att)], ["ps2"], inc=False)
                            mm(PS[3][:, q0:], ONESB, att[:, :nn], kb == 0, last, ["CB", nm_(att)], ["ps3"], inc=True)
                    else:
                        P.op("pool", lambda e: e.memset(CAC[:], 0.0), writes=["tCAC"])
                        mm(PS[2][:, :], ZB, Q[:, 0, 0:TB], True, False, ["CB", "tQ"], ["ps2"], inc=True)
                        nkb = 4 * QB + 4
                        for kb in range(nkb - 1, -1, -1):
                            j = kb - 4 * QB
                            q0 = 128 * max(j, 0)
                            nn = TB - q0
                            qs = slice(QB * TB + q0, (QB + 1) * TB)
                            ps = PS[n % 2]
                            att = ATT[n % 3]
                            n += 1
                            mm(ps[:, :nn], Kt[:, h, kb * 128:(kb + 1) * 128], Q[:, h, qs], True, True, ["tK", "tQ"], [nm_(ps)])
                            P.op("act", lambda e, ps=ps, nn=nn: e.activation(out=E1[:, :nn], in_=ps[:, :nn], func=AF.Exp, scale=float(-scale)), reads=[nm_(ps)], writes=["tE1"])
                            P.op("act", lambda e, nn=nn: e.activation(out=SP[:, :nn], in_=E1[:, :nn], func=AF.Ln, bias=1.0), reads=["tE1"], writes=["tSP"])
                            P.op("dve", lambda e, ps=ps, nn=nn: e.scalar_tensor_tensor(out=NLK[:, :nn], in0=ps[:, :nn], scalar=float(scale), in1=SP[:, :nn], op0=ALU.mult, op1=ALU.add), reads=[nm_(ps), "tSP"], writes=["tNLK"])
                            if j >= 0:
                                P.op("dve", lambda e: e.tensor_tensor(out=NLK[:, :128], in0=NLK[:, :128], in1=M_LT, op=ALU.mult), reads=["tNLK", "CF"], writes=["tNLK"])
                            mm(PS[4][:, :nn], TRIB, NLK[:, :nn], True, True, ["CB", "tNLK"], ["ps4"])
                            mm(PS[5][:, :nn], ONESB, NLK[:, :nn], True, True, ["CB", "tNLK"], ["ps5"])
                            P.op("dve", lambda e, nn=nn: e.tensor_tensor(out=T1[:, :nn], in0=PS[4][:, :nn], in1=SP[:, :nn], op=ALU.add), reads=["ps4", "tSP"], writes=["tT1"])
                            P.op("dve", lambda e, nn=nn, q0=q0: e.tensor_tensor(out=T1[:, :nn], in0=T1[:, :nn], in1=CAC[:, q0:], op=ALU.add), reads=["tT1", "tCAC"], writes=["tT1"])
                            P.op("act", lambda e, att=att, nn=nn: e.activation(out=att[:, :nn], in_=T1[:, :nn], func=AF.Exp, scale=-1.0), reads=["tT1"], writes=[nm_(att)])
                            if j >= 0:
                                P.op("dve", lambda e, att=att: e.tensor_tensor(out=att[:, :128], in0=att[:, :128], in1=M_LT, op=ALU.mult), reads=[nm_(att), "CF"], writes=[nm_(att)])
                            P.op("dve", lambda e, nn=nn, q0=q0: e.tensor_tensor(out=CAC[:, q0:], in0=CAC[:, q0:], in1=PS[5][:, :nn], op=ALU.add), reads=["ps5", "tCAC"], writes=["tCAC"])
                            mm(PS[2][:, q0:], V[:, kb, h * 128:(h + 1) * 128], att[:, :nn], False, kb == 0, ["tV", nm_(att)], ["ps2"], inc=True)
                    if kind == "sb":
                        P.op("act", lambda e, h=h: e.activation(out=OA[:, h, :], in_=PS[2][:, :], func=AF.Copy), reads=["ps2"], writes=["tOA"])
                    else:
                        P.op("dve", lambda e: e.reciprocal(out=RD[:], in_=PS[3][:, :]), reads=["ps3"], writes=["tRD"])
                        P.op("dve", lambda e, h=h: e.tensor_tensor(out=OA[:, h, :], in0=PS[2][:, :], in1=RD[:], op=ALU.mult), reads=["ps2", "tRD"], writes=["tOA"])
                group_norm_store(OA, 4, lambda c: col("mn", mrow + c), dst, QB, SQs, tmp, rstd, stg, 512)
        P.barrier()

    attn_phase("mla")
    attn_phase("sb")
    attn_phase("ca")
    ssd_phase(P, k, env, mm, ftload)
    P.barrier()


def ssd_phase(P, k, env, mm, ftload):
    nc = k.nc
    l = k.l
    PS, FT, col, IDF, ONESB, IDB, M_LE = env["PS"], env["FT"], env["col"], env["IDF"], env["ONESB"], env["IDB"], env["M_LE"]
    sb, nm_, rstd_from_ps = env["sb"], env["nm_"], env["rstd_from_ps"]
    DTR, catT, SEL, dt_bias, a_log, ssm_d, ssm_norm8 = env["DTR"], env["catT"], env["SEL"], env["dt_bias"], env["a_log"], env["ssm_d"], env["ssm_norm8"]
    XSs, CSs, CSCs, DTCs = env["XSs"], env["CSs"], env["CSCs"], env["DTCs"]
    def ssd_a():
      with ExitStack() as st:
        XC = sb(st, "sXC", [128, 3 + S], BF16)
        ACC = sb(st, "sACC", [128, S], F32)
        XF = sb(st, "sXF", [128, S], BF16)
        T1 = sb(st, "sT1", [128, S], F32)
        T2 = sb(st, "sT2", [128, S], F32)
        T3 = sb(st, "sT3", [128, S], F32)
        XO = [sb(st, f"sXO{i}", [128, 512], BF16) for i in range(2)]
        SM = sb(st, "sSM", [8, 4], F32)
        CC = sb(st, "sCC", [128, 32, 8], F32)
        P.op("pool", lambda e: e.memset(XC[:, 0:3], 0.0), writes=["sXC"])
        for c in range(8):
            ch = FT_CH["xs"] + c
            P.dma("sp", lambda e, ch=ch: e.dma_start(out=XC[:, 3:3 + S], in_=FT[ch * 128:(ch + 1) * 128, :]), reads=["FT"], writes=["sXC"])
            P.op("dve", lambda e, c=c: e.tensor_scalar(out=ACC[:], in0=XC[:, 3:3 + S], scalar1=col("cw", 3 * 8 + c), scalar2=None, op0=ALU.mult), reads=["sXC", "COLS"], writes=["sACC"])
            for j in (2, 1, 0):
                P.op("dve", lambda e, c=c, j=j: e.scalar_tensor_tensor(out=ACC[:], in0=XC[:, j:j + S], scalar=col("cw", j * 8 + c), in1=ACC[:], op0=ALU.mult, op1=ALU.add), reads=["sXC", "sACC", "COLS"], writes=["sACC"])
            P.op("act", lambda e, c=c: e.activation(out=XF[:], in_=ACC[:], func=AF.Silu, bias=col("cb", c), scale=1.0), reads=["sACC", "COLS"], writes=["sXF"])
            if c >= 4:
                P.dma("sp", lambda e, ch=ch: e.dma_start(out=FT[ch * 128:(ch + 1) * 128, :], in_=XF[:]), reads=["sXF"], writes=["FT"])
            else:
                for blk in range(32):
                    ps = PS[blk % 4]
                    xo = XO[blk % 2]
                    mm(ps[:, :128], XF[:, blk * 128:(blk + 1) * 128], IDB, True, True, ["sXF", "CB"], [nm_(ps)])
                    P.op("act", lambda e, ps=ps, xo=xo: e.activation(out=xo[:, :128], in_=ps[:, :128], func=AF.Copy), reads=[nm_(ps)], writes=[nm_(xo)])
                    P.dma("sp", lambda e, xo=xo, blk=blk, c=c: e.dma_start(out=XSs[blk * 128:(blk + 1) * 128, c * 128:(c + 1) * 128], in_=xo[:, :128]), reads=[nm_(xo)], writes=["XSs"])
        P.dma("sp", lambda e: e.dma_start(out=T1[:8, :], in_=DTR[:, :]), reads=["DTR"], writes=["sT1"])
        P.dma("sp", lambda e: e.dma_start(out=SM[:, 0:1], in_=dt_bias[l]), writes=["sSM"])
        P.dma("sp", lambda e: e.dma_start(out=SM[:, 1:2], in_=a_log[l]), writes=["sSM"])
        P.op("act", lambda e: e.activation(out=SM[:, 2:3], in_=SM[:, 1:2], func=AF.Exp), reads=["sSM"], writes=["sSM"])
        P.op("act", lambda e: e.activation(out=T1[:8, :], in_=T1[:8, :], func=AF.Exp, bias=SM[:, 0:1], scale=1.0), reads=["sT1", "sSM"], writes=["sT1"])
        P.op("act", lambda e: e.activation(out=T1[:8, :], in_=T1[:8, :], func=AF.Ln, bias=1.0), reads=["sT1"], writes=["sT1"])
        P.op("dve", lambda e: e.tensor_scalar(out=T2[:8, :], in0=T1[:8, :], scalar1=SM[:, 2:3], scalar2=-1.0, op0=ALU.mult, op1=ALU.mult), reads=["sT1", "sSM"], writes=["sT2"])
        P.op("pool", lambda e: e.memset(T3[:8, :], 1.0), writes=["sT3"])
        P.op("dve", lambda e: e.tensor_tensor_scan(out=ACC[:8, :], data0=T3[:8, :], data1=T2[:8, :], initial=0.0, op0=ALU.mult, op1=ALU.add), reads=["sT2", "sT3"], writes=["sACC"])
        P.dma("sp", lambda e: e.dma_start(out=CSs[:, :], in_=ACC[:8, :]), reads=["sACC"], writes=["CSs"])
        for (src, key, dstd) in ((ACC, "sACC", CSCs), (T1, "sT1", DTCs)):
            for blk in range(32):
                mm(PS[blk % 4][:, :8], src[:8, blk * 128:(blk + 1) * 128], IDF[:8, :8], True, True, [key, "CF"], [nm_(PS[blk % 4])])
                P.op("dve", lambda e, blk=blk: e.tensor_copy(out=CC[:, blk, :], in_=PS[blk % 4][:, :8]), reads=[nm_(PS[blk % 4])], writes=["sCC"])
            P.dma("sp", lambda e, dstd=dstd: e.dma_start(out=dstd[:, :], in_=CC[:].rearrange("p a b -> p (a b)")), reads=["sCC"], writes=[nm_(dstd)])
    ssd_a()
    P.barrier()
    def ssd_b():
      with ExitStack() as st:
        BTt = sb(st, "uB", [128, 2, S], BF16)
        CTt = sb(st, "uC", [128, 2, S], BF16)
        XS = sb(st, "uXS", [128, 32, 512], BF16)
        CSr = sb(st, "uCSr", [8, S], F32)
        CSC = sb(st, "uCSC", [128, 32, 8], F32)
        DTC = sb(st, "uDTC", [128, 32, 8], F32)
        CSB = sb(st, "uCSB", [128, 8, TB], F32)
        DIF = [sb(st, f"uDIF{i}", [128, TB], F32) for i in range(2)]
        W = [sb(st, f"uW{i}", [128, TB], BF16) for i in range(3)]
        ZT = sb(st, "uZT", [64, 8, TB], BF16)
        SZ = sb(st, "uSZ", [64, TB], F32)
        OAh = sb(st, "uOA", [64, 8, TB], F32)
        SQs = sb(st, "uSQs", [64, 8, TB], BF16)
        stg = sb(st, "ustg", [64, 8, TB], BF16)
        tmp = sb(st, "utmp", [128, TB], F32)
        rstd = sb(st, "urstd", [128, TB], F32)
        DSK = sb(st, "uDSK", [128, 8], F32)
        DI = sb(st, "uDI", [128, 8, 128], F32)
        G8 = sb(st, "uG8", [64, 8], F32)
        G8t = sb(st, "uG8t", [8, 64], F32)
        for g in range(2):
            ftload(BTt, g, FT_CH["bm"] + g)
            ftload(CTt, g, FT_CH["cm"] + g, FT, "act")
        P.dma("sp", lambda e: e.dma_start(out=XS[:], in_=XSs.rearrange("(b p) c -> p b c", p=128)), reads=["XSs"], writes=["uXS"])
        P.dma("sp", lambda e: e.dma_start(out=CSr[:], in_=CSs[:, :]), reads=["CSs"], writes=["uCSr"])
        P.dma("sp", lambda e: e.dma_start(out=CSC[:].rearrange("p a b -> p (a b)"), in_=CSCs[:, :]), reads=["CSCs"], writes=["uCSC"])
        P.dma("sp", lambda e: e.dma_start(out=DTC[:].rearrange("p a b -> p (a b)"), in_=DTCs[:, :]), reads=["DTCs"], writes=["uDTC"])
        P.dma("sp", lambda e: e.dma_start(out=DSK[:], in_=ssm_d[l:l + 1, :].partition_broadcast(128)), writes=["uDSK"])
        P.dma("sp", lambda e: e.dma_start(out=G8t[:], in_=ssm_norm8[l]), writes=["uG8t"])
        mm(PS[7][:64, :8], G8t[:, :], IDF[:8, :8], True, True, ["uG8t", "CF"], ["ps7"])
        P.op("dve", lambda e: e.tensor_copy(out=G8[:], in_=PS[7][:64, :8]), reads=["ps7"], writes=["uG8"])
        for h in range(8):
            P.op("dve", lambda e, h=h: e.tensor_scalar(out=DI[:, h, :], in0=IDF, scalar1=DSK[:, h:h + 1], scalar2=None, op0=ALU.mult), reads=["CF", "uDSK"], writes=["uDI"])
        n = 0
        for QB in range(NTB):
            sl = slice(QB * TB, (QB + 1) * TB)
            for h in range(8):
                mm(PS[7][:, :], SEL[:, h * 128:(h + 1) * 128], CSr[:, sl], True, True, ["SEL", "uCSr"], ["ps7"])
                P.op("act", lambda e, h=h: e.activation(out=CSB[:, h, :], in_=PS[7][:, :], func=AF.Copy), reads=["ps7"], writes=["uCSB"])
                P.dma("act", lambda e, h=h, sl=sl: e.dma_start(out=ZT[:, h, :], in_=FT[FT_CH["z"] * 128 + 64 * h:FT_CH["z"] * 128 + 64 * h + 64, sl]), reads=["FT"], writes=["uZT"])
            for g in range(2):
                nkb = 4 * QB + 4
                for kb in range(nkb):
                    j = kb - 4 * QB
                    q0 = 128 * max(j, 0)
                    nn = TB - q0
                    gp = PS[n % 2]
                    n += 1
                    mm(gp[:, :nn], BTt[:, g, kb * 128:(kb + 1) * 128], CTt[:, g, QB * TB + q0:(QB + 1) * TB], True, True, ["uB", "uC"], [nm_(gp)])
                    for r in range(4):
                        h = 4 * g + r
                        dif = DIF[(n + r) % 2]
                        w = W[(n + r) % 3]
                        if j >= 0:
                            P.op("dve", lambda e, dif=dif, h=h, kb=kb, q0=q0, nn=nn: e.tensor_scalar(out=dif[:, :nn], in0=CSB[:, h, q0:], scalar1=CSC[:, kb, h:h + 1], scalar2=0.0, op0=ALU.subtract, op1=ALU.min), reads=["uCSB", "uCSC"], writes=[nm_(dif)])
                        else:
                            P.op("dve", lambda e, dif=dif, h=h, kb=kb, q0=q0, nn=nn: e.tensor_scalar(out=dif[:, :nn], in0=CSB[:, h, q0:], scalar1=CSC[:, kb, h:h + 1], scalar2=None, op0=ALU.subtract), reads=["uCSB", "uCSC"], writes=[nm_(dif)])
                        P.op("act", lambda e, dif=dif, nn=nn: e.activation(out=dif[:, :nn], in_=dif[:, :nn], func=AF.Exp), reads=[nm_(dif)], writes=[nm_(dif)])
                        P.op("dve", lambda e, dif=dif, w=w, gp=gp, h=h, kb=kb, nn=nn: e.scalar_tensor_tensor(out=w[:, :nn], in0=dif[:, :nn], scalar=DTC[:, kb, h:h + 1], in1=gp[:, :nn], op0=ALU.mult, op1=ALU.mult), reads=[nm_(dif), nm_(gp), "uDTC"], writes=[nm_(w)])
                        if j >= 0:
                            P.op("dve", lambda e, w=w: e.tensor_tensor(out=w[:, :128], in0=w[:, :128], in1=M_LE, op=ALU.mult), reads=[nm_(w), "CF"], writes=[nm_(w)])
                            P.op("dve", lambda e, w=w, h=h: e.tensor_tensor(out=w[:, :128], in0=w[:, :128], in1=DI[:, h, :], op=ALU.add), reads=[nm_(w), "uDI"], writes=[nm_(w)])
                        mm(PS[2 + r][:64, q0:], XS[:, kb, h * 64:(h + 1) * 64], w[:, :nn], kb == 0, kb == nkb - 1, ["uXS", nm_(w)], [f"ps{2 + r}"], inc=True)
                for r in range(4):
                    h = 4 * g + r
                    P.op("act", lambda e, h=h: e.activation(out=SZ[:], in_=ZT[:, h, :], func=AF.Silu), reads=["uZT"], writes=["uSZ"])
                    P.op("dve", lambda e, h=h, r=r: e.tensor_tensor(out=OAh[:, h, :], in0=PS[2 + r][:64, :], in1=SZ[:], op=ALU.mult), reads=[f"ps{2 + r}", "uSZ"], writes=["uOA"])
                    P.op("act", lambda e, h=h: e.activation(out=SQs[:, h, :], in_=OAh[:, h, :], func=AF.Square), reads=["uOA"], writes=["uSQs"])
                for r in range(4):
                    mm(PS[6][:, :], ONESB[0:64, :], SQs[:, 4 * g + r, :], r == 0, r == 3, ["uSQs", "CB"], ["ps6"])
                rstd_from_ps(PS[6], rstd, 256, tmp)
                for r in range(4):
                    h = 4 * g + r
                    P.op("dve", lambda e, h=h: e.scalar_tensor_tensor(out=stg[:, h, :], in0=OAh[:, h, :], scalar=G8[:, h:h + 1], in1=rstd[:64, :], op0=ALU.mult, op1=ALU.mult), reads=["uOA", "urstd", "uG8"], writes=["ustg"])
            P.dma("sp", lambda e, sl=sl: e.dma_start(out=catT[512:1024, sl].rearrange("(h p) t -> p h t", p=64), in_=stg[:]), reads=["ustg"], writes=["catT"])
    ssd_b()


def tail(P, k, env, n_exp):
    nc = k.nc
    l = k.l
    PS, xT, col, IDF, ONESB, ONESF = env["PS"], env["xT"], env["col"], env["IDF"], env["ONESB"], env["ONESF"]
    sb, nm_, norm_mod, load_xT_blk = env["sb"], env["nm_"], env["norm_mod"], env["load_xT_blk"]
    catT, h2T, GTs, w_out, router_w, router_b = env["catT"], env["h2T"], env["GTs"], env["w_out"], env["router_w"], env["router_b"]
    w1, b1, w2, b2, sel32 = env["w1"], env["b1"], env["w2"], env["b2"], env["sel32"]
    def p3():
      with ExitStack() as st:
        CT = sb(st, "oCT", [128, NKC, TB], BF16)
        XT = sb(st, "oXT", [128, NKC, TB], F32)
        WO = [sb(st, f"oWO{i}", [128, NKC, 512], BF16) for i in range(2)]
        wl = w_out[l].rearrange("(kc p) n -> p kc n", p=128)
        nw = 0
        for tb in range(NTB):
            load_xT_blk(CT, tb, src=catT, q="act")
            load_xT_blk(XT, tb)
            for nb in range(4):
                wo = WO[nw % 2]
                nw += 1
                P.dma("pool", lambda e, wo=wo, nb=nb: e.dma_start(out=wo[:], in_=wl[:, :, nb * 512:(nb + 1) * 512]), writes=[nm_(wo)])
                for j in range(4):
                    n = nb * 4 + j
                    ps = PS[n % 4]
                    for kc in range(NKC):
                        P.op("pe", lambda e, ps=ps, wo=wo, kc=kc, j=j: e.matmul(ps[:, :], lhsT=wo[:, kc, j * 128:(j + 1) * 128], rhs=CT[:, kc, :], start=(kc == 0), stop=(kc == NKC - 1)),
                             reads=[nm_(wo), "oCT"], writes=[nm_(ps)], inc=(kc == NKC - 1), acc=(kc > 0))
                    P.op("dve", lambda e, ps=ps, n=n: e.scalar_tensor_tensor(out=XT[:, n, :], in0=ps[:, :], scalar=col("mod", 32 + n), in1=XT[:, n, :], op0=ALU.mult, op1=ALU.add),
                         reads=[nm_(ps), "oXT", "COLS"], writes=["oXT"])
            P.dma("sp", lambda e, tb=tb: e.dma_start(out=xT[:, tb * TB:(tb + 1) * TB].rearrange("(kc p) t -> p kc t", p=128), in_=XT[:]), reads=["oXT"], writes=["xT"])
    p3()
    P.barrier()
    import os
    if os.environ.get("SKIP_P4"):
        return
    if getattr(k, "dbg_out", False):
        dd0 = nc.dram_tensor("d_x1T", [D, S], F32, kind="ExternalOutput").ap()
        P.dma("sp", lambda e: e.dma_start(out=dd0[:, :], in_=xT[:, :]), reads=[], writes=["d_x1T"])
        P.barrier()
    def p4a():
      with ExitStack() as st:
        XT = sb(st, "rXT", [128, NKC, TB], F32)
        HF = sb(st, "rHF", [128, NKC, TB], F32)
        HT = sb(st, "rHT", [128, NKC, TB], BF16)
        SQ = sb(st, "rSQ", [128, NKC, TB], BF16)
        tmp = sb(st, "rtmp", [128, TB], F32)
        rstd = sb(st, "rrstd", [128, TB], F32)
        t2 = sb(st, "rt2", [128, TB], F32)
        RW = sb(st, "rRW", [128, NKC, NEXP], F32)
        RB = sb(st, "rRB", [1, NEXP], F32)
        LG = sb(st, "rLG", [128, NEXP], F32)
        M8 = sb(st, "rM8", [128, 8], F32)
        NM = sb(st, "rNM", [128, 1], F32)
        MK = sb(st, "rMK", [128, NEXP], F32)
        EE = sb(st, "rEE", [128, NEXP], F32)
        SM = sb(st, "rSM", [128, 1], F32)
        GG = sb(st, "rGG", [128, NEXP], F32)
        GTt = sb(st, "rGT", [32, TB], F32)
        RBB = sb(st, "rRBB", [128, NEXP], F32)
        if not os.environ.get("SKIP_RW"):
            P.dma("sp", lambda e: e.dma_start(out=RW[:], in_=router_w[l].rearrange("(kc p) n -> p kc n", p=128)), writes=["rRW"])
        if not os.environ.get("SKIP_RBB"):
            P.dma("sp", lambda e: e.dma_start(out=RBB[:], in_=router_b[l].partition_broadcast(128)), writes=["rRBB"])
        for tb in range(NTB):
            if not os.environ.get("SKIP_XTLOAD"):
                load_xT_blk(XT, tb)
            if not os.environ.get("SKIP_NORM2"):
                norm_mod((SQ, tmp, rstd, t2), XT, HT, lambda kc: col("A2", kc), lambda kc: col("mod", 48 + kc), tb, HF=(None if os.environ.get("NO_HF") else HF))
            if not os.environ.get("SKIP_H2T"):
                P.dma("sp", lambda e, tb=tb: e.dma_start(out=h2T[:, tb * TB:(tb + 1) * TB].rearrange("(kc p) t -> p kc t", p=128), in_=HT[:]), reads=["rHT"], writes=["h2T"])
            if os.environ.get("SKIP_ROUTER"):
                continue
            for tt in range(4):
                ps = PS[tt % 2]
                for kc in range(NKC):
                    P.op("pe", lambda e, ps=ps, kc=kc, tt=tt: e.matmul(ps[:, :NEXP], lhsT=HF[:, kc, tt * 128:(tt + 1) * 128], rhs=RW[:, kc, :], start=(kc == 0), stop=(kc == NKC - 1)),
                         reads=["rHF", "rRW"], writes=[nm_(ps)], inc=(kc == NKC - 1), acc=(kc > 0))
                P.op("dve", lambda e, ps=ps: e.tensor_tensor(out=LG[:], in0=ps[:, :NEXP], in1=RBB[:], op=ALU.add), reads=[nm_(ps), "rRBB"], writes=["rLG"])
                P.op("dve", lambda e: e.max(out=M8[:], in_=LG[:]), reads=["rLG"], writes=["rM8"])
                P.op("dve", lambda e: e.tensor_scalar(out=NM[:], in0=M8[:, 0:1], scalar1=-1.0, scalar2=None, op0=ALU.mult), reads=["rM8"], writes=["rNM"])
                P.op("dve", lambda e: e.tensor_scalar(out=MK[:], in0=LG[:], scalar1=M8[:, 3:4], scalar2=1e30, op0=ALU.subtract, op1=ALU.mult), reads=["rLG", "rM8"], writes=["rMK"])
                P.op("dve", lambda e: e.tensor_scalar(out=MK[:], in0=MK[:], scalar1=1.0, scalar2=0.0, op0=ALU.add, op1=ALU.max), reads=["rMK"], writes=["rMK"])
                P.op("dve", lambda e: e.tensor_scalar(out=MK[:], in0=MK[:], scalar1=1.0, scalar2=None, op0=ALU.min), reads=["rMK"], writes=["rMK"])
                P.op("act", lambda e: e.activation(out=EE[:], in_=LG[:], func=AF.Exp, bias=NM[:, 0:1], scale=1.0), reads=["rLG", "rNM"], writes=["rEE"])
                P.op("dve", lambda e: e.tensor_tensor(out=EE[:], in0=EE[:], in1=MK[:], op=ALU.mult), reads=["rEE", "rMK"], writes=["rEE"])
                P.op("dve", lambda e: e.tensor_reduce(out=SM[:], in_=EE[:], axis=mybir.AxisListType.X, op=ALU.add), reads=["rEE"], writes=["rSM"])
                P.op("dve", lambda e: e.tensor_scalar(out=SM[:], in0=SM[:], scalar1=1e-30, scalar2=None, op0=ALU.max), reads=["rSM"], writes=["rSM"])
                P.op("dve", lambda e: e.reciprocal(out=SM[:], in_=SM[:]), reads=["rSM"], writes=["rSM"])
                P.op("dve", lambda e: e.tensor_scalar(out=GG[:], in0=EE[:], scalar1=SM[:, 0:1], scalar2=None, op0=ALU.mult), reads=["rEE", "rSM"], writes=["rGG"])
                P.op("pe", lambda e, tt=tt: e.matmul(PS[2][:NEXP, tt * 128:(tt + 1) * 128], lhsT=GG[:], rhs=IDF, start=True, stop=True), reads=["rGG", "CF"], writes=["ps2"])
            P.op("act", lambda e: e.activation(out=GTt[:], in_=PS[2][:NEXP, :], func=AF.Copy), reads=["ps2"], writes=["rGT"])
            P.dma("sp", lambda e, tb=tb: e.dma_start(out=GTs[:, tb * TB:(tb + 1) * TB], in_=GTt[:]), reads=["rGT"], writes=["GT"])
        if getattr(k, "dbg_out", False):
            for nm2, t_, shp in (("d_LG", LG, [128, NEXP]), ("d_M8", M8, [128, 8]), ("d_GG", GG, [128, NEXP]), ("d_RW", RW, [128, NKC * NEXP]), ("d_RBB", RBB, [128, NEXP]), ("d_HF", HF, [128, NKC * TB]), ("d_GTt", GTt, [32, TB]), ("d_XT", XT, [128, NKC * TB]), ("d_rstd", rstd, [128, TB]), ("d_tmp", tmp, [128, TB])):
                dd = nc.dram_tensor(nm2, shp, F32, kind="ExternalOutput").ap()
                src = t_[:] if len(t_.shape) == 2 else t_[:].rearrange("p a b -> p (a b)")
                P.dma("sp", lambda e, dd=dd, src=src: e.dma_start(out=dd[:, :], in_=src), reads=[nm_(t_)], writes=[nm2])
    p4a()
    P.barrier()
    if os.environ.get("SKIP_P4B"):
        return
    def p4b():
      with ExitStack() as st:
        HT = sb(st, "mHT", [128, NKC, TB], BF16)
        YA = sb(st, "mYA", [128, NKC, TB], F32)
        XC = [sb(st, f"mXC{i}", [128, TB], F32) for i in range(2)]
        W1 = [sb(st, f"mW1{i}", [128, NKC, 384], BF16) for i in range(3)]
        W2 = sb(st, "mW2", [128, 6, D], BF16)
        SGL = sb(st, "mSGL", [128, 6, TB], F32)
        AT = sb(st, "mAT", [128, 6, TB], BF16)
        GBC = sb(st, "mGBC", [128, TB], F32)
        G1 = sb(st, "mG1", [128, TB], F32)
        SG = sb(st, "mSG", [128, TB], F32)
        U1 = sb(st, "mU1", [128, TB], F32)
        GTt = sb(st, "mGT", [32, TB], F32)
        SEL = sb(st, "mSEL", [32, 32 * 128], F32)
        B2 = sb(st, "mB2", [32, D], F32)
        B1C = sb(st, "mB1C", [128, 384], F32)
        B1T = sb(st, "mB1T", [128, 128], F32)
        P.dma("sp", lambda e: e.dma_start(out=SEL[:], in_=sel32[:, :]), writes=["mSEL"])
        P.dma("sp", lambda e: e.dma_start(out=B2[:], in_=b2[l]), writes=["mB2"])
        for i in range(3):
            P.dma("sp", lambda e, i=i: e.dma_start(out=B1T[:], in_=b1[l][i * 128:(i + 1) * 128, :]), writes=["mB1T"])
            P.op("pe", lambda e: e.matmul(PS[7][:, :128], lhsT=B1T[:], rhs=IDF, start=True, stop=True), reads=["mB1T", "CF"], writes=["ps7"])
            P.op("dve", lambda e, i=i: e.tensor_copy(out=B1C[:, i * 128:(i + 1) * 128], in_=PS[7][:, :128]), reads=["ps7"], writes=["mB1C"])
        nw = 0
        npz = 0
        for tb in range(NTB):
            load_xT_blk(HT, tb, src=h2T)
            P.dma("sp", lambda e, tb=tb: e.dma_start(out=GTt[:], in_=GTs[:, tb * TB:(tb + 1) * TB]), reads=["GT"], writes=["mGT"])
            for n in range(NKC):
                ps = PS[npz % 4]
                npz += 1
                P.op("pe", lambda e, ps=ps, n=n: e.matmul(ps[:, :], lhsT=B2[:, n * 128:(n + 1) * 128], rhs=GTt[:], start=True, stop=True), reads=["mB2", "mGT"], writes=[nm_(ps)])
                P.op("act", lambda e, ps=ps, n=n: e.activation(out=YA[:, n, :], in_=ps[:, :], func=AF.Copy), reads=[nm_(ps)], writes=["mYA"])
            for ex in range(n_exp):
                P.op("pe", lambda e, ex=ex: e.matmul(PS[5][:, :], lhsT=SEL[:, ex * 128:(ex + 1) * 128], rhs=GTt[:], start=True, stop=True), reads=["mSEL", "mGT"], writes=["ps5"])
                P.op("act", lambda e: e.activation(out=GBC[:], in_=PS[5][:, :], func=AF.Copy), reads=["ps5"], writes=["mGBC"])
                P.dma("pool", lambda e, ex=ex: e.dma_start(out=W2[:], in_=w2[l][ex].rearrange("(fc p) n -> p fc n", p=128)), writes=["mW2"])
                w1l = w1[l][ex].rearrange("(kc p) n -> p kc n", p=128)
                for pc in range(4):
                    wt = W1[nw % 3]
                    nw += 1
                    P.dma("pool", lambda e, wt=wt, pc=pc, w1l=w1l: e.dma_start(out=wt[:], in_=w1l[:, :, pc * 384:(pc + 1) * 384]), writes=[nm_(wt)])
                    for f3 in range(3):
                        f = pc * 3 + f3
                        ps = PS[npz % 4]
                        npz += 1
                        for kc in range(NKC):
                            P.op("pe", lambda e, ps=ps, wt=wt, kc=kc, f3=f3: e.matmul(ps[:, :], lhsT=wt[:, kc, f3 * 128:(f3 + 1) * 128], rhs=HT[:, kc, :], start=(kc == 0), stop=(kc == NKC - 1)),
                                 reads=[nm_(wt), "mHT"], writes=[nm_(ps)], inc=(kc == NKC - 1), acc=(kc > 0))
                        bc = B1C[:, ex * 12 + f:ex * 12 + f + 1]
                        if f < 6:
                            P.op("dve", lambda e, ps=ps, bc=bc: e.tensor_scalar(out=G1[:], in0=ps[:, :], scalar1=bc, scalar2=7.0, op0=ALU.add, op1=ALU.min), reads=[nm_(ps), "mB1C"], writes=["mG1"])
                            P.op("act", lambda e: e.activation(out=SG[:], in_=G1[:], func=AF.Sigmoid, scale=1.702), reads=["mG1"], writes=["mSG"])
                            P.op("dve", lambda e, f=f: e.tensor_tensor(out=SGL[:, f, :], in0=G1[:], in1=SG[:], op=ALU.mult), reads=["mG1", "mSG"], writes=["mSGL"])
                        else:
                            j = f - 6
                            P.op("dve", lambda e, ps=ps, bc=bc: e.tensor_scalar(out=U1[:], in0=ps[:, :], scalar1=bc, scalar2=7.0, op0=ALU.add, op1=ALU.min), reads=[nm_(ps), "mB1C"], writes=["mU1"])
                            P.op("dve", lambda e: e.tensor_scalar(out=U1[:], in0=U1[:], scalar1=-7.0, scalar2=1.0, op0=ALU.max, op1=ALU.add), reads=["mU1"], writes=["mU1"])
                            P.op("dve", lambda e, j=j: e.tensor_tensor(out=U1[:], in0=U1[:], in1=SGL[:, j, :], op=ALU.mult), reads=["mU1", "mSGL"], writes=["mU1"])
                            P.op("dve", lambda e, j=j: e.tensor_tensor(out=AT[:, j, :], in0=U1[:], in1=GBC[:], op=ALU.mult), reads=["mU1", "mGBC"], writes=["mAT"])
                for n in range(NKC):
                    ps = PS[npz % 4]
                    npz += 1
                    for fc in range(6):
                        P.op("pe", lambda e, ps=ps, fc=fc, n=n: e.matmul(ps[:, :], lhsT=W2[:, fc, n * 128:(n + 1) * 128], rhs=AT[:, fc, :], start=(fc == 0), stop=(fc == 5)),
                             reads=["mW2", "mAT"], writes=[nm_(ps)], inc=(fc == 5), acc=(fc > 0))
                    P.op("dve", lambda e, ps=ps, n=n: e.tensor_tensor(out=YA[:, n, :], in0=YA[:, n, :], in1=ps[:, :], op=ALU.add), reads=[nm_(ps), "mYA"], writes=["mYA"])
            for n in range(NKC):
                xc = XC[n % 2]
                P.dma("sp", lambda e, xc=xc, n=n, tb=tb: e.dma_start(out=xc[:], in_=xT[n * 128:(n + 1) * 128, tb * TB:(tb + 1) * TB]), reads=["xT"], writes=[nm_(xc)])
                P.op("dve", lambda e, xc=xc, n=n: e.scalar_tensor_tensor(out=xc[:], in0=YA[:, n, :], scalar=col("mod", 80 + n), in1=xc[:], op0=ALU.mult, op1=ALU.add),
                     reads=["mYA", nm_(xc), "COLS"], writes=[nm_(xc)])
                P.dma("sp", lambda e, xc=xc, n=n, tb=tb: e.dma_start(out=xT[n * 128:(n + 1) * 128, tb * TB:(tb + 1) * TB], in_=xc[:]), reads=[nm_(xc)], writes=["xT"])
    p4b()


def final(P, k, env):
    nc = k.nc
    PS, xT, y_out, col, IDF, ONESB = env["PS"], env["xT"], env["y_out"], env["col"], env["IDF"], env["ONESB"]
    sb, nm_, rstd_from_ps = env["sb"], env["nm_"], env["rstd_from_ps"]
    with ExitStack() as st:
        XT = sb(st, "fXT", [128, NKC, TB], F32)
        SQ = sb(st, "fSQ", [128, NKC, TB], BF16)
        tmp = sb(st, "ftmp", [128, TB], F32)
        rstd = sb(st, "frstd", [128, TB], F32)
        OT = [sb(st, f"fOT{i}", [128, D], F32) for i in range(2)]
        nn_ = [0]
        xo_out = env.get("xo_out")

        def emit_T(dst, dkey, tb):
            for tt in range(4):
                ot = OT[nn_[0] % 2]
                nn_[0] += 1
                for g in range(4):
                    ps = PS[g]
                    for j in range(4):
                        kc = g * 4 + j
                        P.op("pe", lambda e, ps=ps, j=j, kc=kc, tt=tt: e.matmul(ps[:, j * 128:(j + 1) * 128], lhsT=XT[:, kc, tt * 128:(tt + 1) * 128], rhs=IDF, start=True, stop=True),
                             reads=["fXT", "CF"], writes=[nm_(ps)], inc=(j == 3))
                    P.op("act", lambda e, ps=ps, ot=ot, g=g: e.activation(out=ot[:, g * 512:(g + 1) * 512], in_=ps[:, :], func=AF.Copy), reads=[nm_(ps)], writes=[nm_(ot)])
                r0 = tb * TB + tt * 128
                P.dma("sp", lambda e, ot=ot, r0=r0, dst=dst: e.dma_start(out=dst[r0:r0 + 128, :], in_=ot[:]), reads=[nm_(ot)], writes=[dkey])

        for tb in range(NTB):
            P.dma("sp", lambda e, tb=tb: e.dma_start(out=XT[:], in_=xT[:, tb * TB:(tb + 1) * TB].rearrange("(kc p) t -> p kc t", p=128)),
                  reads=["xT"], writes=["fXT"])
            if xo_out is not None:
                emit_T(xo_out, "xo", tb)
            P.op("act", lambda e: e.activation(out=SQ[:].rearrange("p a b -> p (a b)"), in_=XT[:].rearrange("p a b -> p (a b)"), func=AF.Square),
                 reads=["fXT"], writes=["fSQ"])
            for kc in range(NKC):
                P.op("pe", lambda e, kc=kc: e.matmul(PS[6][:, :], lhsT=ONESB, rhs=SQ[:, kc, :], start=(kc == 0), stop=(kc == NKC - 1)),
                     reads=["fSQ", "CB"], writes=["ps6"], inc=(kc == NKC - 1), acc=(kc > 0))
            rstd_from_ps(PS[6], rstd, D, tmp)
            for kc in range(NKC):
                P.op("dve", lambda e, kc=kc: e.scalar_tensor_tensor(out=XT[:, kc, :], in0=XT[:, kc, :], scalar=col("fin", kc), in1=rstd[:], op0=ALU.mult, op1=ALU.mult),
                     reads=["fXT", "frstd", "COLS"], writes=["fXT"])
            emit_T(y_out, "y", tb)
    P.barrier()


def _consts():
    p = np.arange(128)[:, None]
    f = np.arange(128)[None, :]
    c = np.zeros((128, 8, 128), np.float32)
    c[:, 0] = (p == f)
    c[:, 1] = 1.0
    c[:, 2] = (p < f)
    c[:, 3] = (p <= f)
    c[:, 4] = ((p // 64) <= (f // 64))
    c[:, 5] = (p > f)
    sel = np.zeros((8, 8 * 128), np.float32)
    for h in range(8):
        sel[h, h * 128:(h + 1) * 128] = 1.0
    sel32 = np.zeros((32, 32 * 128), np.float32)
    for h in range(32):
        sel32[h, h * 128:(h + 1) * 128] = 1.0
    inv = 1.0 / (10000.0 ** (np.arange(0, 64, 2, dtype=np.float32) / 64))
    ang = np.arange(S, dtype=np.float32)[:, None] * inv[None, :]
    cosT = np.cos(ang).T.astype(np.float32)
    sinT = np.sin(ang).T.astype(np.float32)
    rc = np.concatenate([cosT, cosT, cosT, cosT], 0)
    rs = np.concatenate([-sinT, sinT, -sinT, sinT], 0)
    return c, sel, np.ascontiguousarray(rc), np.ascontiguousarray(rs), sel32


def _ca_bias_tables(ca_rel_bias):
    L = ca_rel_bias.shape[0]
    kk = np.arange(640)[:, None]
    qq = np.arange(128)[None, :]
    dist = (512 + qq) - kk
    idx = np.clip(dist, -63, 256) + 63
    qch = (512 + qq) // 64
    kch = kk // 64
    valid = (kch <= qch) & (kch >= qch - 8)
    out = np.empty((L, 4, 640, 128), np.float32)
    for l in range(L):
        for h in range(4):
            t = ca_rel_bias[l, h][idx]
            out[l, h] = np.where(valid, t, np.float32(-30000.0))
    return np.ascontiguousarray(out.reshape(L, 4, 5, 128, 128))


_CACHE = {}


def prep_inputs(inputs, depth, l0=0):
    L = depth
    cst, sel, rc, rs, sel32 = _consts()
    g = lambda n: np.ascontiguousarray(np.asarray(inputs[n], np.float32)[l0:l0 + L])
    shared = {
        "attn_norm": g("attn_norm").reshape(L, 16, 128), "ffn_norm": g("ffn_norm").reshape(L, 16, 128),
        "mod_w": g("mod_w"), "mod_b": g("mod_b").reshape(L, 96, 128), "w_in": g("w_in"),
        "mla_q_norm": g("mla_q_norm").reshape(L, 4, 128), "mla_w_q_up": g("mla_w_q_up"),
        "mla_kv_norm": g("mla_kv_norm").reshape(L, 2, 128), "mla_w_kv_up": g("mla_w_kv_up"),
        "ssm_conv_w": g("ssm_conv_w").reshape(L, 32, 128), "ssm_conv_b": g("ssm_conv_b").reshape(L, 8, 128),
        "ssm_dt_bias": g("ssm_dt_bias").reshape(L, 8, 1), "ssm_a_log": g("ssm_a_log").reshape(L, 8, 1),
        "ssm_d": g("ssm_d"), "ssm_norm": g("ssm_norm").reshape(L, 4, 128), "ssm_norm8": g("ssm_norm").reshape(L, 8, 64),
        "ca_biasT": _ca_bias_tables(g("ca_rel_bias")), "mix_out_norm": g("mix_out_norm").reshape(L, 12, 128),
        "w_out": g("w_out"), "router_w": g("router_w"), "router_b": g("router_b").reshape(L, 1, NEXP),
        "moe_w1": g("moe_w1"), "moe_b1": g("moe_b1").reshape(L, NEXP * 12, 128), "moe_w2": g("moe_w2"), "moe_b2": g("moe_b2"),
        "final_norm": np.asarray(inputs["final_norm"], np.float32).reshape(16, 128),
        "cst": cst, "sel8": sel, "sel32": sel32, "rope_cos": rc, "rope_sin": rs,
    }
    return shared


def run(inputs, depth=4, ncores=NCORES, n_exp=NEXP, dbg_cat=None, dbg_out=False):
    nc, k = build(depth, n_exp, dbg_cat is not None, dbg_out)
    shared = prep_inputs(inputs, depth)
    x = np.asarray(inputs["x"], np.float32)
    c = np.asarray(inputs["c"], np.float32)
    in_maps = []
    for i in range(ncores):
        b = i % 4
        m = dict(shared)
        m["x"] = np.ascontiguousarray(x[b])
        m["c"] = np.ascontiguousarray(c[b].reshape(16, 128))
        if dbg_cat is not None:
            m["catT_in"] = dbg_cat
        in_maps.append(m)
    res = run_bass_kernel_spmd(nc, in_maps, core_ids=list(range(ncores)))
    if dbg_out:
        return [r for r in res.results]
    return [r["y"] for r in res.results]


LAYERS_PER_LAUNCH = 1


def kernel(**inputs):
    depth = 4
    lpl = LAYERS_PER_LAUNCH
    nl = depth // lpl
    nc, k = build(lpl, NEXP, chain=(nl > 1))
    x = np.asarray(inputs["x"], np.float32)
    c = np.asarray(inputs["c"], np.float32)
    xs = [np.ascontiguousarray(x[b]) for b in range(4)]
    outs = None
    for i in range(nl):
        shared = prep_inputs(inputs, lpl, l0=i * lpl)
        in_maps = []
        for b in range(NCORES):
            m = dict(shared)
            m["x"] = xs[b % 4]
            m["c"] = np.ascontiguousarray(c[b % 4].reshape(16, 128))
            in_maps.append(m)
        res = run_bass_kernel_spmd(nc, in_maps, core_ids=list(range(NCORES)))
        outs = res.results
        if nl > 1:
            xs = [np.ascontiguousarray(np.asarray(outs[b]["xo"], np.float32)) for b in range(4)]
    return np.stack([np.asarray(outs[b]["y"], np.float32) for b in range(4)], 0)
```

```python
from contextlib import ExitStack
import os
import numpy as np
import concourse.bass as bass
import concourse.mybir as mybir
from concourse.bass_utils import run_bass_kernel_spmd

F32 = mybir.dt.float32
BF16 = mybir.dt.bfloat16
AF = mybir.ActivationFunctionType
ALU = mybir.AluOpType

SEM_LIMIT = 30000
NOARENA = not bool(os.environ.get("USE_ARENA"))
NDMA_SLOTS = 6

D = 2048
S = 4096
NKC = 16
TB = 512
NTB = S // TB
EPS = 1e-6
NEXP = 32
DEXP = 768
NCORES = 4


class _Res:
    __slots__ = ("lw", "rd")

    def __init__(self):
        self.lw = None
        self.rd = {}


class Prog:
    ENGS = ("pe", "act", "dve", "pool", "sp")

    def __init__(self, nc, stack):
        self.nc = nc
        self.stack = stack
        self.streams = {e: [] for e in self.ENGS}
        self.seen = {e: {} for e in self.ENGS}
        self.clk = {}
        self.res = {}
        self.nsem = 0
        self.ninstr = 0
        self.pending = {}
        self.allclk = []
        for e in ("pe", "act", "dve", "pool"):
            self.clk[e] = self._newclk(e)
        self.dslots = {q: [self._newclk(f"d{q}{i}") for i in range(NDMA_SLOTS)] for q in ("sp", "pool", "act")}
        self.dnext = {q: 0 for q in ("sp", "pool", "act")}

    def _newclk(self, name):
        self.nsem += 1
        c = [self.stack.enter_context(self.nc.semaphore(f"{name}_{self.nsem}")), 0]
        self.allclk.append(c)
        return c

    def _r(self, key):
        r = self.res.get(key)
        if r is None:
            r = self.res[key] = _Res()
        return r

    def _deps(self, eng, reads, writes, acc, is_dma):
        need = {}

        def add(d, kind):
            if d is None:
                return
            clk, tick, deng = d
            if (not is_dma) and deng == eng and kind != "raw":
                return
            if self.seen[eng].get(clk, 0) >= tick:
                return
            if need.get(clk, 0) < tick:
                need[clk] = tick

        for k in reads:
            add(self._r(k).lw, "raw")
        for k in writes:
            r = self._r(k)
            if not (acc and r.lw is not None and r.lw[2] == eng):
                add(r.lw, "waw")
            for d in r.rd.values():
                add(d, "war")
        return need

    def _commit(self, reads, writes, done):
        for k in reads:
            self._r(k).rd[done[0]] = done
        for k in writes:
            r = self._r(k)
            r.lw = done
            r.rd = {}

    def _emit_waits(self, eng, need):
        for clk, tick in need.items():
            self.seen[eng][clk] = tick
            self.streams[eng].append(("w", clk, tick))

    def op(self, eng, fn, reads=(), writes=(), inc=True, acc=False):
        need = self._deps(eng, reads, writes, acc, False)
        self._emit_waits(eng, need)
        c = self.clk[eng]
        if c[1] >= SEM_LIMIT and not self.pending.get(eng, False):
            c = self.clk[eng] = self._newclk(eng)
        self.pending[eng] = not inc
        if inc:
            c[1] += 1
            tick = c[1]
        else:
            tick = c[1] + 1
        done = (c[0], tick, eng)
        self.streams[eng].append(("i", fn, c[0] if inc else None))
        self._commit(reads, writes, done)
        self.ninstr += 1
        return done

    def dma(self, q, fn, reads=(), writes=()):
        need = self._deps(q, reads, writes, False, True)
        i = self.dnext[q]
        self.dnext[q] = (i + 1) % NDMA_SLOTS
        slot = self.dslots[q][i]
        if slot[1] >= SEM_LIMIT:
            slot = self.dslots[q][i] = self._newclk(f"d{q}{i}")
        if slot[1] > 0 and self.seen[q].get(slot[0], 0) < slot[1]:
            if need.get(slot[0], 0) < slot[1]:
                need[slot[0]] = slot[1]
        self._emit_waits(q, need)
        slot[1] += 16
        done = (slot[0], slot[1], "dma")
        self.streams[q].append(("d", fn, slot[0]))
        self._commit(reads, writes, done)
        self.ninstr += 1
        return done

    def barrier(self):
        for e in self.ENGS:
            need = {}
            for c in self.allclk:
                if c[1] > 0 and self.seen[e].get(c[0], 0) < c[1]:
                    need[c[0]] = c[1]
            self._emit_waits(e, need)
        self.res = {}

    def finish(self):
        nc = self.nc
        engmap = {"pe": "tensor", "act": "scalar", "dve": "vector", "pool": "gpsimd", "sp": "sync"}
        with nc.Block() as block:
            for e in self.ENGS:
                items = self.streams[e]

                def body(engobj, items=items):
                    for it in items:
                        if it[0] == "w":
                            engobj.wait_ge(it[1], it[2])
                        elif it[0] == "i":
                            ins = it[1](engobj)
                            if it[2] is not None:
                                ins.then_inc(it[2], 1)
                        else:
                            it[1](engobj).then_inc(it[2], 16)

                getattr(block, engmap[e])(body)


IN_A, IN_B, IN_C, IN_D = 0, 832, 2376, 3912
FT_CH = {}
_c = 0
for _nm, _n in (("qlat", 4), ("kvlat", 2), ("kr", 1), ("krs", 1), ("z", 4), ("xs", 4), ("bm", 2), ("cm", 2),
                ("qc", 4), ("kc", 4), ("qd", 4), ("kd", 4)):
    FT_CH[_nm] = _c
    _c += _n
NFT = _c


class K:
    pass


def build(depth, n_exp=NEXP, dbg_cat=False, dbg_out=False, chain=False):
    nc = bass.Bass("TRN2", target_bir_lowering=False)
    k = K()
    k.nc = nc

    def din(name, shape, dt=F32):
        return nc.dram_tensor(name, list(shape), dt, kind="ExternalInput").ap()

    def dscr(name, shape, dt):
        return nc.dram_tensor(name, list(shape), dt, kind="Internal").ap()

    L = depth
    x_in = din("x", [S, D])
    c_in = din("c", [16, 128])
    attn_norm = din("attn_norm", [L, 16, 128])
    ffn_norm = din("ffn_norm", [L, 16, 128])
    mod_w = din("mod_w", [L, D, 6 * D])
    mod_b = din("mod_b", [L, 96, 128])
    w_in = din("w_in", [L, D, 5448])
    mla_q_norm = din("mla_q_norm", [L, 4, 128])
    wq = din("mla_w_q_up", [L, 512, 768])
    mla_kv_norm = din("mla_kv_norm", [L, 2, 128])
    wkv = din("mla_w_kv_up", [L, 256, 1024])
    conv_w = din("ssm_conv_w", [L, 4 * 8, 128])
    conv_b = din("ssm_conv_b", [L, 8, 128])
    dt_bias = din("ssm_dt_bias", [L, 8, 1])
    a_log = din("ssm_a_log", [L, 8, 1])
    ssm_d = din("ssm_d", [L, 8])
    ssm_norm = din("ssm_norm", [L, 4, 128])
    ssm_norm8 = din("ssm_norm8", [L, 8, 64])
    ca_bias = din("ca_biasT", [L, 4, 5, 128, 128])
    mix_norm = din("mix_out_norm", [L, 12, 128])
    w_out = din("w_out", [L, D, D])
    router_w = din("router_w", [L, D, NEXP])
    router_b = din("router_b", [L, 1, NEXP])
    w1 = din("moe_w1", [L, NEXP, D, 2 * DEXP])
    b1 = din("moe_b1", [L, NEXP * 12, 128])
    w2 = din("moe_w2", [L, NEXP, DEXP, D])
    b2 = din("moe_b2", [L, NEXP, D])
    final_norm = din("final_norm", [16, 128])
    cst = din("cst", [128, 8, 128])
    sel8 = din("sel8", [8, 8 * 128])
    sel32 = din("sel32", [32, 32 * 128])
    rope_cos = din("rope_cos", [128, S])
    rope_sin = din("rope_sin", [128, S])
    y_out = nc.dram_tensor("y", [S, D], F32, kind="ExternalOutput").ap()
    xo_out = nc.dram_tensor("xo", [S, D], F32, kind="ExternalOutput").ap() if chain else None

    xT = dscr("xT", [D, S], F32)
    FT = dscr("FT", [NFT * 128, S], BF16)
    DTR = dscr("DTR", [8, S], F32)
    VCs = dscr("VC", [S, 512], BF16)
    VDs = dscr("VD", [S, 512], BF16)
    catT = din("catT_in", [D, S], BF16) if dbg_cat else dscr("catT", [D, S], BF16)
    k.dbg_cat = dbg_cat
    k.dbg_out = dbg_out
    cat_dump = nc.dram_tensor("cat_dump", [D, S], BF16, kind="ExternalOutput").ap() if (dbg_out and not dbg_cat) else None
    h2T = dscr("h2T", [D, S], BF16)
    GTs = dscr("GT", [NEXP, S], F32)
    FT2 = dscr("FT2", [11 * 128, S], BF16)
    VMs = dscr("VM", [S, 512], BF16)
    XSs = dscr("XSs", [S, 512], BF16)
    CSs = dscr("CSs", [8, S], F32)
    CSCs = dscr("CSCs", [128, 256], F32)
    DTCs = dscr("DTCs", [128, 256], F32)

    with ExitStack() as gst:
        P = Prog(nc, gst)

        ARENA_BYTES = 196608
        ARENA = None if NOARENA else gst.enter_context(nc.sbuf_tensor("ARENA", [128, ARENA_BYTES // 2], BF16))
        aoff = [0]

        class Tile:
            def __init__(self, ap, name, shape):
                self.ap, self.name, self.shape = ap, name, tuple(shape)

            def __getitem__(self, key):
                return self.ap[key]

        sbcnt = [0]

        def sb(st, name, shape, dt):
            if NOARENA:
                sbcnt[0] += 1
                return Tile(st.enter_context(nc.sbuf_tensor(f"{name}__u{sbcnt[0]}", list(shape), dt)), name, shape)
            esz = 4 if dt == F32 else 2
            nel = 1
            for d_ in shape[1:]:
                nel *= d_
            nbytes = (nel * esz + 63) // 64 * 64
            off = aoff[0]
            assert off + nbytes <= ARENA_BYTES, (name, off, nbytes)
            aoff[0] = off + nbytes
            if st is not gst:
                st.callback(lambda off=off: aoff.__setitem__(0, off))
            v = ARENA[0:shape[0], off // 2:off // 2 + nel * esz // 2]
            if dt == F32:
                v = v.bitcast(F32)
            if len(shape) == 3:
                v = v.rearrange("p (a b) -> p a b", a=shape[1])
            return Tile(v, name, shape)

        PS = [gst.enter_context(nc.psum_tensor(f"ps{i}", [128, 512], F32)) for i in range(8)]
        CF = sb(gst, "CF", [128, 8, 128], F32)
        CB = sb(gst, "CB", [128, 8, 128], BF16)
        SEL = sb(gst, "SEL", [8, 8 * 128], F32)
        P.dma("sp", lambda e: e.dma_start(out=CF[:], in_=cst[:, :, :]), writes=["CF"])
        P.dma("sp", lambda e: e.dma_start(out=SEL[:], in_=sel8[:, :]), writes=["SEL"])
        P.op("dve", lambda e: e.tensor_copy(out=CB[:], in_=CF[:]), reads=["CF"], writes=["CB"])
        IDF, ONESF = CF[:, 0, :], CF[:, 1, :]
        IDB, ONESB, TRIB, ZB = CB[:, 0, :], CB[:, 1, :], CB[:, 5, :], CB[:, 6, :]
        M_LT, M_LE, M_MLA = CF[:, 2, :], CF[:, 3, :], CF[:, 4, :]
        COLS = sb(gst, "COLS", [128, 400], F32)
        colmap = {}
        _o = 0
        for nm, n in (("an", 16), ("fn", 16), ("modb", 96), ("mod", 96), ("A1", 16), ("A2", 16), ("qn", 4), ("kvn", 2),
                      ("cw", 32), ("cb", 8), ("sn", 4), ("mn", 12), ("cond", 16), ("fin", 16), ("b1", 12)):
            colmap[nm] = (_o, n)
            _o += n
        assert _o <= 400

        def col(nm, j=0, n=1):
            o = colmap[nm][0] + j
            return COLS[:, o:o + n]

        cnt = [0]

        def uid(s):
            cnt[0] += 1
            return f"{s}{cnt[0]}"

        def colize(st, src_ap, n, nm, j0=0):
            t = sb(st, uid("cz"), [128, 128], F32)
            key = uid("czk")
            P.dma("sp", lambda e: e.dma_start(out=t[:n, :], in_=src_ap), writes=[key])
            P.op("pe", lambda e: e.matmul(PS[7][:, :n], lhsT=t[:n, :], rhs=IDF[:n, :n], start=True, stop=True),
                 reads=[key, "CF"], writes=["ps7"])
            P.op("dve", lambda e: e.tensor_copy(out=col(nm, j0, n), in_=PS[7][:, :n]), reads=["ps7"], writes=["COLS"])

        def rstd_from_ps(ps_t, out_t, dim, tmp_t, p=128):
            P.op("dve", lambda e: e.tensor_scalar(out=tmp_t[:p, :], in0=ps_t[:p, :], scalar1=1.0 / dim, scalar2=EPS, op0=ALU.mult, op1=ALU.add),
                 reads=[nm_(ps_t)], writes=[nm_(tmp_t)])
            P.op("act", lambda e: e.activation(out=tmp_t[:p, :], in_=tmp_t[:p, :], func=AF.Sqrt), reads=[nm_(tmp_t)], writes=[nm_(tmp_t)])
            P.op("dve", lambda e: e.reciprocal(out=out_t[:p, :], in_=tmp_t[:p, :]), reads=[nm_(tmp_t)], writes=[nm_(out_t)])

        def nm_(t):
            if isinstance(t, str):
                return t
            if isinstance(t, Tile):
                return t.name
            n_ = t.tensor.name if hasattr(t, "tensor") else t.name
            assert n_ != "ARENA", "arena AP passed to nm_"
            return n_

        with ExitStack() as st:
            XI = [sb(st, f"XI{i}", [128, D], F32) for i in range(2)]
            XO = [sb(st, f"XO{i}", [128, 4, 128], F32) for i in range(2)]
            n = 0
            for tt in range(S // 128):
                xi = XI[tt % 2]
                P.dma("sp", lambda e, xi=xi, tt=tt: e.dma_start(out=xi[:], in_=x_in[tt * 128:(tt + 1) * 128, :]), writes=[nm_(xi)])
                for g in range(4):
                    ps = PS[n % 4]
                    xo = XO[n % 2]
                    for j in range(4):
                        kc = g * 4 + j
                        P.op("pe", lambda e, ps=ps, xi=xi, j=j, kc=kc: e.matmul(ps[:, j * 128:(j + 1) * 128], lhsT=xi[:, kc * 128:(kc + 1) * 128], rhs=IDF, start=True, stop=True),
                             reads=[nm_(xi), "CF"], writes=[nm_(ps)], inc=(j == 3))
                    P.op("act", lambda e, ps=ps, xo=xo: e.activation(out=xo[:].rearrange("p a b -> p (a b)"), in_=ps[:, :], func=AF.Copy), reads=[nm_(ps)], writes=[nm_(xo)])
                    P.dma("sp", lambda e, xo=xo, g=g, tt=tt: e.dma_start(
                        out=xT[g * 512:(g + 1) * 512, tt * 128:(tt + 1) * 128].rearrange("(a p) t -> p a t", p=128), in_=xo[:]),
                        reads=[nm_(xo)], writes=["xT"])
                    n += 1
            colize(st, c_in[:, :], 16, "cond")
            P.op("act", lambda e: e.activation(out=col("cond", 0, 16), in_=col("cond", 0, 16), func=AF.Silu), reads=["COLS"], writes=["COLS"])
            colize(st, final_norm[:, :], 16, "fin")
        P.barrier()

        def load_xT_blk(XT, tb, src=xT, q="sp"):
            P.dma(q, lambda e: e.dma_start(out=XT[:], in_=src[:, tb * TB:(tb + 1) * TB].rearrange("(kc p) t -> p kc t", p=128)),
                  reads=[nm_(src)], writes=[nm_(XT)])

        def norm_mod(st_tiles, XT, HT, Acol, Scol, tb, HF=None):
            SQ, tmp, rstd, t2 = st_tiles
            P.op("act", lambda e: e.activation(out=SQ[:].rearrange("p a b -> p (a b)"), in_=XT[:].rearrange("p a b -> p (a b)"), func=AF.Square),
                 reads=[nm_(XT)], writes=[nm_(SQ)])
            for kc in range(NKC):
                P.op("pe", lambda e, kc=kc: e.matmul(PS[6][:, :], lhsT=ONESB, rhs=SQ[:, kc, :], start=(kc == 0), stop=(kc == NKC - 1)),
                     reads=[nm_(SQ), "CB"], writes=["ps6"], inc=(kc == NKC - 1), acc=(kc > 0))
            rstd_from_ps(PS[6], rstd, D, tmp)
            for kc in range(NKC):
                P.op("dve", lambda e, kc=kc: e.scalar_tensor_tensor(out=t2[:], in0=XT[:, kc, :], scalar=Acol(kc), in1=rstd[:], op0=ALU.mult, op1=ALU.mult),
                     reads=[nm_(XT), nm_(rstd), "COLS"], writes=[nm_(t2)])
                if HF is None:
                    P.op("dve", lambda e, kc=kc: e.tensor_scalar(out=HT[:, kc, :], in0=t2[:], scalar1=Scol(kc), scalar2=None, op0=ALU.add),
                         reads=[nm_(t2), "COLS"], writes=[nm_(HT)])
                else:
                    P.op("dve", lambda e, kc=kc: e.tensor_scalar(out=HF[:, kc, :], in0=t2[:], scalar1=Scol(kc), scalar2=None, op0=ALU.add),
                         reads=[nm_(t2), "COLS"], writes=[nm_(HF)])
                    P.op("act", lambda e, kc=kc: e.activation(out=HT[:, kc, :], in_=HF[:, kc, :], func=AF.Copy), reads=[nm_(HF)], writes=[nm_(HT)])

        def group_norm_store(OA, nch, gcolf, dst_rows, tb, SQs, tmp, rstd, stg, dim):
            P.op("act", lambda e: e.activation(out=SQs[:, :nch, :].rearrange("p a b -> p (a b)"), in_=OA[:, :nch, :].rearrange("p a b -> p (a b)"), func=AF.Square),
                 reads=[nm_(OA)], writes=[nm_(SQs)])
            for c in range(nch):
                P.op("pe", lambda e, c=c: e.matmul(PS[6][:, :], lhsT=ONESB, rhs=SQs[:, c, :], start=(c == 0), stop=(c == nch - 1)),
                     reads=[nm_(SQs), "CB"], writes=["ps6"], inc=(c == nch - 1), acc=(c > 0))
            rstd_from_ps(PS[6], rstd, dim, tmp)
            for c in range(nch):
                P.op("dve", lambda e, c=c: e.scalar_tensor_tensor(out=stg[:, c, :], in0=OA[:, c, :], scalar=gcolf(c), in1=rstd[:], op0=ALU.mult, op1=ALU.mult),
                     reads=[nm_(OA), nm_(rstd), "COLS"], writes=[nm_(stg)])
            P.dma("sp", lambda e: e.dma_start(out=catT[dst_rows:dst_rows + nch * 128, tb * TB:(tb + 1) * TB].rearrange("(a p) t -> p a t", p=128), in_=stg[:, :nch, :]),
                  reads=[nm_(stg)], writes=["catT"])

        benv = dict(locals())

        def do_layer(l):
            with ExitStack() as st:
                colize(st, attn_norm[l], 16, "an")
                colize(st, ffn_norm[l], 16, "fn")
                colize(st, mod_b[l], 96, "modb")
                colize(st, mla_q_norm[l], 4, "qn")
                colize(st, mla_kv_norm[l], 2, "kvn")
                colize(st, conv_w[l], 32, "cw")
                colize(st, conv_b[l], 8, "cb")
                colize(st, ssm_norm[l], 4, "sn")
                colize(st, mix_norm[l], 12, "mn")
                MW = [sb(st, f"MW{i}", [128, NKC, 512], F32) for i in range(2)]
                for nb in range(24):
                    mw = MW[nb % 2]
                    P.dma("sp" if nb % 2 == 0 else "act", lambda e, mw=mw, nb=nb: e.dma_start(out=mw[:], in_=mod_w[l][:, nb * 512:(nb + 1) * 512].rearrange("(kc p) n -> p kc n", p=128)),
                          writes=[nm_(mw)])
                    for j in range(4):
                        for kc in range(NKC):
                            P.op("pe", lambda e, mw=mw, j=j, kc=kc: e.matmul(PS[7][:, j:j + 1], lhsT=mw[:, kc, j * 128:(j + 1) * 128], rhs=col("cond", kc, 1), start=(kc == 0), stop=(kc == NKC - 1)),
                                 reads=[nm_(mw), "COLS"], writes=["ps7"], inc=(kc == NKC - 1), acc=(kc > 0))
                    P.op("dve", lambda e, nb=nb: e.tensor_tensor(out=col("mod", nb * 4, 4), in0=PS[7][:, 0:4], in1=col("modb", nb * 4, 4), op=ALU.add),
                         reads=["ps7", "COLS"], writes=["COLS"])
                P.op("dve", lambda e: e.scalar_tensor_tensor(out=col("A1", 0, 16), in0=col("mod", 16, 16), scalar=1.0, in1=col("an", 0, 16), op0=ALU.add, op1=ALU.mult),
                     reads=["COLS"], writes=["COLS"])
                P.op("dve", lambda e: e.scalar_tensor_tensor(out=col("A2", 0, 16), in0=col("mod", 64, 16), scalar=1.0, in1=col("fn", 0, 16), op0=ALU.add, op1=ALU.mult),
                     reads=["COLS"], writes=["COLS"])
            P.barrier()

            with ExitStack() as st:
                XT = sb(st, "XT", [128, NKC, TB], F32)
                HT = sb(st, "HT", [128, NKC, TB], BF16)
                SQ = sb(st, "SQ", [128, NKC, TB], BF16)
                tmp = sb(st, "tmp", [128, TB], F32)
                rstd = sb(st, "rstd", [128, TB], F32)
                t2 = sb(st, "t2", [128, TB], F32)
                WB = [sb(st, f"WB{i}", [128, NKC, 512], BF16) for i in range(2)]
                STG = [sb(st, f"STG{i}", [128, TB], BF16) for i in range(3)]
                STF = sb(st, "STF", [128, TB], F32)
                blocks = []
                blocks.append(([(IN_A, 512)], [("ft", FT_CH["qlat"] + i) for i in range(4)]))
                blocks.append(([(IN_A + 512, 256), (IN_A + 768, 64), (IN_A + 768, 64), (IN_A + 800, 32), (IN_A + 768, 32), (IN_A + 800, 32), (IN_A + 768, 32)],
                               [("ft", FT_CH["kvlat"]), ("ft", FT_CH["kvlat"] + 1), ("ft", FT_CH["kr"]), None]))
                blocks[-1] = (blocks[-1][0], [("ft", FT_CH["kvlat"]), ("ft", FT_CH["kvlat"] + 1), ("ft", FT_CH["kr"]), ("ft64", FT_CH["krs"])])
                blocks.append(([(IN_B, 512)], [("ft", FT_CH["z"] + i) for i in range(4)]))
                blocks.append(([(IN_B + 512, 512)], [("ft", FT_CH["xs"] + i) for i in range(4)]))
                blocks.append(([(IN_B + 1024, 512)], [("ft", FT_CH["bm"]), ("ft", FT_CH["bm"] + 1), ("ft", FT_CH["cm"]), ("ft", FT_CH["cm"] + 1)]))
                blocks.append(([(IN_B + 1536, 8)], [("dtr", 0)]))
                blocks.append(([(IN_C, 512)], [("ft", FT_CH["qc"] + i) for i in range(4)]))
                blocks.append(([(IN_C + 512, 512)], [("ft", FT_CH["kc"] + i) for i in range(4)]))
                blocks.append(([(IN_D, 512)], [("ft", FT_CH["qd"] + i) for i in range(4)]))
                blocks.append(([(IN_D + 512, 512)], [("ft", FT_CH["kd"] + i) for i in range(4)]))
                blocks.append(([(IN_C + 1024, 512)], [("v", VCs)]))
                blocks.append(([(IN_D + 1024, 512)], [("v", VDs)]))
                wl = w_in[l].rearrange("(kc p) n -> p kc n", p=128)
                nw = 0
                ne = 0
                for tb in range(NTB):
                    load_xT_blk(XT, tb)
                    norm_mod((SQ, tmp, rstd, t2), XT, HT, lambda kc: col("A1", kc), lambda kc: col("mod", kc), tb)
                    for segs, dests in blocks:
                        wb = WB[nw % 2]
                        nw += 1
                        o = 0
                        for (c0, n_) in segs:
                            P.dma("pool", lambda e, wb=wb, o=o, c0=c0, n_=n_: e.dma_start(out=wb[:, :, o:o + n_], in_=wl[:, :, c0:c0 + n_]), writes=[nm_(wb)])
                            o += n_
                        if dests[0][0] == "v":
                            for tt in range(4):
                                ps = PS[ne % 4]
                                stg = STG[ne % 3]
                                ne += 1
                                for kc in range(NKC):
                                    P.op("pe", lambda e, ps=ps, wb=wb, kc=kc, tt=tt: e.matmul(ps[:, :], lhsT=HT[:, kc, tt * 128:(tt + 1) * 128], rhs=wb[:, kc, :], start=(kc == 0), stop=(kc == NKC - 1)),
                                         reads=[nm_(wb), "HT"], writes=[nm_(ps)], inc=(kc == NKC - 1), acc=(kc > 0))
                                P.op("act", lambda e, ps=ps, stg=stg: e.activation(out=stg[:], in_=ps[:, :], func=AF.Copy), reads=[nm_(ps)], writes=[nm_(stg)])
                                dstv = dests[0][1]
                                r0 = tb * TB + tt * 128
                                P.dma("sp", lambda e, stg=stg, dstv=dstv, r0=r0: e.dma_start(out=dstv[r0:r0 + 128, :], in_=stg[:]), reads=[nm_(stg)], writes=[nm_(dstv)])
                            continue
                        for j, dst in enumerate(dests):
                            m = 128
                            if dst[0] == "dtr":
                                m = 8
                            ps = PS[ne % 4]
                            stg = STG[ne % 3]
                            ne += 1
                            for kc in range(NKC):
                                P.op("pe", lambda e, ps=ps, wb=wb, kc=kc, j=j, m=m: e.matmul(ps[:m, :], lhsT=wb[:, kc, j * 128:j * 128 + m], rhs=HT[:, kc, :], start=(kc == 0), stop=(kc == NKC - 1)),
                                     reads=[nm_(wb), "HT"], writes=[nm_(ps)], inc=(kc == NKC - 1), acc=(kc > 0))
                            if dst[0] == "dtr":
                                P.op("act", lambda e, ps=ps: e.activation(out=STF[:8, :], in_=ps[:8, :], func=AF.Copy), reads=[nm_(ps)], writes=["STF"])
                                P.dma("sp", lambda e, tb=tb: e.dma_start(out=DTR[:, tb * TB:(tb + 1) * TB], in_=STF[:8, :]), reads=["STF"], writes=["DTR"])
                            else:
                                P.op("act", lambda e, ps=ps, stg=stg: e.activation(out=stg[:], in_=ps[:, :], func=AF.Copy), reads=[nm_(ps)], writes=[nm_(stg)])
                                ch = dst[1]
                                if dst[0] == "ft64":
                                    pass
                                P.dma("sp", lambda e, stg=stg, ch=ch, tb=tb: e.dma_start(out=FT[ch * 128:(ch + 1) * 128, tb * TB:(tb + 1) * TB], in_=stg[:]), reads=[nm_(stg)], writes=["FT"])
            P.barrier()
            k.l = l
            if not dbg_cat:
                mixers(P, k, {**benv, **locals()})
            P.barrier()
            if cat_dump is not None and l == 0:
                P.dma("sp", lambda e: e.dma_start(out=cat_dump[:, :], in_=catT[:, :]), reads=["catT"], writes=["cat_dump"])
                P.barrier()
            tail(P, k, {**benv, **locals()}, n_exp)
            P.barrier()


        for l_ in range(L):
            do_layer(l_)

        if dbg_out:
            for nm2, src, shp, dt_ in (("d_h2T", h2T, [D, S], BF16), ("d_GT", GTs, [NEXP, S], F32), ("d_xT", xT, [D, S], F32)):
                dd = nc.dram_tensor(nm2, shp, dt_, kind="ExternalOutput").ap()
                P.dma("sp", lambda e, dd=dd, src=src: e.dma_start(out=dd[:, :], in_=src[:, :]), reads=[], writes=[nm2])
            P.barrier()
        final(P, k, locals())
        P.finish()
    k.P = P
    return nc, k


def mixers(P, k, env):
    nc = k.nc
    l = k.l
    PS, FT, col, IDF, ONESB, ONESF = env["PS"], env["FT"], env["col"], env["IDF"], env["ONESB"], env["ONESF"]
    IDB, TRIB, ZB, M_LT, M_LE, M_MLA = env["IDB"], env["TRIB"], env["ZB"], env["M_LT"], env["M_LE"], env["M_MLA"]
    sb, nm_, rstd_from_ps, group_norm_store = env["sb"], env["nm_"], env["rstd_from_ps"], env["group_norm_store"]
    wq, wkv, VCs, VDs, DTR, ca_bias, catT = env["wq"], env["wkv"], env["VCs"], env["VDs"], env["DTR"], env["ca_bias"], env["catT"]
    rope_cos, rope_sin, FT2, VMs, SEL = env["rope_cos"], env["rope_sin"], env["FT2"], env["VMs"], env["SEL"]
    dt_bias, a_log, ssm_d, CF = env["dt_bias"], env["a_log"], env["ssm_d"], env["CF"]

    def ftload(dst, h, ch, src=FT, q="sp"):
        P.dma(q, lambda e: e.dma_start(out=dst[:, h, :], in_=src[ch * 128:(ch + 1) * 128, :]), reads=[nm_(src)], writes=[nm_(dst)])

    def mm(out, lhsT, rhs, start, stop, reads, writes, inc=None):
        P.op("pe", lambda e: e.matmul(out, lhsT=lhsT, rhs=rhs, start=start, stop=stop), reads=reads, writes=writes, inc=(stop if inc is None else inc), acc=not start)

    def mla_prep():
      with ExitStack() as st:
        LT = sb(st, "aLT", [128, 6, TB], BF16)
        KRt = sb(st, "aKR", [128, 2, TB], BF16)
        COS = sb(st, "aCOS", [128, TB], F32)
        SIN = sb(st, "aSIN", [128, TB], F32)
        SQ = sb(st, "aSQ", [128, 6, TB], BF16)
        NRM = sb(st, "aNRM", [128, 6, TB], BF16)
        tmp = sb(st, "atmp", [128, TB], F32)
        rstd = sb(st, "arstd", [128, TB], F32)
        t1 = sb(st, "at1", [128, TB], F32)
        t2 = sb(st, "at2", [128, TB], F32)
        WQ = sb(st, "aWQ", [128, 4, 1280], BF16)
        WKV = sb(st, "aWKV", [128, 2, 1536], BF16)
        STG = [sb(st, f"aSTG{i}", [128, TB], BF16) for i in range(3)]
        wql = wq[l].rearrange("(kc p) n -> p kc n", p=128)
        wkl = wkv[l].rearrange("(kc p) n -> p kc n", p=128)
        P.dma("pool", lambda e: e.dma_start(out=WQ[:, :, 0:768], in_=wql[:, :, :]), writes=["aWQ"])
        P.dma("pool", lambda e: e.dma_start(out=WKV[:, :, 0:1024], in_=wkl[:, :, :]), writes=["aWKV"])
        for h in range(4):
            P.dma("pool", lambda e, h=h: e.dma_start(out=WQ[:, :, 768 + 64 * h:768 + 64 * h + 64], in_=wql[:, :, 192 * h + 128:192 * h + 192]), writes=["aWQ"])
            P.dma("pool", lambda e, h=h: e.dma_start(out=WQ[:, :, 1024 + 64 * h:1024 + 64 * h + 32], in_=wql[:, :, 192 * h + 160:192 * h + 192]), writes=["aWQ"])
            P.dma("pool", lambda e, h=h: e.dma_start(out=WQ[:, :, 1024 + 64 * h + 32:1024 + 64 * h + 64], in_=wql[:, :, 192 * h + 128:192 * h + 160]), writes=["aWQ"])
            P.dma("pool", lambda e, h=h: e.dma_start(out=WKV[:, :, 1024 + 128 * h:1024 + 128 * h + 128], in_=wkl[:, :, 256 * h + 128:256 * h + 256]), writes=["aWKV"])
        ne = 0
        for tb in range(NTB):
            sl = slice(tb * TB, (tb + 1) * TB)
            for c in range(6):
                P.dma("sp", lambda e, c=c, sl=sl: e.dma_start(out=LT[:, c, :], in_=FT[(FT_CH["qlat"] + c) * 128:(FT_CH["qlat"] + c + 1) * 128, sl]), reads=["FT"], writes=["aLT"])
            for c in range(2):
                P.dma("sp", lambda e, c=c, sl=sl: e.dma_start(out=KRt[:, c, :], in_=FT[(FT_CH["kr"] + c) * 128:(FT_CH["kr"] + c + 1) * 128, sl]), reads=["FT"], writes=["aKR"])
            P.dma("act", lambda e, sl=sl: e.dma_start(out=COS[:], in_=rope_cos[:, sl]), writes=["aCOS"])
            P.dma("act", lambda e, sl=sl: e.dma_start(out=SIN[:], in_=rope_sin[:, sl]), writes=["aSIN"])
            P.op("act", lambda e: e.activation(out=SQ[:].rearrange("p a b -> p (a b)"), in_=LT[:].rearrange("p a b -> p (a b)"), func=AF.Square), reads=["aLT"], writes=["aSQ"])
            for (c0, n, gname, dim) in ((0, 4, "qn", 512), (4, 2, "kvn", 256)):
                for c in range(n):
                    mm(PS[6][:, :], ONESB, SQ[:, c0 + c, :], c == 0, c == n - 1, ["aSQ", "CB"], ["ps6"])
                rstd_from_ps(PS[6], rstd, dim, tmp)
                for c in range(n):
                    P.op("dve", lambda e, c=c, c0=c0, gname=gname: e.scalar_tensor_tensor(out=NRM[:, c0 + c, :], in0=LT[:, c0 + c, :], scalar=col(gname, c), in1=rstd[:], op0=ALU.mult, op1=ALU.mult),
                         reads=["aLT", "arstd", "COLS"], writes=["aNRM"])
            for h in range(8):
                ps = PS[ne % 4]
                stg = STG[ne % 3]
                ne += 1
                if h < 4:
                    for kc in range(4):
                        mm(ps[:, :], WQ[:, kc, 192 * h:192 * h + 128], NRM[:, kc, :], kc == 0, kc == 3, ["aWQ", "aNRM"], [nm_(ps)])
                    row = h
                else:
                    hh = h - 4
                    for kc in range(2):
                        mm(ps[:, :], WKV[:, kc, 256 * hh:256 * hh + 128], NRM[:, 4 + kc, :], kc == 0, kc == 1, ["aWKV", "aNRM"], [nm_(ps)])
                    row = 6 + hh
                P.op("act", lambda e, ps=ps, stg=stg: e.activation(out=stg[:], in_=ps[:, :], func=AF.Copy), reads=[nm_(ps)], writes=[nm_(stg)])
                P.dma("sp", lambda e, stg=stg, row=row, sl=sl: e.dma_start(out=FT2[row * 128:(row + 1) * 128, sl], in_=stg[:]), reads=[nm_(stg)], writes=["FT2"])
            for ab in range(2):
                psa, pss = PS[4], PS[5]
                for kc in range(4):
                    mm(psa[:, :], WQ[:, kc, 768 + 128 * ab:768 + 128 * ab + 128], NRM[:, kc, :], kc == 0, kc == 3, ["aWQ", "aNRM"], ["ps4"])
                for kc in range(4):
                    mm(pss[:, :], WQ[:, kc, 1024 + 128 * ab:1024 + 128 * ab + 128], NRM[:, kc, :], kc == 0, kc == 3, ["aWQ", "aNRM"], ["ps5"])
                stg = STG[ne % 3]
                ne += 1
                P.op("dve", lambda e: e.tensor_tensor(out=t1[:], in0=psa[:, :], in1=COS[:], op=ALU.mult), reads=["ps4", "aCOS"], writes=["at1"])
                P.op("dve", lambda e: e.tensor_tensor(out=t2[:], in0=pss[:, :], in1=SIN[:], op=ALU.mult), reads=["ps5", "aSIN"], writes=["at2"])
                P.op("dve", lambda e, stg=stg: e.tensor_tensor(out=stg[:], in0=t1[:], in1=t2[:], op=ALU.add), reads=["at1", "at2"], writes=[nm_(stg)])
                P.dma("sp", lambda e, stg=stg, ab=ab, sl=sl: e.dma_start(out=FT2[(4 + ab) * 128:(5 + ab) * 128, sl], in_=stg[:]), reads=[nm_(stg)], writes=["FT2"])
            stg = STG[ne % 3]
            ne += 1
            P.op("dve", lambda e: e.tensor_tensor(out=t1[:], in0=KRt[:, 0, :], in1=COS[:], op=ALU.mult), reads=["aKR", "aCOS"], writes=["at1"])
            P.op("dve", lambda e: e.tensor_tensor(out=t2[:], in0=KRt[:, 1, :], in1=SIN[:], op=ALU.mult), reads=["aKR", "aSIN"], writes=["at2"])
            P.op("dve", lambda e, stg=stg: e.tensor_tensor(out=stg[:], in0=t1[:], in1=t2[:], op=ALU.add), reads=["at1", "at2"], writes=[nm_(stg)])
            P.dma("sp", lambda e, stg=stg, sl=sl: e.dma_start(out=FT2[10 * 128:11 * 128, sl], in_=stg[:]), reads=[nm_(stg)], writes=["FT2"])
            for tt in range(4):
                ps = PS[ne % 4]
                stg = STG[ne % 3]
                ne += 1
                for kc in range(2):
                    mm(ps[:, :], NRM[:, 4 + kc, tt * 128:(tt + 1) * 128], WKV[:, kc, 1024:1536], kc == 0, kc == 1, ["aWKV", "aNRM"], [nm_(ps)])
                P.op("act", lambda e, ps=ps, stg=stg: e.activation(out=stg[:], in_=ps[:, :], func=AF.Copy), reads=[nm_(ps)], writes=[nm_(stg)])
                r0 = tb * TB + tt * 128
                P.dma("sp", lambda e, stg=stg, r0=r0: e.dma_start(out=VMs[r0:r0 + 128, :], in_=stg[:]), reads=[nm_(stg)], writes=["VM"])
    mla_prep()
    P.barrier()

    def attn_phase(kind):
        with ExitStack() as st:
            Q = sb(st, "tQ", [128, 4, S], BF16)
            Kt = sb(st, "tK", [128, 4, S], BF16)
            V = sb(st, "tV", [128, 32, 512], BF16)
            OA = sb(st, "tOA", [128, 4, TB], F32)
            SQs = sb(st, "tSQs", [128, 4, TB], BF16)
            tmp = sb(st, "ttmp", [128, TB], F32)
            rstd = sb(st, "trstd", [128, TB], F32)
            stg = sb(st, "tstg", [128, 4, TB], BF16)
            ATT = [sb(st, f"tATT{i}", [128, TB], BF16) for i in range(3)]
            RD = sb(st, "tRD", [128, TB], F32)
            if kind == "mla":
                QR = sb(st, "tQR", [128, 2, S], BF16)
                KRD = sb(st, "tKRD", [128, 1, S], BF16)
                for h in range(4):
                    ftload(Q, h, h, FT2)
                    ftload(Kt, h, 6 + h, FT2, "act")
                ftload(QR, 0, 4, FT2)
                ftload(QR, 1, 5, FT2)
                ftload(KRD, 0, 10, FT2)
                vsrc, scale, mrow, dst = VMs, 192.0 ** -0.5, 0, 0
            elif kind == "sb":
                for h in range(4):
                    ftload(Q, h, FT_CH["qc"] + h)
                    ftload(Kt, h, FT_CH["kc"] + h, FT, "act")
                vsrc, scale, mrow, dst = VCs, 128.0 ** -0.5, 4, 1024
                CAC = sb(st, "tCAC", [128, TB], F32)
                E1 = sb(st, "tE1", [128, TB], F32)
                SP = sb(st, "tSP", [128, TB], F32)
                NLK = sb(st, "tNLK", [128, TB], BF16)
                T1 = sb(st, "tT1", [128, TB], F32)
            else:
                for h in range(4):
                    ftload(Q, h, FT_CH["qd"] + h)
                    ftload(Kt, h, FT_CH["kd"] + h, FT, "act")
                vsrc, scale, mrow, dst = VDs, 128.0 ** -0.5, 8, 1536
                BT = sb(st, "tBT", [128, 20, 128], F32)
                P.dma("sp", lambda e: e.dma_start(out=BT[:], in_=ca_bias[l].rearrange("h j k q -> k (h j) q")), writes=["tBT"])
                P.op("dve", lambda e: e.tensor_scalar(out=BT[:].rearrange("p a b -> p (a b)"), in0=BT[:].rearrange("p a b -> p (a b)"), scalar1=float(128.0 ** 0.5), scalar2=None, op0=ALU.mult), reads=["tBT"], writes=["tBT"])
            P.dma("sp", lambda e: e.dma_start(out=V[:], in_=vsrc.rearrange("(b p) c -> p b c", p=128)), reads=[nm_(vsrc)], writes=["tV"])
            n = 0
            for QB in range(NTB):
                for h in range(4):
                    if kind == "ca":
                        for qi in range(4):
                            qt = QB * 4 + qi
                            blks = [jb for jb in range(5) if qt - 4 + jb >= 0]
                            for idx, jb in enumerate(blks):
                                kb = qt - 4 + jb
                                ps = PS[n % 2]
                                att = ATT[n % 3]
                                n += 1
                                mm(ps[:, :128], Kt[:, h, kb * 128:(kb + 1) * 128], Q[:, h, qt * 128:(qt + 1) * 128], True, False, ["tK", "tQ"], [nm_(ps)])
                                mm(ps[:, :128], IDF, BT[:, h * 5 + jb, :], False, True, ["tBT", "CF"], [nm_(ps)])
                                P.op("act", lambda e, ps=ps, att=att: e.activation(out=att[:, :128], in_=ps[:, :128], func=AF.Exp, scale=float(scale)), reads=[nm_(ps)], writes=[nm_(att)])
                                last = idx == len(blks) - 1
                                mm(PS[2][:, qi * 128:(qi + 1) * 128], V[:, kb, h * 128:(h + 1) * 128], att[:, :128], idx == 0, last, ["tV", nm_(att)], ["ps2"], inc=False)
                                mm(PS[3][:, qi * 128:(qi + 1) * 128], ONESB, att[:, :128], idx == 0, last, ["CB", nm_(att)], ["ps3"], inc=True)
                    elif kind == "mla":
                        nkb = 4 * QB + 4
                        for kb in range(nkb):
                            j = kb - 4 * QB
                            q0 = 128 * max(j, 0)
                            nn = TB - q0
                            qs = slice(QB * TB + q0, (QB + 1) * TB)
                            ps = PS[n % 2]
                            att = ATT[n % 3]
                            n += 1
                            r0 = 64 * (h % 2)
                            mm(ps[:, :nn], Kt[:, h, kb * 128:(kb + 1) * 128], Q[:, h, qs], True, False, ["tK", "tQ"], [nm_(ps)])
                            mm(ps[:, :nn], KRD[r0:r0 + 64, 0, kb * 128:(kb + 1) * 128], QR[r0:r0 + 64, h // 2, qs], False, True, ["tKRD", "tQR"], [nm_(ps)])
                            P.op("act", lambda e, ps=ps, att=att, nn=nn: e.activation(out=att[:, :nn], in_=ps[:, :nn], func=AF.Exp, scale=float(scale)), reads=[nm_(ps)], writes=[nm_(att)])
                            if j >= 0:
                                P.op("dve", lambda e, att=att: e.tensor_tensor(out=att[:, :128], in0=att[:, :128], in1=M_MLA, op=ALU.mult), reads=[nm_(att), "CF"], writes=[nm_(att)])
                            last = kb == nkb - 1
                            mm(PS[2][:, q0:], V[:, kb, h * 128:(h + 1) * 128], att[:, :nn], kb == 0, last, ["tV", nm_(att)], ["ps2"], inc=False)
                            mm(PS[3][:, q0:], ONESB, att[:, :nn], kb == 0, last, ["CB", nm_(att)], ["ps3"], inc=True)
                    else:
                        P.op("pool", lambda e: e.memset(CAC[:], 0.0), writes=["tCAC"])
                        mm(PS[2][:, :], ZB, Q[:, 0, 0:TB], True, False, ["CB", "tQ"], ["ps2"], inc=True)
                        nkb = 4 * QB + 4
                        for kb in range(nkb - 1, -1, -1):
                            j = kb - 4 * QB
                            q0 = 128 * max(j, 0)
                            nn = TB - q0
                            qs = slice(QB * TB + q0, (QB + 1) * TB)
                            ps = PS[n % 2]
                            att = ATT[n % 3]
                            n += 1
                            mm(ps[:, :nn], Kt[:, h, kb * 128:(kb + 1) * 128], Q[:, h, qs], True, True, ["tK", "tQ"], [nm_(ps)])
                            P.op("act", lambda e, ps=ps, nn=nn: e.activation(out=E1[:, :nn], in_=ps[:, :nn], func=AF.Exp, scale=float(-scale)), reads=[nm_(ps)], writes=["tE1"])
                            P.op("act", lambda e, nn=nn: e.activation(out=SP[:, :nn], in_=E1[:, :nn], func=AF.Ln, bias=1.0), reads=["tE1"], writes=["tSP"])
                            P.op("dve", lambda e, ps=ps, nn=nn: e.scalar_tensor_tensor(out=NLK[:, :nn], in0=ps[:, :nn], scalar=float(scale), in1=SP[:, :nn], op0=ALU.mult, op1=ALU.add), reads=[nm_(ps), "tSP"], writes=["tNLK"])
                            if j >= 0:
                                P.op("dve", lambda e: e.tensor_tensor(out=NLK[:, :128], in0=NLK[:, :128], in1=M_LT, op=ALU.mult), reads=["tNLK", "CF"], writes=["tNLK"])
                            mm(PS[4][:, :nn], TRIB, NLK[:, :nn], True, True, ["CB", "tNLK"], ["ps4"])
                            mm(PS[5][:, :nn], ONESB, NLK[:, :nn], True, True, ["CB", "tNLK"], ["ps5"])
                            P.op("dve", lambda e, nn=nn: e.tensor_tensor(out=T1[:, :nn], in0=PS[4][:, :nn], in1=SP[:, :nn], op=ALU.add), reads=["ps4", "tSP"], writes=["tT1"])
                            P.op("dve", lambda e, nn=nn, q0=q0: e.tensor_tensor(out=T1[:, :nn], in0=T1[:, :nn], in1=CAC[:, q0:], op=ALU.add), reads=["tT1", "tCAC"], writes=["tT1"])
                            P.op("act", lambda e, att=att, nn=nn: e.activation(out=att[:, :nn], in_=T1[:, :nn], func=AF.Exp, scale=-1.0), reads=["tT1"], writes=[nm_(att)])
                            if j >= 0:
                                P.op("dve", lambda e, att=att: e.tensor_tensor(out=att[:, :128], in0=att[:, :128], in1=M_LT, op=ALU.mult), reads=[nm_(att), "CF"], writes=[nm_(att)])
                            P.op("dve", lambda e, nn=nn, q0=q0: e.tensor_tensor(out=CAC[:, q0:], in0=CAC[:, q0:], in1=PS[5][:, :nn], op=ALU.add), reads=["ps5", "tCAC"], writes=["tCAC"])
                            mm(PS[2][:, q0:], V[:, kb, h * 128:(h + 1) * 128], att[:, :nn], False, kb == 0, ["tV", nm_(att)], ["ps2"], inc=True)
                    if kind == "sb":
                        P.op("act", lambda e, h=h: e.activation(out=OA[:, h, :], in_=PS[2][:, :], func=AF.Copy), reads=["ps2"], writes=["tOA"])
                    else:
                        P.op("dve", lambda e: e.reciprocal(out=RD[:], in_=PS[3][:, :]), reads=["ps3"], writes=["tRD"])
                        P.op("dve", lambda e, h=h: e.tensor_tensor(out=OA[:, h, :], in0=PS[2][:, :], in1=RD[:], op=ALU.mult), reads=["ps2", "tRD"], writes=["tOA"])
                group_norm_store(OA, 4, lambda c: col("mn", mrow + c), dst, QB, SQs, tmp, rstd, stg, 512)
        P.barrier()

    attn_phase("mla")
    attn_phase("sb")
    attn_phase("ca")
    ssd_phase(P, k, env, mm, ftload)
    P.barrier()


def ssd_phase(P, k, env, mm, ftload):
    nc = k.nc
    l = k.l
    PS, FT, col, IDF, ONESB, IDB, M_LE = env["PS"], env["FT"], env["col"], env["IDF"], env["ONESB"], env["IDB"], env["M_LE"]
    sb, nm_, rstd_from_ps = env["sb"], env["nm_"], env["rstd_from_ps"]
    DTR, catT, SEL, dt_bias, a_log, ssm_d, ssm_norm8 = env["DTR"], env["catT"], env["SEL"], env["dt_bias"], env["a_log"], env["ssm_d"], env["ssm_norm8"]
    XSs, CSs, CSCs, DTCs = env["XSs"], env["CSs"], env["CSCs"], env["DTCs"]
    def ssd_a():
      with ExitStack() as st:
        XC = sb(st, "sXC", [128, 3 + S], BF16)
        ACC = sb(st, "sACC", [128, S], F32)
        XF = sb(st, "sXF", [128, S], BF16)
        T1 = sb(st, "sT1", [128, S], F32)
        T2 = sb(st, "sT2", [128, S], F32)
        T3 = sb(st, "sT3", [128, S], F32)
        XO = [sb(st, f"sXO{i}", [128, 512], BF16) for i in range(2)]
        SM = sb(st, "sSM", [8, 4], F32)
        CC = sb(st, "sCC", [128, 32, 8], F32)
        P.op("pool", lambda e: e.memset(XC[:, 0:3], 0.0), writes=["sXC"])
        for c in range(8):
            ch = FT_CH["xs"] + c
            P.dma("sp", lambda e, ch=ch: e.dma_start(out=XC[:, 3:3 + S], in_=FT[ch * 128:(ch + 1) * 128, :]), reads=["FT"], writes=["sXC"])
            P.op("dve", lambda e, c=c: e.tensor_scalar(out=ACC[:], in0=XC[:, 3:3 + S], scalar1=col("cw", 3 * 8 + c), scalar2=None, op0=ALU.mult), reads=["sXC", "COLS"], writes=["sACC"])
            for j in (2, 1, 0):
                P.op("dve", lambda e, c=c, j=j: e.scalar_tensor_tensor(out=ACC[:], in0=XC[:, j:j + S], scalar=col("cw", j * 8 + c), in1=ACC[:], op0=ALU.mult, op1=ALU.add), reads=["sXC", "sACC", "COLS"], writes=["sACC"])
            P.op("act", lambda e, c=c: e.activation(out=XF[:], in_=ACC[:], func=AF.Silu, bias=col("cb", c), scale=1.0), reads=["sACC", "COLS"], writes=["sXF"])
            if c >= 4:
                P.dma("sp", lambda e, ch=ch: e.dma_start(out=FT[ch * 128:(ch + 1) * 128, :], in_=XF[:]), reads=["sXF"], writes=["FT"])
            else:
                for blk in range(32):
                    ps = PS[blk % 4]
                    xo = XO[blk % 2]
                    mm(ps[:, :128], XF[:, blk * 128:(blk + 1) * 128], IDB, True, True, ["sXF", "CB"], [nm_(ps)])
                    P.op("act", lambda e, ps=ps, xo=xo: e.activation(out=xo[:, :128], in_=ps[:, :128], func=AF.Copy), reads=[nm_(ps)], writes=[nm_(xo)])
                    P.dma("sp", lambda e, xo=xo, blk=blk, c=c: e.dma_start(out=XSs[blk * 128:(blk + 1) * 128, c * 128:(c + 1) * 128], in_=xo[:, :128]), reads=[nm_(xo)], writes=["XSs"])
        P.dma("sp", lambda e: e.dma_start(out=T1[:8, :], in_=DTR[:, :]), reads=["DTR"], writes=["sT1"])
        P.dma("sp", lambda e: e.dma_start(out=SM[:, 0:1], in_=dt_bias[l]), writes=["sSM"])
        P.dma("sp", lambda e: e.dma_start(out=SM[:, 1:2], in_=a_log[l]), writes=["sSM"])
        P.op("act", lambda e: e.activation(out=SM[:, 2:3], in_=SM[:, 1:2], func=AF.Exp), reads=["sSM"], writes=["sSM"])
        P.op("act", lambda e: e.activation(out=T1[:8, :], in_=T1[:8, :], func=AF.Exp, bias=SM[:, 0:1], scale=1.0), reads=["sT1", "sSM"], writes=["sT1"])
        P.op("act", lambda e: e.activation(out=T1[:8, :], in_=T1[:8, :], func=AF.Ln, bias=1.0), reads=["sT1"], writes=["sT1"])
        P.op("dve", lambda e: e.tensor_scalar(out=T2[:8, :], in0=T1[:8, :], scalar1=SM[:, 2:3], scalar2=-1.0, op0=ALU.mult, op1=ALU.mult), reads=["sT1", "sSM"], writes=["sT2"])
        P.op("pool", lambda e: e.memset(T3[:8, :], 1.0), writes=["sT3"])
        P.op("dve", lambda e: e.tensor_tensor_scan(out=ACC[:8, :], data0=T3[:8, :], data1=T2[:8, :], initial=0.0, op0=ALU.mult, op1=ALU.add), reads=["sT2", "sT3"], writes=["sACC"])
        P.dma("sp", lambda e: e.dma_start(out=CSs[:, :], in_=ACC[:8, :]), reads=["sACC"], writes=["CSs"])
        for (src, key, dstd) in ((ACC, "sACC", CSCs), (T1, "sT1", DTCs)):
            for blk in range(32):
                mm(PS[blk % 4][:, :8], src[:8, blk * 128:(blk + 1) * 128], IDF[:8, :8], True, True, [key, "CF"], [nm_(PS[blk % 4])])
                P.op("dve", lambda e, blk=blk: e.tensor_copy(out=CC[:, blk, :], in_=PS[blk % 4][:, :8]), reads=[nm_(PS[blk % 4])], writes=["sCC"])
            P.dma("sp", lambda e, dstd=dstd: e.dma_start(out=dstd[:, :], in_=CC[:].rearrange("p a b -> p (a b)")), reads=["sCC"], writes=[nm_(dstd)])
    ssd_a()
    P.barrier()
    def ssd_b():
      with ExitStack() as st:
        BTt = sb(st, "uB", [128, 2, S], BF16)
        CTt = sb(st, "uC", [128, 2, S], BF16)
        XS = sb(st, "uXS", [128, 32, 512], BF16)
        CSr = sb(st, "uCSr", [8, S], F32)
        CSC = sb(st, "uCSC", [128, 32, 8], F32)
        DTC = sb(st, "uDTC", [128, 32, 8], F32)
        CSB = sb(st, "uCSB", [128, 8, TB], F32)
        DIF = [sb(st, f"uDIF{i}", [128, TB], F32) for i in range(2)]
        W = [sb(st, f"uW{i}", [128, TB], BF16) for i in range(3)]
        ZT = sb(st, "uZT", [64, 8, TB], BF16)
        SZ = sb(st, "uSZ", [64, TB], F32)
        OAh = sb(st, "uOA", [64, 8, TB], F32)
        SQs = sb(st, "uSQs", [64, 8, TB], BF16)
        stg = sb(st, "ustg", [64, 8, TB], BF16)
        tmp = sb(st, "utmp", [128, TB], F32)
        rstd = sb(st, "urstd", [128, TB], F32)
        DSK = sb(st, "uDSK", [128, 8], F32)
        DI = sb(st, "uDI", [128, 8, 128], F32)
        G8 = sb(st, "uG8", [64, 8], F32)
        G8t = sb(st, "uG8t", [8, 64], F32)
        for g in range(2):
            ftload(BTt, g, FT_CH["bm"] + g)
            ftload(CTt, g, FT_CH["cm"] + g, FT, "act")
        P.dma("sp", lambda e: e.dma_start(out=XS[:], in_=XSs.rearrange("(b p) c -> p b c", p=128)), reads=["XSs"], writes=["uXS"])
        P.dma("sp", lambda e: e.dma_start(out=CSr[:], in_=CSs[:, :]), reads=["CSs"], writes=["uCSr"])
        P.dma("sp", lambda e: e.dma_start(out=CSC[:].rearrange("p a b -> p (a b)"), in_=CSCs[:, :]), reads=["CSCs"], writes=["uCSC"])
        P.dma("sp", lambda e: e.dma_start(out=DTC[:].rearrange("p a b -> p (a b)"), in_=DTCs[:, :]), reads=["DTCs"], writes=["uDTC"])
        P.dma("sp", lambda e: e.dma_start(out=DSK[:], in_=ssm_d[l:l + 1, :].partition_broadcast(128)), writes=["uDSK"])
        P.dma("sp", lambda e: e.dma_start(out=G8t[:], in_=ssm_norm8[l]), writes=["uG8t"])
        mm(PS[7][:64, :8], G8t[:, :], IDF[:8, :8], True, True, ["uG8t", "CF"], ["ps7"])
        P.op("dve", lambda e: e.tensor_copy(out=G8[:], in_=PS[7][:64, :8]), reads=["ps7"], writes=["uG8"])
        for h in range(8):
            P.op("dve", lambda e, h=h: e.tensor_scalar(out=DI[:, h, :], in0=IDF, scalar1=DSK[:, h:h + 1], scalar2=None, op0=ALU.mult), reads=["CF", "uDSK"], writes=["uDI"])
        n = 0
        for QB in range(NTB):
            sl = slice(QB * TB, (QB + 1) * TB)
            for h in range(8):
                mm(PS[7][:, :], SEL[:, h * 128:(h + 1) * 128], CSr[:, sl], True, True, ["SEL", "uCSr"], ["ps7"])
                P.op("act", lambda e, h=h: e.activation(out=CSB[:, h, :], in_=PS[7][:, :], func=AF.Copy), reads=["ps7"], writes=["uCSB"])
                P.dma("act", lambda e, h=h, sl=sl: e.dma_start(out=ZT[:, h, :], in_=FT[FT_CH["z"] * 128 + 64 * h:FT_CH["z"] * 128 + 64 * h + 64, sl]), reads=["FT"], writes=["uZT"])
            for g in range(2):
                nkb = 4 * QB + 4
                for kb in range(nkb):
                    j = kb - 4 * QB
                    q0 = 128 * max(j, 0)
                    nn = TB - q0
                    gp = PS[n % 2]
                    n += 1
                    mm(gp[:, :nn], BTt[:, g, kb * 128:(kb + 1) * 128], CTt[:, g, QB * TB + q0:(QB + 1) * TB], True, True, ["uB", "uC"], [nm_(gp)])
                    for r in range(4):
                        h = 4 * g + r
                        dif = DIF[(n + r) % 2]
                        w = W[(n + r) % 3]
                        if j >= 0:
                            P.op("dve", lambda e, dif=dif, h=h, kb=kb, q0=q0, nn=nn: e.tensor_scalar(out=dif[:, :nn], in0=CSB[:, h, q0:], scalar1=CSC[:, kb, h:h + 1], scalar2=0.0, op0=ALU.subtract, op1=ALU.min), reads=["uCSB", "uCSC"], writes=[nm_(dif)])
                        else:
                            P.op("dve", lambda e, dif=dif, h=h, kb=kb, q0=q0, nn=nn: e.tensor_scalar(out=dif[:, :nn], in0=CSB[:, h, q0:], scalar1=CSC[:, kb, h:h + 1], scalar2=None, op0=ALU.subtract), reads=["uCSB", "uCSC"], writes=[nm_(dif)])
                        P.op("act", lambda e, dif=dif, nn=nn: e.activation(out=dif[:, :nn], in_=dif[:, :nn], func=AF.Exp), reads=[nm_(dif)], writes=[nm_(dif)])
                        P.op("dve", lambda e, dif=dif, w=w, gp=gp, h=h, kb=kb, nn=nn: e.scalar_tensor_tensor(out=w[:, :nn], in0=dif[:, :nn], scalar=DTC[:, kb, h:h + 1], in1=gp[:, :nn], op0=ALU.mult, op1=ALU.mult), reads=[nm_(dif), nm_(gp), "uDTC"], writes=[nm_(w)])
                        if j >= 0:
                            P.op("dve", lambda e, w=w: e.tensor_tensor(out=w[:, :128], in0=w[:, :128], in1=M_LE, op=ALU.mult), reads=[nm_(w), "CF"], writes=[nm_(w)])
                            P.op("dve", lambda e, w=w, h=h: e.tensor_tensor(out=w[:, :128], in0=w[:, :128], in1=DI[:, h, :], op=ALU.add), reads=[nm_(w), "uDI"], writes=[nm_(w)])
                        mm(PS[2 + r][:64, q0:], XS[:, kb, h * 64:(h + 1) * 64], w[:, :nn], kb == 0, kb == nkb - 1, ["uXS", nm_(w)], [f"ps{2 + r}"], inc=True)
                for r in range(4):
                    h = 4 * g + r
                    P.op("act", lambda e, h=h: e.activation(out=SZ[:], in_=ZT[:, h, :], func=AF.Silu), reads=["uZT"], writes=["uSZ"])
                    P.op("dve", lambda e, h=h, r=r: e.tensor_tensor(out=OAh[:, h, :], in0=PS[2 + r][:64, :], in1=SZ[:], op=ALU.mult), reads=[f"ps{2 + r}", "uSZ"], writes=["uOA"])
                    P.op("act", lambda e, h=h: e.activation(out=SQs[:, h, :], in_=OAh[:, h, :], func=AF.Square), reads=["uOA"], writes=["uSQs"])
                for r in range(4):
                    mm(PS[6][:, :], ONESB[0:64, :], SQs[:, 4 * g + r, :], r == 0, r == 3, ["uSQs", "CB"], ["ps6"])
                rstd_from_ps(PS[6], rstd, 256, tmp)
                for r in range(4):
                    h = 4 * g + r
                    P.op("dve", lambda e, h=h: e.scalar_tensor_tensor(out=stg[:, h, :], in0=OAh[:, h, :], scalar=G8[:, h:h + 1], in1=rstd[:64, :], op0=ALU.mult, op1=ALU.mult), reads=["uOA", "urstd", "uG8"], writes=["ustg"])
            P.dma("sp", lambda e, sl=sl: e.dma_start(out=catT[512:1024, sl].rearrange("(h p) t -> p h t", p=64), in_=stg[:]), reads=["ustg"], writes=["catT"])
    ssd_b()


def tail(P, k, env, n_exp):
    nc = k.nc
    l = k.l
    PS, xT, col, IDF, ONESB, ONESF = env["PS"], env["xT"], env["col"], env["IDF"], env["ONESB"], env["ONESF"]
    sb, nm_, norm_mod, load_xT_blk = env["sb"], env["nm_"], env["norm_mod"], env["load_xT_blk"]
    catT, h2T, GTs, w_out, router_w, router_b = env["catT"], env["h2T"], env["GTs"], env["w_out"], env["router_w"], env["router_b"]
    w1, b1, w2, b2, sel32 = env["w1"], env["b1"], env["w2"], env["b2"], env["sel32"]
    def p3():
      with ExitStack() as st:
        CT = sb(st, "oCT", [128, NKC, TB], BF16)
        XT = sb(st, "oXT", [128, NKC, TB], F32)
        WO = [sb(st, f"oWO{i}", [128, NKC, 512], BF16) for i in range(2)]
        wl = w_out[l].rearrange("(kc p) n -> p kc n", p=128)
        nw = 0
        for tb in range(NTB):
            load_xT_blk(CT, tb, src=catT, q="act")
            load_xT_blk(XT, tb)
            for nb in range(4):
                wo = WO[nw % 2]
                nw += 1
                P.dma("pool", lambda e, wo=wo, nb=nb: e.dma_start(out=wo[:], in_=wl[:, :, nb * 512:(nb + 1) * 512]), writes=[nm_(wo)])
                for j in range(4):
                    n = nb * 4 + j
                    ps = PS[n % 4]
                    for kc in range(NKC):
                        P.op("pe", lambda e, ps=ps, wo=wo, kc=kc, j=j: e.matmul(ps[:, :], lhsT=wo[:, kc, j * 128:(j + 1) * 128], rhs=CT[:, kc, :], start=(kc == 0), stop=(kc == NKC - 1)),
                             reads=[nm_(wo), "oCT"], writes=[nm_(ps)], inc=(kc == NKC - 1), acc=(kc > 0))
                    P.op("dve", lambda e, ps=ps, n=n: e.scalar_tensor_tensor(out=XT[:, n, :], in0=ps[:, :], scalar=col("mod", 32 + n), in1=XT[:, n, :], op0=ALU.mult, op1=ALU.add),
                         reads=[nm_(ps), "oXT", "COLS"], writes=["oXT"])
            P.dma("sp", lambda e, tb=tb: e.dma_start(out=xT[:, tb * TB:(tb + 1) * TB].rearrange("(kc p) t -> p kc t", p=128), in_=XT[:]), reads=["oXT"], writes=["xT"])
    p3()
    P.barrier()
    import os
    if os.environ.get("SKIP_P4"):
        return
    if getattr(k, "dbg_out", False):
        dd0 = nc.dram_tensor("d_x1T", [D, S], F32, kind="ExternalOutput").ap()
        P.dma("sp", lambda e: e.dma_start(out=dd0[:, :], in_=xT[:, :]), reads=[], writes=["d_x1T"])
        P.barrier()
    def p4a():
      with ExitStack() as st:
        XT = sb(st, "rXT", [128, NKC, TB], F32)
        HF = sb(st, "rHF", [128, NKC, TB], F32)
        HT = sb(st, "rHT", [128, NKC, TB], BF16)
        SQ = sb(st, "rSQ", [128, NKC, TB], BF16)
        tmp = sb(st, "rtmp", [128, TB], F32)
        rstd = sb(st, "rrstd", [128, TB], F32)
        t2 = sb(st, "rt2", [128, TB], F32)
        RW = sb(st, "rRW", [128, NKC, NEXP], F32)
        RB = sb(st, "rRB", [1, NEXP], F32)
        LG = sb(st, "rLG", [128, NEXP], F32)
        M8 = sb(st, "rM8", [128, 8], F32)
        NM = sb(st, "rNM", [128, 1], F32)
        MK = sb(st, "rMK", [128, NEXP], F32)
        EE = sb(st, "rEE", [128, NEXP], F32)
        SM = sb(st, "rSM", [128, 1], F32)
        GG = sb(st, "rGG", [128, NEXP], F32)
        GTt = sb(st, "rGT", [32, TB], F32)
        RBB = sb(st, "rRBB", [128, NEXP], F32)
        if not os.environ.get("SKIP_RW"):
            P.dma("sp", lambda e: e.dma_start(out=RW[:], in_=router_w[l].rearrange("(kc p) n -> p kc n", p=128)), writes=["rRW"])
        if not os.environ.get("SKIP_RBB"):
            P.dma("sp", lambda e: e.dma_start(out=RBB[:], in_=router_b[l].partition_broadcast(128)), writes=["rRBB"])
        for tb in range(NTB):
            if not os.environ.get("SKIP_XTLOAD"):
                load_xT_blk(XT, tb)
            if not os.environ.get("SKIP_NORM2"):
                norm_mod((SQ, tmp, rstd, t2), XT, HT, lambda kc: col("A2", kc), lambda kc: col("mod", 48 + kc), tb, HF=(None if os.environ.get("NO_HF") else HF))
            if not os.environ.get("SKIP_H2T"):
                P.dma("sp", lambda e, tb=tb: e.dma_start(out=h2T[:, tb * TB:(tb + 1) * TB].rearrange("(kc p) t -> p kc t", p=128), in_=HT[:]), reads=["rHT"], writes=["h2T"])
            if os.environ.get("SKIP_ROUTER"):
                continue
            for tt in range(4):
                ps = PS[tt % 2]
                for kc in range(NKC):
                    P.op("pe", lambda e, ps=ps, kc=kc, tt=tt: e.matmul(ps[:, :NEXP], lhsT=HF[:, kc, tt * 128:(tt + 1) * 128], rhs=RW[:, kc, :], start=(kc == 0), stop=(kc == NKC - 1)),
                         reads=["rHF", "rRW"], writes=[nm_(ps)], inc=(kc == NKC - 1), acc=(kc > 0))
                P.op("dve", lambda e, ps=ps: e.tensor_tensor(out=LG[:], in0=ps[:, :NEXP], in1=RBB[:], op=ALU.add), reads=[nm_(ps), "rRBB"], writes=["rLG"])
                P.op("dve", lambda e: e.max(out=M8[:], in_=LG[:]), reads=["rLG"], writes=["rM8"])
                P.op("dve", lambda e: e.tensor_scalar(out=NM[:], in0=M8[:, 0:1], scalar1=-1.0, scalar2=None, op0=ALU.mult), reads=["rM8"], writes=["rNM"])
                P.op("dve", lambda e: e.tensor_scalar(out=MK[:], in0=LG[:], scalar1=M8[:, 3:4], scalar2=1e30, op0=ALU.subtract, op1=ALU.mult), reads=["rLG", "rM8"], writes=["rMK"])
                P.op("dve", lambda e: e.tensor_scalar(out=MK[:], in0=MK[:], scalar1=1.0, scalar2=0.0, op0=ALU.add, op1=ALU.max), reads=["rMK"], writes=["rMK"])
                P.op("dve", lambda e: e.tensor_scalar(out=MK[:], in0=MK[:], scalar1=1.0, scalar2=None, op0=ALU.min), reads=["rMK"], writes=["rMK"])
                P.op("act", lambda e: e.activation(out=EE[:], in_=LG[:], func=AF.Exp, bias=NM[:, 0:1], scale=1.0), reads=["rLG", "rNM"], writes=["rEE"])
                P.op("dve", lambda e: e.tensor_tensor(out=EE[:], in0=EE[:], in1=MK[:], op=ALU.mult), reads=["rEE", "rMK"], writes=["rEE"])
                P.op("dve", lambda e: e.tensor_reduce(out=SM[:], in_=EE[:], axis=mybir.AxisListType.X, op=ALU.add), reads=["rEE"], writes=["rSM"])
                P.op("dve", lambda e: e.tensor_scalar(out=SM[:], in0=SM[:], scalar1=1e-30, scalar2=None, op0=ALU.max), reads=["rSM"], writes=["rSM"])
                P.op("dve", lambda e: e.reciprocal(out=SM[:], in_=SM[:]), reads=["rSM"], writes=["rSM"])
                P.op("dve", lambda e: e.tensor_scalar(out=GG[:], in0=EE[:], scalar1=SM[:, 0:1], scalar2=None, op0=ALU.mult), reads=["rEE", "rSM"], writes=["rGG"])
                P.op("pe", lambda e, tt=tt: e.matmul(PS[2][:NEXP, tt * 128:(tt + 1) * 128], lhsT=GG[:], rhs=IDF, start=True, stop=True), reads=["rGG", "CF"], writes=["ps2"])
            P.op("act", lambda e: e.activation(out=GTt[:], in_=PS[2][:NEXP, :], func=AF.Copy), reads=["ps2"], writes=["rGT"])
            P.dma("sp", lambda e, tb=tb: e.dma_start(out=GTs[:, tb * TB:(tb + 1) * TB], in_=GTt[:]), reads=["rGT"], writes=["GT"])
        if getattr(k, "dbg_out", False):
            for nm2, t_, shp in (("d_LG", LG, [128, NEXP]), ("d_M8", M8, [128, 8]), ("d_GG", GG, [128, NEXP]), ("d_RW", RW, [128, NKC * NEXP]), ("d_RBB", RBB, [128, NEXP]), ("d_HF", HF, [128, NKC * TB]), ("d_GTt", GTt, [32, TB]), ("d_XT", XT, [128, NKC * TB]), ("d_rstd", rstd, [128, TB]), ("d_tmp", tmp, [128, TB])):
                dd = nc.dram_tensor(nm2, shp, F32, kind="ExternalOutput").ap()
                src = t_[:] if len(t_.shape) == 2 else t_[:].rearrange("p a b -> p (a b)")
                P.dma("sp", lambda e, dd=dd, src=src: e.dma_start(out=dd[:, :], in_=src), reads=[nm_(t_)], writes=[nm2])
    p4a()
    P.barrier()
    if os.environ.get("SKIP_P4B"):
        return
    def p4b():
      with ExitStack() as st:
        HT = sb(st, "mHT", [128, NKC, TB], BF16)
        YA = sb(st, "mYA", [128, NKC, TB], F32)
        XC = [sb(st, f"mXC{i}", [128, TB], F32) for i in range(2)]
        W1 = [sb(st, f"mW1{i}", [128, NKC, 384], BF16) for i in range(3)]
        W2 = sb(st, "mW2", [128, 6, D], BF16)
        SGL = sb(st, "mSGL", [128, 6, TB], F32)
        AT = sb(st, "mAT", [128, 6, TB], BF16)
        GBC = sb(st, "mGBC", [128, TB], F32)
        G1 = sb(st, "mG1", [128, TB], F32)
        SG = sb(st, "mSG", [128, TB], F32)
        U1 = sb(st, "mU1", [128, TB], F32)
        GTt = sb(st, "mGT", [32, TB], F32)
        SEL = sb(st, "mSEL", [32, 32 * 128], F32)
        B2 = sb(st, "mB2", [32, D], F32)
        B1C = sb(st, "mB1C", [128, 384], F32)
        B1T = sb(st, "mB1T", [128, 128], F32)
        P.dma("sp", lambda e: e.dma_start(out=SEL[:], in_=sel32[:, :]), writes=["mSEL"])
        P.dma("sp", lambda e: e.dma_start(out=B2[:], in_=b2[l]), writes=["mB2"])
        for i in range(3):
            P.dma("sp", lambda e, i=i: e.dma_start(out=B1T[:], in_=b1[l][i * 128:(i + 1) * 128, :]), writes=["mB1T"])
            P.op("pe", lambda e: e.matmul(PS[7][:, :128], lhsT=B1T[:], rhs=IDF, start=True, stop=True), reads=["mB1T", "CF"], writes=["ps7"])
            P.op("dve", lambda e, i=i: e.tensor_copy(out=B1C[:, i * 128:(i + 1) * 128], in_=PS[7][:, :128]), reads=["ps7"], writes=["mB1C"])
        nw = 0
        npz = 0
        for tb in range(NTB):
            load_xT_blk(HT, tb, src=h2T)
            P.dma("sp", lambda e, tb=tb: e.dma_start(out=GTt[:], in_=GTs[:, tb * TB:(tb + 1) * TB]), reads=["GT"], writes=["mGT"])
            for n in range(NKC):
                ps = PS[npz % 4]
                npz += 1
                P.op("pe", lambda e, ps=ps, n=n: e.matmul(ps[:, :], lhsT=B2[:, n * 128:(n + 1) * 128], rhs=GTt[:], start=True, stop=True), reads=["mB2", "mGT"], writes=[nm_(ps)])
                P.op("act", lambda e, ps=ps, n=n: e.activation(out=YA[:, n, :], in_=ps[:, :], func=AF.Copy), reads=[nm_(ps)], writes=["mYA"])
            for ex in range(n_exp):
                P.op("pe", lambda e, ex=ex: e.matmul(PS[5][:, :], lhsT=SEL[:, ex * 128:(ex + 1) * 128], rhs=GTt[:], start=True, stop=True), reads=["mSEL", "mGT"], writes=["ps5"])
                P.op("act", lambda e: e.activation(out=GBC[:], in_=PS[5][:, :], func=AF.Copy), reads=["ps5"], writes=["mGBC"])
                P.dma("pool", lambda e, ex=ex: e.dma_start(out=W2[:], in_=w2[l][ex].rearrange("(fc p) n -> p fc n", p=128)), writes=["mW2"])
                w1l = w1[l][ex].rearrange("(kc p) n -> p kc n", p=128)
                for pc in range(4):
                    wt = W1[nw % 3]
                    nw += 1
                    P.dma("pool", lambda e, wt=wt, pc=pc, w1l=w1l: e.dma_start(out=wt[:], in_=w1l[:, :, pc * 384:(pc + 1) * 384]), writes=[nm_(wt)])
                    for f3 in range(3):
                        f = pc * 3 + f3
                        ps = PS[npz % 4]
                        npz += 1
                        for kc in range(NKC):
                            P.op("pe", lambda e, ps=ps, wt=wt, kc=kc, f3=f3: e.matmul(ps[:, :], lhsT=wt[:, kc, f3 * 128:(f3 + 1) * 128], rhs=HT[:, kc, :], start=(kc == 0), stop=(kc == NKC - 1)),
                                 reads=[nm_(wt), "mHT"], writes=[nm_(ps)], inc=(kc == NKC - 1), acc=(kc > 0))
                        bc = B1C[:, ex * 12 + f:ex * 12 + f + 1]
                        if f < 6:
                            P.op("dve", lambda e, ps=ps, bc=bc: e.tensor_scalar(out=G1[:], in0=ps[:, :], scalar1=bc, scalar2=7.0, op0=ALU.add, op1=ALU.min), reads=[nm_(ps), "mB1C"], writes=["mG1"])
                            P.op("act", lambda e: e.activation(out=SG[:], in_=G1[:], func=AF.Sigmoid, scale=1.702), reads=["mG1"], writes=["mSG"])
                            P.op("dve", lambda e, f=f: e.tensor_tensor(out=SGL[:, f, :], in0=G1[:], in1=SG[:], op=ALU.mult), reads=["mG1", "mSG"], writes=["mSGL"])
                        else:
                            j = f - 6
                            P.op("dve", lambda e, ps=ps, bc=bc: e.tensor_scalar(out=U1[:], in0=ps[:, :], scalar1=bc, scalar2=7.0, op0=ALU.add, op1=ALU.min), reads=[nm_(ps), "mB1C"], writes=["mU1"])
                            P.op("dve", lambda e: e.tensor_scalar(out=U1[:], in0=U1[:], scalar1=-7.0, scalar2=1.0, op0=ALU.max, op1=ALU.add), reads=["mU1"], writes=["mU1"])
                            P.op("dve", lambda e, j=j: e.tensor_tensor(out=U1[:], in0=U1[:], in1=SGL[:, j, :], op=ALU.mult), reads=["mU1", "mSGL"], writes=["mU1"])
                            P.op("dve", lambda e, j=j: e.tensor_tensor(out=AT[:, j, :], in0=U1[:], in1=GBC[:], op=ALU.mult), reads=["mU1", "mGBC"], writes=["mAT"])
                for n in range(NKC):
                    ps = PS[npz % 4]
                    npz += 1
                    for fc in range(6):
                        P.op("pe", lambda e, ps=ps, fc=fc, n=n: e.matmul(ps[:, :], lhsT=W2[:, fc, n * 128:(n + 1) * 128], rhs=AT[:, fc, :], start=(fc == 0), stop=(fc == 5)),
                             reads=["mW2", "mAT"], writes=[nm_(ps)], inc=(fc == 5), acc=(fc > 0))
                    P.op("dve", lambda e, ps=ps, n=n: e.tensor_tensor(out=YA[:, n, :], in0=YA[:, n, :], in1=ps[:, :], op=ALU.add), reads=[nm_(ps), "mYA"], writes=["mYA"])
            for n in range(NKC):
                xc = XC[n % 2]
                P.dma("sp", lambda e, xc=xc, n=n, tb=tb: e.dma_start(out=xc[:], in_=xT[n * 128:(n + 1) * 128, tb * TB:(tb + 1) * TB]), reads=["xT"], writes=[nm_(xc)])
                P.op("dve", lambda e, xc=xc, n=n: e.scalar_tensor_tensor(out=xc[:], in0=YA[:, n, :], scalar=col("mod", 80 + n), in1=xc[:], op0=ALU.mult, op1=ALU.add),
                     reads=["mYA", nm_(xc), "COLS"], writes=[nm_(xc)])
                P.dma("sp", lambda e, xc=xc, n=n, tb=tb: e.dma_start(out=xT[n * 128:(n + 1) * 128, tb * TB:(tb + 1) * TB], in_=xc[:]), reads=[nm_(xc)], writes=["xT"])
    p4b()


def final(P, k, env):
    nc = k.nc
    PS, xT, y_out, col, IDF, ONESB = env["PS"], env["xT"], env["y_out"], env["col"], env["IDF"], env["ONESB"]
    sb, nm_, rstd_from_ps = env["sb"], env["nm_"], env["rstd_from_ps"]
    with ExitStack() as st:
        XT = sb(st, "fXT", [128, NKC, TB], F32)
        SQ = sb(st, "fSQ", [128, NKC, TB], BF16)
        tmp = sb(st, "ftmp", [128, TB], F32)
        rstd = sb(st, "frstd", [128, TB], F32)
        OT = [sb(st, f"fOT{i}", [128, D], F32) for i in range(2)]
        nn_ = [0]
        xo_out = env.get("xo_out")

        def emit_T(dst, dkey, tb):
            for tt in range(4):
                ot = OT[nn_[0] % 2]
                nn_[0] += 1
                for g in range(4):
                    ps = PS[g]
                    for j in range(4):
                        kc = g * 4 + j
                        P.op("pe", lambda e, ps=ps, j=j, kc=kc, tt=tt: e.matmul(ps[:, j * 128:(j + 1) * 128], lhsT=XT[:, kc, tt * 128:(tt + 1) * 128], rhs=IDF, start=True, stop=True),
                             reads=["fXT", "CF"], writes=[nm_(ps)], inc=(j == 3))
                    P.op("act", lambda e, ps=ps, ot=ot, g=g: e.activation(out=ot[:, g * 512:(g + 1) * 512], in_=ps[:, :], func=AF.Copy), reads=[nm_(ps)], writes=[nm_(ot)])
                r0 = tb * TB + tt * 128
                P.dma("sp", lambda e, ot=ot, r0=r0, dst=dst: e.dma_start(out=dst[r0:r0 + 128, :], in_=ot[:]), reads=[nm_(ot)], writes=[dkey])

        for tb in range(NTB):
            P.dma("sp", lambda e, tb=tb: e.dma_start(out=XT[:], in_=xT[:, tb * TB:(tb + 1) * TB].rearrange("(kc p) t -> p kc t", p=128)),
                  reads=["xT"], writes=["fXT"])
            if xo_out is not None:
                emit_T(xo_out, "xo", tb)
            P.op("act", lambda e: e.activation(out=SQ[:].rearrange("p a b -> p (a b)"), in_=XT[:].rearrange("p a b -> p (a b)"), func=AF.Square),
                 reads=["fXT"], writes=["fSQ"])
            for kc in range(NKC):
                P.op("pe", lambda e, kc=kc: e.matmul(PS[6][:, :], lhsT=ONESB, rhs=SQ[:, kc, :], start=(kc == 0), stop=(kc == NKC - 1)),
                     reads=["fSQ", "CB"], writes=["ps6"], inc=(kc == NKC - 1), acc=(kc > 0))
            rstd_from_ps(PS[6], rstd, D, tmp)
            for kc in range(NKC):
                P.op("dve", lambda e, kc=kc: e.scalar_tensor_tensor(out=XT[:, kc, :], in0=XT[:, kc, :], scalar=col("fin", kc), in1=rstd[:], op0=ALU.mult, op1=ALU.mult),
                     reads=["fXT", "frstd", "COLS"], writes=["fXT"])
            emit_T(y_out, "y", tb)
    P.barrier()


def _consts():
    p = np.arange(128)[:, None]
    f = np.arange(128)[None, :]
    c = np.zeros((128, 8, 128), np.float32)
    c[:, 0] = (p == f)
    c[:, 1] = 1.0
    c[:, 2] = (p < f)
    c[:, 3] = (p <= f)
    c[:, 4] = ((p // 64) <= (f // 64))
    c[:, 5] = (p > f)
    sel = np.zeros((8, 8 * 128), np.float32)
    for h in range(8):
        sel[h, h * 128:(h + 1) * 128] = 1.0
    sel32 = np.zeros((32, 32 * 128), np.float32)
    for h in range(32):
        sel32[h, h * 128:(h + 1) * 128] = 1.0
    inv = 1.0 / (10000.0 ** (np.arange(0, 64, 2, dtype=np.float32) / 64))
    ang = np.arange(S, dtype=np.float32)[:, None] * inv[None, :]
    cosT = np.cos(ang).T.astype(np.float32)
    sinT = np.sin(ang).T.astype(np.float32)
    rc = np.concatenate([cosT, cosT, cosT, cosT], 0)
    rs = np.concatenate([-sinT, sinT, -sinT, sinT], 0)
    return c, sel, np.ascontiguousarray(rc), np.ascontiguousarray(rs), sel32


def _ca_bias_tables(ca_rel_bias):
    L = ca_rel_bias.shape[0]
    kk = np.arange(640)[:, None]
    qq = np.arange(128)[None, :]
    dist = (512 + qq) - kk
    idx = np.clip(dist, -63, 256) + 63
    qch = (512 + qq) // 64
    kch = kk // 64
    valid = (kch <= qch) & (kch >= qch - 8)
    out = np.empty((L, 4, 640, 128), np.float32)
    for l in range(L):
        for h in range(4):
            t = ca_rel_bias[l, h][idx]
            out[l, h] = np.where(valid, t, np.float32(-30000.0))
    return np.ascontiguousarray(out.reshape(L, 4, 5, 128, 128))


_CACHE = {}


def prep_inputs(inputs, depth, l0=0):
    L = depth
    cst, sel, rc, rs, sel32 = _consts()
    g = lambda n: np.ascontiguousarray(np.asarray(inputs[n], np.float32)[l0:l0 + L])
    shared = {
        "attn_norm": g("attn_norm").reshape(L, 16, 128), "ffn_norm": g("ffn_norm").reshape(L, 16, 128),
        "mod_w": g("mod_w"), "mod_b": g("mod_b").reshape(L, 96, 128), "w_in": g("w_in"),
        "mla_q_norm": g("mla_q_norm").reshape(L, 4, 128), "mla_w_q_up": g("mla_w_q_up"),
        "mla_kv_norm": g("mla_kv_norm").reshape(L, 2, 128), "mla_w_kv_up": g("mla_w_kv_up"),
        "ssm_conv_w": g("ssm_conv_w").reshape(L, 32, 128), "ssm_conv_b": g("ssm_conv_b").reshape(L, 8, 128),
        "ssm_dt_bias": g("ssm_dt_bias").reshape(L, 8, 1), "ssm_a_log": g("ssm_a_log").reshape(L, 8, 1),
        "ssm_d": g("ssm_d"), "ssm_norm": g("ssm_norm").reshape(L, 4, 128), "ssm_norm8": g("ssm_norm").reshape(L, 8, 64),
        "ca_biasT": _ca_bias_tables(g("ca_rel_bias")), "mix_out_norm": g("mix_out_norm").reshape(L, 12, 128),
        "w_out": g("w_out"), "router_w": g("router_w"), "router_b": g("router_b").reshape(L, 1, NEXP),
        "moe_w1": g("moe_w1"), "moe_b1": g("moe_b1").reshape(L, NEXP * 12, 128), "moe_w2": g("moe_w2"), "moe_b2": g("moe_b2"),
        "final_norm": np.asarray(inputs["final_norm"], np.float32).reshape(16, 128),
        "cst": cst, "sel8": sel, "sel32": sel32, "rope_cos": rc, "rope_sin": rs,
    }
    return shared


def run(inputs, depth=4, ncores=NCORES, n_exp=NEXP, dbg_cat=None, dbg_out=False):
    nc, k = build(depth, n_exp, dbg_cat is not None, dbg_out)
    shared = prep_inputs(inputs, depth)
    x = np.asarray(inputs["x"], np.float32)
    c = np.asarray(inputs["c"], np.float32)
    in_maps = []
    for i in range(ncores):
        b = i % 4
        m = dict(shared)
        m["x"] = np.ascontiguousarray(x[b])
        m["c"] = np.ascontiguousarray(c[b].reshape(16, 128))
        if dbg_cat is not None:
            m["catT_in"] = dbg_cat
        in_maps.append(m)
    res = run_bass_kernel_spmd(nc, in_maps, core_ids=list(range(ncores)))
    if dbg_out:
        return [r for r in res.results]
    return [r["y"] for r in res.results]


LAYERS_PER_LAUNCH = 4


def kernel(**inputs):
    depth = 4
    lpl = LAYERS_PER_LAUNCH
    nl = depth // lpl
    nc, k = build(lpl, NEXP, chain=(nl > 1))
    x = np.asarray(inputs["x"], np.float32)
    c = np.asarray(inputs["c"], np.float32)
    xs = [np.ascontiguousarray(x[b]) for b in range(4)]
    outs = None
    for i in range(nl):
        shared = prep_inputs(inputs, lpl, l0=i * lpl)
        in_maps = []
        for b in range(NCORES):
            m = dict(shared)
            m["x"] = xs[b % 4]
            m["c"] = np.ascontiguousarray(c[b % 4].reshape(16, 128))
            in_maps.append(m)
        res = run_bass_kernel_spmd(nc, in_maps, core_ids=list(range(NCORES)))
        outs = res.results
        if nl > 1:
            xs = [np.ascontiguousarray(np.asarray(outs[b]["xo"], np.float32)) for b in range(4)]
    return np.stack([np.asarray(outs[b]["y"], np.float32) for b in range(4)], 0)
```

```python
from contextlib import ExitStack
import os
import numpy as np
import concourse.bass as bass
import concourse.mybir as mybir
from concourse.bass_utils import run_bass_kernel_spmd

F32 = mybir.dt.float32
BF16 = mybir.dt.bfloat16
AF = mybir.ActivationFunctionType
ALU = mybir.AluOpType

SEM_LIMIT = 30000
NOARENA = not bool(os.environ.get("USE_ARENA"))
NDMA_SLOTS = 8

D = 2048
S = 4096
NKC = 16
TB = 512
NTB = S // TB
EPS = 1e-6
NEXP = 32
DEXP = 768
NCORES = 4


class _Res:
    __slots__ = ("lw", "rd")

    def __init__(self):
        self.lw = None
        self.rd = {}


class Prog:
    ENGS = ("pe", "act", "dve", "pool", "sp")

    def __init__(self, nc, stack):
        self.nc = nc
        self.stack = stack
        self.streams = {e: [] for e in self.ENGS}
        self.seen = {e: {} for e in self.ENGS}
        self.clk = {}
        self.res = {}
        self.nsem = 0
        self.ninstr = 0
        self.pending = {}
        self.allclk = []
        for e in ("pe", "act", "dve", "pool"):
            self.clk[e] = self._newclk(e)
        self.dslots = {q: [self._newclk(f"d{q}{i}") for i in range(NDMA_SLOTS)] for q in ("sp", "pool", "act")}
        self.dnext = {q: 0 for q in ("sp", "pool", "act")}

    def _newclk(self, name):
        self.nsem += 1
        c = [self.stack.enter_context(self.nc.semaphore(f"{name}_{self.nsem}")), 0]
        self.allclk.append(c)
        return c

    def _r(self, key):
        r = self.res.get(key)
        if r is None:
            r = self.res[key] = _Res()
        return r

    def _deps(self, eng, reads, writes, acc, is_dma):
        need = {}

        def add(d, kind):
            if d is None:
                return
            clk, tick, deng = d
            if (not is_dma) and deng == eng and kind != "raw":
                return
            if self.seen[eng].get(clk, 0) >= tick:
                return
            if need.get(clk, 0) < tick:
                need[clk] = tick

        for k in reads:
            add(self._r(k).lw, "raw")
        for k in writes:
            r = self._r(k)
            if not (acc and r.lw is not None and r.lw[2] == eng):
                add(r.lw, "waw")
            for d in r.rd.values():
                add(d, "war")
        return need

    def _commit(self, reads, writes, done):
        for k in reads:
            self._r(k).rd[done[0]] = done
        for k in writes:
            r = self._r(k)
            r.lw = done
            r.rd = {}

    def _emit_waits(self, eng, need):
        for clk, tick in need.items():
            self.seen[eng][clk] = tick
            self.streams[eng].append(("w", clk, tick))

    def op(self, eng, fn, reads=(), writes=(), inc=True, acc=False):
        need = self._deps(eng, reads, writes, acc, False)
        self._emit_waits(eng, need)
        c = self.clk[eng]
        if c[1] >= SEM_LIMIT and not self.pending.get(eng, False):
            c = self.clk[eng] = self._newclk(eng)
        self.pending[eng] = not inc
        if inc:
            c[1] += 1
            tick = c[1]
        else:
            tick = c[1] + 1
        done = (c[0], tick, eng)
        self.streams[eng].append(("i", fn, c[0] if inc else None))
        self._commit(reads, writes, done)
        self.ninstr += 1
        return done

    def dma(self, q, fn, reads=(), writes=()):
        need = self._deps(q, reads, writes, False, True)
        i = self.dnext[q]
        self.dnext[q] = (i + 1) % NDMA_SLOTS
        slot = self.dslots[q][i]
        if slot[1] >= SEM_LIMIT:
            slot = self.dslots[q][i] = self._newclk(f"d{q}{i}")
        if slot[1] > 0 and self.seen[q].get(slot[0], 0) < slot[1]:
            if need.get(slot[0], 0) < slot[1]:
                need[slot[0]] = slot[1]
        self._emit_waits(q, need)
        slot[1] += 16
        done = (slot[0], slot[1], "dma")
        self.streams[q].append(("d", fn, slot[0]))
        self._commit(reads, writes, done)
        self.ninstr += 1
        return done

    def barrier(self):
        for e in self.ENGS:
            need = {}
            for c in self.allclk:
                if c[1] > 0 and self.seen[e].get(c[0], 0) < c[1]:
                    need[c[0]] = c[1]
            self._emit_waits(e, need)
        self.res = {}

    def finish(self):
        nc = self.nc
        engmap = {"pe": "tensor", "act": "scalar", "dve": "vector", "pool": "gpsimd", "sp": "sync"}
        with nc.Block() as block:
            for e in self.ENGS:
                items = self.streams[e]

                def body(engobj, items=items):
                    for it in items:
                        if it[0] == "w":
                            engobj.wait_ge(it[1], it[2])
                        elif it[0] == "i":
                            ins = it[1](engobj)
                            if it[2] is not None:
                                ins.then_inc(it[2], 1)
                        else:
                            it[1](engobj).then_inc(it[2], 16)

                getattr(block, engmap[e])(body)


IN_A, IN_B, IN_C, IN_D = 0, 832, 2376, 3912
FT_CH = {}
_c = 0
for _nm, _n in (("qlat", 4), ("kvlat", 2), ("kr", 1), ("krs", 1), ("z", 4), ("xs", 4), ("bm", 2), ("cm", 2),
                ("qc", 4), ("kc", 4), ("qd", 4), ("kd", 4)):
    FT_CH[_nm] = _c
    _c += _n
NFT = _c


class K:
    pass


def build(depth, n_exp=NEXP, dbg_cat=False, dbg_out=False, chain=False):
    nc = bass.Bass("TRN2", target_bir_lowering=False)
    k = K()
    k.nc = nc

    def din(name, shape, dt=F32):
        return nc.dram_tensor(name, list(shape), dt, kind="ExternalInput").ap()

    def dscr(name, shape, dt):
        return nc.dram_tensor(name, list(shape), dt, kind="Internal").ap()

    L = depth
    x_in = din("x", [S, D])
    c_in = din("c", [16, 128])
    attn_norm = din("attn_norm", [L, 16, 128])
    ffn_norm = din("ffn_norm", [L, 16, 128])
    mod_w = din("mod_w", [L, D, 6 * D])
    mod_b = din("mod_b", [L, 96, 128])
    w_in = din("w_in", [L, D, 5448])
    mla_q_norm = din("mla_q_norm", [L, 4, 128])
    wq = din("mla_w_q_up", [L, 512, 768])
    mla_kv_norm = din("mla_kv_norm", [L, 2, 128])
    wkv = din("mla_w_kv_up", [L, 256, 1024])
    conv_w = din("ssm_conv_w", [L, 4 * 8, 128])
    conv_b = din("ssm_conv_b", [L, 8, 128])
    dt_bias = din("ssm_dt_bias", [L, 8, 1])
    a_log = din("ssm_a_log", [L, 8, 1])
    ssm_d = din("ssm_d", [L, 8])
    ssm_norm = din("ssm_norm", [L, 4, 128])
    ssm_norm8 = din("ssm_norm8", [L, 8, 64])
    ca_bias = din("ca_biasT", [L, 4, 5, 128, 128])
    mix_norm = din("mix_out_norm", [L, 12, 128])
    w_out = din("w_out", [L, D, D])
    router_w = din("router_w", [L, D, NEXP])
    router_b = din("router_b", [L, 1, NEXP])
    w1 = din("moe_w1", [L, NEXP, D, 2 * DEXP])
    b1 = din("moe_b1", [L, NEXP * 12, 128])
    w2 = din("moe_w2", [L, NEXP, DEXP, D])
    b2 = din("moe_b2", [L, NEXP, D])
    final_norm = din("final_norm", [16, 128])
    cst = din("cst", [128, 8, 128])
    sel8 = din("sel8", [8, 8 * 128])
    sel32 = din("sel32", [32, 32 * 128])
    rope_cos = din("rope_cos", [128, S])
    rope_sin = din("rope_sin", [128, S])
    y_out = nc.dram_tensor("y", [S, D], F32, kind="ExternalOutput").ap()
    xo_out = nc.dram_tensor("xo", [S, D], F32, kind="ExternalOutput").ap() if chain else None

    xT = dscr("xT", [D, S], F32)
    FT = dscr("FT", [NFT * 128, S], BF16)
    DTR = dscr("DTR", [8, S], F32)
    VCs = dscr("VC", [S, 512], BF16)
    VDs = dscr("VD", [S, 512], BF16)
    catT = din("catT_in", [D, S], BF16) if dbg_cat else dscr("catT", [D, S], BF16)
    k.dbg_cat = dbg_cat
    k.dbg_out = dbg_out
    cat_dump = nc.dram_tensor("cat_dump", [D, S], BF16, kind="ExternalOutput").ap() if (dbg_out and not dbg_cat) else None
    h2T = dscr("h2T", [D, S], BF16)
    GTs = dscr("GT", [NEXP, S], F32)
    FT2 = dscr("FT2", [11 * 128, S], BF16)
    VMs = dscr("VM", [S, 512], BF16)
    XSs = dscr("XSs", [S, 512], BF16)
    CSs = dscr("CSs", [8, S], F32)
    CSCs = dscr("CSCs", [128, 256], F32)
    DTCs = dscr("DTCs", [128, 256], F32)

    with ExitStack() as gst:
        P = Prog(nc, gst)

        ARENA_BYTES = 196608
        ARENA = None if NOARENA else gst.enter_context(nc.sbuf_tensor("ARENA", [128, ARENA_BYTES // 2], BF16))
        aoff = [0]

        class Tile:
            def __init__(self, ap, name, shape):
                self.ap, self.name, self.shape = ap, name, tuple(shape)

            def __getitem__(self, key):
                return self.ap[key]

        sbcnt = [0]

        def sb(st, name, shape, dt):
            if NOARENA:
                sbcnt[0] += 1
                return Tile(st.enter_context(nc.sbuf_tensor(f"{name}__u{sbcnt[0]}", list(shape), dt)), name, shape)
            esz = 4 if dt == F32 else 2
            nel = 1
            for d_ in shape[1:]:
                nel *= d_
            nbytes = (nel * esz + 63) // 64 * 64
            off = aoff[0]
            assert off + nbytes <= ARENA_BYTES, (name, off, nbytes)
            aoff[0] = off + nbytes
            if st is not gst:
                st.callback(lambda off=off: aoff.__setitem__(0, off))
            v = ARENA[0:shape[0], off // 2:off // 2 + nel * esz // 2]
            if dt == F32:
                v = v.bitcast(F32)
            if len(shape) == 3:
                v = v.rearrange("p (a b) -> p a b", a=shape[1])
            return Tile(v, name, shape)

        PS = [gst.enter_context(nc.psum_tensor(f"ps{i}", [128, 512], F32)) for i in range(8)]
        CF = sb(gst, "CF", [128, 8, 128], F32)
        CB = sb(gst, "CB", [128, 8, 128], BF16)
        SEL = sb(gst, "SEL", [8, 8 * 128], F32)
        P.dma("sp", lambda e: e.dma_start(out=CF[:], in_=cst[:, :, :]), writes=["CF"])
        P.dma("sp", lambda e: e.dma_start(out=SEL[:], in_=sel8[:, :]), writes=["SEL"])
        P.op("dve", lambda e: e.tensor_copy(out=CB[:], in_=CF[:]), reads=["CF"], writes=["CB"])
        IDF, ONESF = CF[:, 0, :], CF[:, 1, :]
        IDB, ONESB, TRIB, ZB = CB[:, 0, :], CB[:, 1, :], CB[:, 5, :], CB[:, 6, :]
        M_LT, M_LE, M_MLA = CF[:, 2, :], CF[:, 3, :], CF[:, 4, :]
        COLS = sb(gst, "COLS", [128, 400], F32)
        colmap = {}
        _o = 0
        for nm, n in (("an", 16), ("fn", 16), ("modb", 96), ("mod", 96), ("A1", 16), ("A2", 16), ("qn", 4), ("kvn", 2),
                      ("cw", 32), ("cb", 8), ("sn", 4), ("mn", 12), ("cond", 16), ("fin", 16), ("b1", 12)):
            colmap[nm] = (_o, n)
            _o += n
        assert _o <= 400

        def col(nm, j=0, n=1):
            o = colmap[nm][0] + j
            return COLS[:, o:o + n]

        cnt = [0]

        def uid(s):
            cnt[0] += 1
            return f"{s}{cnt[0]}"

        def colize(st, src_ap, n, nm, j0=0):
            t = sb(st, uid("cz"), [128, 128], F32)
            key = uid("czk")
            P.dma("sp", lambda e: e.dma_start(out=t[:n, :], in_=src_ap), writes=[key])
            P.op("pe", lambda e: e.matmul(PS[7][:, :n], lhsT=t[:n, :], rhs=IDF[:n, :n], start=True, stop=True),
                 reads=[key, "CF"], writes=["ps7"])
            P.op("dve", lambda e: e.tensor_copy(out=col(nm, j0, n), in_=PS[7][:, :n]), reads=["ps7"], writes=["COLS"])

        def rstd_from_ps(ps_t, out_t, dim, tmp_t, p=128):
            P.op("dve", lambda e: e.tensor_scalar(out=tmp_t[:p, :], in0=ps_t[:p, :], scalar1=1.0 / dim, scalar2=EPS, op0=ALU.mult, op1=ALU.add),
                 reads=[nm_(ps_t)], writes=[nm_(tmp_t)])
            P.op("act", lambda e: e.activation(out=tmp_t[:p, :], in_=tmp_t[:p, :], func=AF.Sqrt), reads=[nm_(tmp_t)], writes=[nm_(tmp_t)])
            P.op("dve", lambda e: e.reciprocal(out=out_t[:p, :], in_=tmp_t[:p, :]), reads=[nm_(tmp_t)], writes=[nm_(out_t)])

        def nm_(t):
            if isinstance(t, str):
                return t
            if isinstance(t, Tile):
                return t.name
            n_ = t.tensor.name if hasattr(t, "tensor") else t.name
            assert n_ != "ARENA", "arena AP passed to nm_"
            return n_

        with ExitStack() as st:
            XI = [sb(st, f"XI{i}", [128, D], F32) for i in range(2)]
            XO = [sb(st, f"XO{i}", [128, 4, 128], F32) for i in range(2)]
            n = 0
            for tt in range(S // 128):
                xi = XI[tt % 2]
                P.dma("sp", lambda e, xi=xi, tt=tt: e.dma_start(out=xi[:], in_=x_in[tt * 128:(tt + 1) * 128, :]), writes=[nm_(xi)])
                for g in range(4):
                    ps = PS[n % 4]
                    xo = XO[n % 2]
                    for j in range(4):
                        kc = g * 4 + j
                        P.op("pe", lambda e, ps=ps, xi=xi, j=j, kc=kc: e.matmul(ps[:, j * 128:(j + 1) * 128], lhsT=xi[:, kc * 128:(kc + 1) * 128], rhs=IDF, start=True, stop=True),
                             reads=[nm_(xi), "CF"], writes=[nm_(ps)], inc=(j == 3))
                    P.op("act", lambda e, ps=ps, xo=xo: e.activation(out=xo[:].rearrange("p a b -> p (a b)"), in_=ps[:, :], func=AF.Copy), reads=[nm_(ps)], writes=[nm_(xo)])
                    P.dma("sp", lambda e, xo=xo, g=g, tt=tt: e.dma_start(
                        out=xT[g * 512:(g + 1) * 512, tt * 128:(tt + 1) * 128].rearrange("(a p) t -> p a t", p=128), in_=xo[:]),
                        reads=[nm_(xo)], writes=["xT"])
                    n += 1
            colize(st, c_in[:, :], 16, "cond")
            P.op("act", lambda e: e.activation(out=col("cond", 0, 16), in_=col("cond", 0, 16), func=AF.Silu), reads=["COLS"], writes=["COLS"])
            colize(st, final_norm[:, :], 16, "fin")
        P.barrier()

        def load_xT_blk(XT, tb, src=xT, q="sp"):
            P.dma(q, lambda e: e.dma_start(out=XT[:], in_=src[:, tb * TB:(tb + 1) * TB].rearrange("(kc p) t -> p kc t", p=128)),
                  reads=[nm_(src)], writes=[nm_(XT)])

        def norm_mod(st_tiles, XT, HT, Acol, Scol, tb, HF=None):
            SQ, tmp, rstd, t2 = st_tiles
            P.op("act", lambda e: e.activation(out=SQ[:].rearrange("p a b -> p (a b)"), in_=XT[:].rearrange("p a b -> p (a b)"), func=AF.Square),
                 reads=[nm_(XT)], writes=[nm_(SQ)])
            for kc in range(NKC):
                P.op("pe", lambda e, kc=kc: e.matmul(PS[6][:, :], lhsT=ONESB, rhs=SQ[:, kc, :], start=(kc == 0), stop=(kc == NKC - 1)),
                     reads=[nm_(SQ), "CB"], writes=["ps6"], inc=(kc == NKC - 1), acc=(kc > 0))
            rstd_from_ps(PS[6], rstd, D, tmp)
            for kc in range(NKC):
                P.op("dve", lambda e, kc=kc: e.scalar_tensor_tensor(out=t2[:], in0=XT[:, kc, :], scalar=Acol(kc), in1=rstd[:], op0=ALU.mult, op1=ALU.mult),
                     reads=[nm_(XT), nm_(rstd), "COLS"], writes=[nm_(t2)])
                if HF is None:
                    P.op("dve", lambda e, kc=kc: e.tensor_scalar(out=HT[:, kc, :], in0=t2[:], scalar1=Scol(kc), scalar2=None, op0=ALU.add),
                         reads=[nm_(t2), "COLS"], writes=[nm_(HT)])
                else:
                    P.op("dve", lambda e, kc=kc: e.tensor_scalar(out=HF[:, kc, :], in0=t2[:], scalar1=Scol(kc), scalar2=None, op0=ALU.add),
                         reads=[nm_(t2), "COLS"], writes=[nm_(HF)])
                    P.op("act", lambda e, kc=kc: e.activation(out=HT[:, kc, :], in_=HF[:, kc, :], func=AF.Copy), reads=[nm_(HF)], writes=[nm_(HT)])

        def group_norm_store(OA, nch, gcolf, dst_rows, tb, SQs, tmp, rstd, stg, dim):
            P.op("act", lambda e: e.activation(out=SQs[:, :nch, :].rearrange("p a b -> p (a b)"), in_=OA[:, :nch, :].rearrange("p a b -> p (a b)"), func=AF.Square),
                 reads=[nm_(OA)], writes=[nm_(SQs)])
            for c in range(nch):
                P.op("pe", lambda e, c=c: e.matmul(PS[6][:, :], lhsT=ONESB, rhs=SQs[:, c, :], start=(c == 0), stop=(c == nch - 1)),
                     reads=[nm_(SQs), "CB"], writes=["ps6"], inc=(c == nch - 1), acc=(c > 0))
            rstd_from_ps(PS[6], rstd, dim, tmp)
            for c in range(nch):
                P.op("dve", lambda e, c=c: e.scalar_tensor_tensor(out=stg[:, c, :], in0=OA[:, c, :], scalar=gcolf(c), in1=rstd[:], op0=ALU.mult, op1=ALU.mult),
                     reads=[nm_(OA), nm_(rstd), "COLS"], writes=[nm_(stg)])
            P.dma("sp", lambda e: e.dma_start(out=catT[dst_rows:dst_rows + nch * 128, tb * TB:(tb + 1) * TB].rearrange("(a p) t -> p a t", p=128), in_=stg[:, :nch, :]),
                  reads=[nm_(stg)], writes=["catT"])

        benv = dict(locals())

        def do_layer(l):
            with ExitStack() as st:
                colize(st, attn_norm[l], 16, "an")
                colize(st, ffn_norm[l], 16, "fn")
                colize(st, mod_b[l], 96, "modb")
                colize(st, mla_q_norm[l], 4, "qn")
                colize(st, mla_kv_norm[l], 2, "kvn")
                colize(st, conv_w[l], 32, "cw")
                colize(st, conv_b[l], 8, "cb")
                colize(st, ssm_norm[l], 4, "sn")
                colize(st, mix_norm[l], 12, "mn")
                MW = [sb(st, f"MW{i}", [128, NKC, 512], F32) for i in range(2)]
                for nb in range(24):
                    mw = MW[nb % 2]
                    P.dma("sp" if nb % 2 == 0 else "act", lambda e, mw=mw, nb=nb: e.dma_start(out=mw[:], in_=mod_w[l][:, nb * 512:(nb + 1) * 512].rearrange("(kc p) n -> p kc n", p=128)),
                          writes=[nm_(mw)])
                    for j in range(4):
                        for kc in range(NKC):
                            P.op("pe", lambda e, mw=mw, j=j, kc=kc: e.matmul(PS[7][:, j:j + 1], lhsT=mw[:, kc, j * 128:(j + 1) * 128], rhs=col("cond", kc, 1), start=(kc == 0), stop=(kc == NKC - 1)),
                                 reads=[nm_(mw), "COLS"], writes=["ps7"], inc=(kc == NKC - 1), acc=(kc > 0))
                    P.op("dve", lambda e, nb=nb: e.tensor_tensor(out=col("mod", nb * 4, 4), in0=PS[7][:, 0:4], in1=col("modb", nb * 4, 4), op=ALU.add),
                         reads=["ps7", "COLS"], writes=["COLS"])
                P.op("dve", lambda e: e.scalar_tensor_tensor(out=col("A1", 0, 16), in0=col("mod", 16, 16), scalar=1.0, in1=col("an", 0, 16), op0=ALU.add, op1=ALU.mult),
                     reads=["COLS"], writes=["COLS"])
                P.op("dve", lambda e: e.scalar_tensor_tensor(out=col("A2", 0, 16), in0=col("mod", 64, 16), scalar=1.0, in1=col("fn", 0, 16), op0=ALU.add, op1=ALU.mult),
                     reads=["COLS"], writes=["COLS"])
            P.barrier()

            with ExitStack() as st:
                XT = sb(st, "XT", [128, NKC, TB], F32)
                HT = sb(st, "HT", [128, NKC, TB], BF16)
                SQ = sb(st, "SQ", [128, NKC, TB], BF16)
                tmp = sb(st, "tmp", [128, TB], F32)
                rstd = sb(st, "rstd", [128, TB], F32)
                t2 = sb(st, "t2", [128, TB], F32)
                WB = [sb(st, f"WB{i}", [128, NKC, 512], BF16) for i in range(2)]
                STG = [sb(st, f"STG{i}", [128, TB], BF16) for i in range(3)]
                STF = sb(st, "STF", [128, TB], F32)
                blocks = []
                blocks.append(([(IN_A, 512)], [("ft", FT_CH["qlat"] + i) for i in range(4)]))
                blocks.append(([(IN_A + 512, 256), (IN_A + 768, 64), (IN_A + 768, 64), (IN_A + 800, 32), (IN_A + 768, 32), (IN_A + 800, 32), (IN_A + 768, 32)],
                               [("ft", FT_CH["kvlat"]), ("ft", FT_CH["kvlat"] + 1), ("ft", FT_CH["kr"]), None]))
                blocks[-1] = (blocks[-1][0], [("ft", FT_CH["kvlat"]), ("ft", FT_CH["kvlat"] + 1), ("ft", FT_CH["kr"]), ("ft64", FT_CH["krs"])])
                blocks.append(([(IN_B, 512)], [("ft", FT_CH["z"] + i) for i in range(4)]))
                blocks.append(([(IN_B + 512, 512)], [("ft", FT_CH["xs"] + i) for i in range(4)]))
                blocks.append(([(IN_B + 1024, 512)], [("ft", FT_CH["bm"]), ("ft", FT_CH["bm"] + 1), ("ft", FT_CH["cm"]), ("ft", FT_CH["cm"] + 1)]))
                blocks.append(([(IN_B + 1536, 8)], [("dtr", 0)]))
                blocks.append(([(IN_C, 512)], [("ft", FT_CH["qc"] + i) for i in range(4)]))
                blocks.append(([(IN_C + 512, 512)], [("ft", FT_CH["kc"] + i) for i in range(4)]))
                blocks.append(([(IN_D, 512)], [("ft", FT_CH["qd"] + i) for i in range(4)]))
                blocks.append(([(IN_D + 512, 512)], [("ft", FT_CH["kd"] + i) for i in range(4)]))
                blocks.append(([(IN_C + 1024, 512)], [("v", VCs)]))
                blocks.append(([(IN_D + 1024, 512)], [("v", VDs)]))
                wl = w_in[l].rearrange("(kc p) n -> p kc n", p=128)
                nw = 0
                ne = 0
                for tb in range(NTB):
                    load_xT_blk(XT, tb)
                    norm_mod((SQ, tmp, rstd, t2), XT, HT, lambda kc: col("A1", kc), lambda kc: col("mod", kc), tb)
                    for segs, dests in blocks:
                        wb = WB[nw % 2]
                        nw += 1
                        o = 0
                        for (c0, n_) in segs:
                            P.dma("pool", lambda e, wb=wb, o=o, c0=c0, n_=n_: e.dma_start(out=wb[:, :, o:o + n_], in_=wl[:, :, c0:c0 + n_]), writes=[nm_(wb)])
                            o += n_
                        if dests[0][0] == "v":
                            for tt in range(4):
                                ps = PS[ne % 4]
                                stg = STG[ne % 3]
                                ne += 1
                                for kc in range(NKC):
                                    P.op("pe", lambda e, ps=ps, wb=wb, kc=kc, tt=tt: e.matmul(ps[:, :], lhsT=HT[:, kc, tt * 128:(tt + 1) * 128], rhs=wb[:, kc, :], start=(kc == 0), stop=(kc == NKC - 1)),
                                         reads=[nm_(wb), "HT"], writes=[nm_(ps)], inc=(kc == NKC - 1), acc=(kc > 0))
                                P.op("act", lambda e, ps=ps, stg=stg: e.activation(out=stg[:], in_=ps[:, :], func=AF.Copy), reads=[nm_(ps)], writes=[nm_(stg)])
                                dstv = dests[0][1]
                                r0 = tb * TB + tt * 128
                                P.dma("sp", lambda e, stg=stg, dstv=dstv, r0=r0: e.dma_start(out=dstv[r0:r0 + 128, :], in_=stg[:]), reads=[nm_(stg)], writes=[nm_(dstv)])
                            continue
                        for j, dst in enumerate(dests):
                            m = 128
                            if dst[0] == "dtr":
                                m = 8
                            ps = PS[ne % 4]
                            stg = STG[ne % 3]
                            ne += 1
                            for kc in range(NKC):
                                P.op("pe", lambda e, ps=ps, wb=wb, kc=kc, j=j, m=m: e.matmul(ps[:m, :], lhsT=wb[:, kc, j * 128:j * 128 + m], rhs=HT[:, kc, :], start=(kc == 0), stop=(kc == NKC - 1)),
                                     reads=[nm_(wb), "HT"], writes=[nm_(ps)], inc=(kc == NKC - 1), acc=(kc > 0))
                            if dst[0] == "dtr":
                                P.op("act", lambda e, ps=ps: e.activation(out=STF[:8, :], in_=ps[:8, :], func=AF.Copy), reads=[nm_(ps)], writes=["STF"])
                                P.dma("sp", lambda e, tb=tb: e.dma_start(out=DTR[:, tb * TB:(tb + 1) * TB], in_=STF[:8, :]), reads=["STF"], writes=["DTR"])
                            else:
                                P.op("act", lambda e, ps=ps, stg=stg: e.activation(out=stg[:], in_=ps[:, :], func=AF.Copy), reads=[nm_(ps)], writes=[nm_(stg)])
                                ch = dst[1]
                                if dst[0] == "ft64":
                                    pass
                                P.dma("sp", lambda e, stg=stg, ch=ch, tb=tb: e.dma_start(out=FT[ch * 128:(ch + 1) * 128, tb * TB:(tb + 1) * TB], in_=stg[:]), reads=[nm_(stg)], writes=["FT"])
            P.barrier()
            k.l = l
            if not dbg_cat:
                mixers(P, k, {**benv, **locals()})
            P.barrier()
            if cat_dump is not None and l == 0:
                P.dma("sp", lambda e: e.dma_start(out=cat_dump[:, :], in_=catT[:, :]), reads=["catT"], writes=["cat_dump"])
                P.barrier()
            tail(P, k, {**benv, **locals()}, n_exp)
            P.barrier()


        for l_ in range(L):
            do_layer(l_)

        if dbg_out:
            for nm2, src, shp, dt_ in (("d_h2T", h2T, [D, S], BF16), ("d_GT", GTs, [NEXP, S], F32), ("d_xT", xT, [D, S], F32)):
                dd = nc.dram_tensor(nm2, shp, dt_, kind="ExternalOutput").ap()
                P.dma("sp", lambda e, dd=dd, src=src: e.dma_start(out=dd[:, :], in_=src[:, :]), reads=[], writes=[nm2])
            P.barrier()
        final(P, k, locals())
        P.finish()
    k.P = P
    return nc, k


def mixers(P, k, env):
    nc = k.nc
    l = k.l
    PS, FT, col, IDF, ONESB, ONESF = env["PS"], env["FT"], env["col"], env["IDF"], env["ONESB"], env["ONESF"]
    IDB, TRIB, ZB, M_LT, M_LE, M_MLA = env["IDB"], env["TRIB"], env["ZB"], env["M_LT"], env["M_LE"], env["M_MLA"]
    sb, nm_, rstd_from_ps, group_norm_store = env["sb"], env["nm_"], env["rstd_from_ps"], env["group_norm_store"]
    wq, wkv, VCs, VDs, DTR, ca_bias, catT = env["wq"], env["wkv"], env["VCs"], env["VDs"], env["DTR"], env["ca_bias"], env["catT"]
    rope_cos, rope_sin, FT2, VMs, SEL = env["rope_cos"], env["rope_sin"], env["FT2"], env["VMs"], env["SEL"]
    dt_bias, a_log, ssm_d, CF = env["dt_bias"], env["a_log"], env["ssm_d"], env["CF"]

    def ftload(dst, h, ch, src=FT, q="sp"):
        P.dma(q, lambda e: e.dma_start(out=dst[:, h, :], in_=src[ch * 128:(ch + 1) * 128, :]), reads=[nm_(src)], writes=[nm_(dst)])

    def mm(out, lhsT, rhs, start, stop, reads, writes, inc=None):
        P.op("pe", lambda e: e.matmul(out, lhsT=lhsT, rhs=rhs, start=start, stop=stop), reads=reads, writes=writes, inc=(stop if inc is None else inc), acc=not start)

    def mla_prep():
      with ExitStack() as st:
        LT = sb(st, "aLT", [128, 6, TB], BF16)
        KRt = sb(st, "aKR", [128, 2, TB], BF16)
        COS = sb(st, "aCOS", [128, TB], F32)
        SIN = sb(st, "aSIN", [128, TB], F32)
        SQ = sb(st, "aSQ", [128, 6, TB], BF16)
        NRM = sb(st, "aNRM", [128, 6, TB], BF16)
        tmp = sb(st, "atmp", [128, TB], F32)
        rstd = sb(st, "arstd", [128, TB], F32)
        t1 = sb(st, "at1", [128, TB], F32)
        t2 = sb(st, "at2", [128, TB], F32)
        WQ = sb(st, "aWQ", [128, 4, 1280], BF16)
        WKV = sb(st, "aWKV", [128, 2, 1536], BF16)
        STG = [sb(st, f"aSTG{i}", [128, TB], BF16) for i in range(3)]
        wql = wq[l].rearrange("(kc p) n -> p kc n", p=128)
        wkl = wkv[l].rearrange("(kc p) n -> p kc n", p=128)
        P.dma("pool", lambda e: e.dma_start(out=WQ[:, :, 0:768], in_=wql[:, :, :]), writes=["aWQ"])
        P.dma("pool", lambda e: e.dma_start(out=WKV[:, :, 0:1024], in_=wkl[:, :, :]), writes=["aWKV"])
        for h in range(4):
            P.dma("pool", lambda e, h=h: e.dma_start(out=WQ[:, :, 768 + 64 * h:768 + 64 * h + 64], in_=wql[:, :, 192 * h + 128:192 * h + 192]), writes=["aWQ"])
            P.dma("pool", lambda e, h=h: e.dma_start(out=WQ[:, :, 1024 + 64 * h:1024 + 64 * h + 32], in_=wql[:, :, 192 * h + 160:192 * h + 192]), writes=["aWQ"])
            P.dma("pool", lambda e, h=h: e.dma_start(out=WQ[:, :, 1024 + 64 * h + 32:1024 + 64 * h + 64], in_=wql[:, :, 192 * h + 128:192 * h + 160]), writes=["aWQ"])
            P.dma("pool", lambda e, h=h: e.dma_start(out=WKV[:, :, 1024 + 128 * h:1024 + 128 * h + 128], in_=wkl[:, :, 256 * h + 128:256 * h + 256]), writes=["aWKV"])
        ne = 0
        for tb in range(NTB):
            sl = slice(tb * TB, (tb + 1) * TB)
            for c in range(6):
                P.dma("sp", lambda e, c=c, sl=sl: e.dma_start(out=LT[:, c, :], in_=FT[(FT_CH["qlat"] + c) * 128:(FT_CH["qlat"] + c + 1) * 128, sl]), reads=["FT"], writes=["aLT"])
            for c in range(2):
                P.dma("sp", lambda e, c=c, sl=sl: e.dma_start(out=KRt[:, c, :], in_=FT[(FT_CH["kr"] + c) * 128:(FT_CH["kr"] + c + 1) * 128, sl]), reads=["FT"], writes=["aKR"])
            P.dma("act", lambda e, sl=sl: e.dma_start(out=COS[:], in_=rope_cos[:, sl]), writes=["aCOS"])
            P.dma("act", lambda e, sl=sl: e.dma_start(out=SIN[:], in_=rope_sin[:, sl]), writes=["aSIN"])
            P.op("act", lambda e: e.activation(out=SQ[:].rearrange("p a b -> p (a b)"), in_=LT[:].rearrange("p a b -> p (a b)"), func=AF.Square), reads=["aLT"], writes=["aSQ"])
            for (c0, n, gname, dim) in ((0, 4, "qn", 512), (4, 2, "kvn", 256)):
                for c in range(n):
                    mm(PS[6][:, :], ONESB, SQ[:, c0 + c, :], c == 0, c == n - 1, ["aSQ", "CB"], ["ps6"])
                rstd_from_ps(PS[6], rstd, dim, tmp)
                for c in range(n):
                    P.op("dve", lambda e, c=c, c0=c0, gname=gname: e.scalar_tensor_tensor(out=NRM[:, c0 + c, :], in0=LT[:, c0 + c, :], scalar=col(gname, c), in1=rstd[:], op0=ALU.mult, op1=ALU.mult),
                         reads=["aLT", "arstd", "COLS"], writes=["aNRM"])
            for h in range(8):
                ps = PS[ne % 4]
                stg = STG[ne % 3]
                ne += 1
                if h < 4:
                    for kc in range(4):
                        mm(ps[:, :], WQ[:, kc, 192 * h:192 * h + 128], NRM[:, kc, :], kc == 0, kc == 3, ["aWQ", "aNRM"], [nm_(ps)])
                    row = h
                else:
                    hh = h - 4
                    for kc in range(2):
                        mm(ps[:, :], WKV[:, kc, 256 * hh:256 * hh + 128], NRM[:, 4 + kc, :], kc == 0, kc == 1, ["aWKV", "aNRM"], [nm_(ps)])
                    row = 6 + hh
                P.op("act", lambda e, ps=ps, stg=stg: e.activation(out=stg[:], in_=ps[:, :], func=AF.Copy), reads=[nm_(ps)], writes=[nm_(stg)])
                P.dma("sp", lambda e, stg=stg, row=row, sl=sl: e.dma_start(out=FT2[row * 128:(row + 1) * 128, sl], in_=stg[:]), reads=[nm_(stg)], writes=["FT2"])
            for ab in range(2):
                psa, pss = PS[4], PS[5]
                for kc in range(4):
                    mm(psa[:, :], WQ[:, kc, 768 + 128 * ab:768 + 128 * ab + 128], NRM[:, kc, :], kc == 0, kc == 3, ["aWQ", "aNRM"], ["ps4"])
                for kc in range(4):
                    mm(pss[:, :], WQ[:, kc, 1024 + 128 * ab:1024 + 128 * ab + 128], NRM[:, kc, :], kc == 0, kc == 3, ["aWQ", "aNRM"], ["ps5"])
                stg = STG[ne % 3]
                ne += 1
                P.op("dve", lambda e: e.tensor_tensor(out=t1[:], in0=psa[:, :], in1=COS[:], op=ALU.mult), reads=["ps4", "aCOS"], writes=["at1"])
                P.op("dve", lambda e: e.tensor_tensor(out=t2[:], in0=pss[:, :], in1=SIN[:], op=ALU.mult), reads=["ps5", "aSIN"], writes=["at2"])
                P.op("dve", lambda e, stg=stg: e.tensor_tensor(out=stg[:], in0=t1[:], in1=t2[:], op=ALU.add), reads=["at1", "at2"], writes=[nm_(stg)])
                P.dma("sp", lambda e, stg=stg, ab=ab, sl=sl: e.dma_start(out=FT2[(4 + ab) * 128:(5 + ab) * 128, sl], in_=stg[:]), reads=[nm_(stg)], writes=["FT2"])
            stg = STG[ne % 3]
            ne += 1
            P.op("dve", lambda e: e.tensor_tensor(out=t1[:], in0=KRt[:, 0, :], in1=COS[:], op=ALU.mult), reads=["aKR", "aCOS"], writes=["at1"])
            P.op("dve", lambda e: e.tensor_tensor(out=t2[:], in0=KRt[:, 1, :], in1=SIN[:], op=ALU.mult), reads=["aKR", "aSIN"], writes=["at2"])
            P.op("dve", lambda e, stg=stg: e.tensor_tensor(out=stg[:], in0=t1[:], in1=t2[:], op=ALU.add), reads=["at1", "at2"], writes=[nm_(stg)])
            P.dma("sp", lambda e, stg=stg, sl=sl: e.dma_start(out=FT2[10 * 128:11 * 128, sl], in_=stg[:]), reads=[nm_(stg)], writes=["FT2"])
            for tt in range(4):
                ps = PS[ne % 4]
                stg = STG[ne % 3]
                ne += 1
                for kc in range(2):
                    mm(ps[:, :], NRM[:, 4 + kc, tt * 128:(tt + 1) * 128], WKV[:, kc, 1024:1536], kc == 0, kc == 1, ["aWKV", "aNRM"], [nm_(ps)])
                P.op("act", lambda e, ps=ps, stg=stg: e.activation(out=stg[:], in_=ps[:, :], func=AF.Copy), reads=[nm_(ps)], writes=[nm_(stg)])
                r0 = tb * TB + tt * 128
                P.dma("sp", lambda e, stg=stg, r0=r0: e.dma_start(out=VMs[r0:r0 + 128, :], in_=stg[:]), reads=[nm_(stg)], writes=["VM"])
    mla_prep()
    P.barrier()

    def attn_phase(kind):
        with ExitStack() as st:
            Q = sb(st, "tQ", [128, 4, S], BF16)
            Kt = sb(st, "tK", [128, 4, S], BF16)
            V = sb(st, "tV", [128, 32, 512], BF16)
            OA = sb(st, "tOA", [128, 4, TB], F32)
            SQs = sb(st, "tSQs", [128, 4, TB], BF16)
            tmp = sb(st, "ttmp", [128, TB], F32)
            rstd = sb(st, "trstd", [128, TB], F32)
            stg = sb(st, "tstg", [128, 4, TB], BF16)
            ATT = [sb(st, f"tATT{i}", [128, TB], BF16) for i in range(3)]
            RD = sb(st, "tRD", [128, TB], F32)
            if kind == "mla":
                QR = sb(st, "tQR", [128, 2, S], BF16)
                KRD = sb(st, "tKRD", [128, 1, S], BF16)
                for h in range(4):
                    ftload(Q, h, h, FT2)
                    ftload(Kt, h, 6 + h, FT2, "act")
                ftload(QR, 0, 4, FT2)
                ftload(QR, 1, 5, FT2)
                ftload(KRD, 0, 10, FT2)
                vsrc, scale, mrow, dst = VMs, 192.0 ** -0.5, 0, 0
            elif kind == "sb":
                for h in range(4):
                    ftload(Q, h, FT_CH["qc"] + h)
                    ftload(Kt, h, FT_CH["kc"] + h, FT, "act")
                vsrc, scale, mrow, dst = VCs, 128.0 ** -0.5, 4, 1024
                CAC = sb(st, "tCAC", [128, TB], F32)
                E1 = sb(st, "tE1", [128, TB], F32)
                SP = sb(st, "tSP", [128, TB], F32)
                NLK = sb(st, "tNLK", [128, TB], BF16)
                T1 = sb(st, "tT1", [128, TB], F32)
            else:
                for h in range(4):
                    ftload(Q, h, FT_CH["qd"] + h)
                    ftload(Kt, h, FT_CH["kd"] + h, FT, "act")
                vsrc, scale, mrow, dst = VDs, 128.0 ** -0.5, 8, 1536
                BT = sb(st, "tBT", [128, 20, 128], F32)
                P.dma("sp", lambda e: e.dma_start(out=BT[:], in_=ca_bias[l].rearrange("h j k q -> k (h j) q")), writes=["tBT"])
                P.op("dve", lambda e: e.tensor_scalar(out=BT[:].rearrange("p a b -> p (a b)"), in0=BT[:].rearrange("p a b -> p (a b)"), scalar1=float(128.0 ** 0.5), scalar2=None, op0=ALU.mult), reads=["tBT"], writes=["tBT"])
            P.dma("sp", lambda e: e.dma_start(out=V[:], in_=vsrc.rearrange("(b p) c -> p b c", p=128)), reads=[nm_(vsrc)], writes=["tV"])
            n = 0
            for QB in range(NTB):
                for h in range(4):
                    if kind == "ca":
                        for qi in range(4):
                            qt = QB * 4 + qi
                            blks = [jb for jb in range(5) if qt - 4 + jb >= 0]
                            for idx, jb in enumerate(blks):
                                kb = qt - 4 + jb
                                ps = PS[n % 2]
                                att = ATT[n % 3]
                                n += 1
                                mm(ps[:, :128], Kt[:, h, kb * 128:(kb + 1) * 128], Q[:, h, qt * 128:(qt + 1) * 128], True, False, ["tK", "tQ"], [nm_(ps)])
                                mm(ps[:, :128], IDF, BT[:, h * 5 + jb, :], False, True, ["tBT", "CF"], [nm_(ps)])
                                P.op("act", lambda e, ps=ps, att=att: e.activation(out=att[:, :128], in_=ps[:, :128], func=AF.Exp, scale=float(scale)), reads=[nm_(ps)], writes=[nm_(att)])
                                last = idx == len(blks) - 1
                                mm(PS[2][:, qi * 128:(qi + 1) * 128], V[:, kb, h * 128:(h + 1) * 128], att[:, :128], idx == 0, last, ["tV", nm_(att)], ["ps2"], inc=False)
                                mm(PS[3][:, qi * 128:(qi + 1) * 128], ONESB, att[:, :128], idx == 0, last, ["CB", nm_(att)], ["ps3"], inc=True)
                    elif kind == "mla":
                        nkb = 4 * QB + 4
                        for kb in range(nkb):
                            j = kb - 4 * QB
                            q0 = 128 * max(j, 0)
                            nn = TB - q0
                            qs = slice(QB * TB + q0, (QB + 1) * TB)
                            ps = PS[n % 2]
                            att = ATT[n % 3]
                            n += 1
                            r0 = 64 * (h % 2)
                            mm(ps[:, :nn], Kt[:, h, kb * 128:(kb + 1) * 128], Q[:, h, qs], True, False, ["tK", "tQ"], [nm_(ps)])
                            mm(ps[:, :nn], KRD[r0:r0 + 64, 0, kb * 128:(kb + 1) * 128], QR[r0:r0 + 64, h // 2, qs], False, True, ["tKRD", "tQR"], [nm_(ps)])
                            P.op("act", lambda e, ps=ps, att=att, nn=nn: e.activation(out=att[:, :nn], in_=ps[:, :nn], func=AF.Exp, scale=float(scale)), reads=[nm_(ps)], writes=[nm_(att)])
                            if j >= 0:
                                P.op("dve", lambda e, att=att: e.tensor_tensor(out=att[:, :128], in0=att[:, :128], in1=M_MLA, op=ALU.mult), reads=[nm_(att), "CF"], writes=[nm_(att)])
                            last = kb == nkb - 1
                            mm(PS[2][:, q0:], V[:, kb, h * 128:(h + 1) * 128], att[:, :nn], kb == 0, last, ["tV", nm_(att)], ["ps2"], inc=False)
                            mm(PS[3][:, q0:], ONESB, att[:, :nn], kb == 0, last, ["CB", nm_(att)], ["ps3"], inc=True)
                    else:
                        P.op("pool", lambda e: e.memset(CAC[:], 0.0), writes=["tCAC"])
                        mm(PS[2][:, :], ZB, Q[:, 0, 0:TB], True, False, ["CB", "tQ"], ["ps2"], inc=True)
                        nkb = 4 * QB + 4
                        for kb in range(nkb - 1, -1, -1):
                            j = kb - 4 * QB
                            q0 = 128 * max(j, 0)
                            nn = TB - q0
                            qs = slice(QB * TB + q0, (QB + 1) * TB)
                            ps = PS[n % 2]
                            att = ATT[n % 3]
                            n += 1
                            mm(ps[:, :nn], Kt[:, h, kb * 128:(kb + 1) * 128], Q[:, h, qs], True, True, ["tK", "tQ"], [nm_(ps)])
                            P.op("act", lambda e, ps=ps, nn=nn: e.activation(out=E1[:, :nn], in_=ps[:, :nn], func=AF.Exp, scale=float(-scale)), reads=[nm_(ps)], writes=["tE1"])
                            P.op("act", lambda e, nn=nn: e.activation(out=SP[:, :nn], in_=E1[:, :nn], func=AF.Ln, bias=1.0), reads=["tE1"], writes=["tSP"])
                            P.op("dve", lambda e, ps=ps, nn=nn: e.scalar_tensor_tensor(out=NLK[:, :nn], in0=ps[:, :nn], scalar=float(scale), in1=SP[:, :nn], op0=ALU.mult, op1=ALU.add), reads=[nm_(ps), "tSP"], writes=["tNLK"])
                            if j >= 0:
                                P.op("dve", lambda e: e.tensor_tensor(out=NLK[:, :128], in0=NLK[:, :128], in1=M_LT, op=ALU.mult), reads=["tNLK", "CF"], writes=["tNLK"])
                            mm(PS[4][:, :nn], TRIB, NLK[:, :nn], True, True, ["CB", "tNLK"], ["ps4"])
                            mm(PS[5][:, :nn], ONESB, NLK[:, :nn], True, True, ["CB", "tNLK"], ["ps5"])
                            P.op("dve", lambda e, nn=nn: e.tensor_tensor(out=T1[:, :nn], in0=PS[4][:, :nn], in1=SP[:, :nn], op=ALU.add), reads=["ps4", "tSP"], writes=["tT1"])
                            P.op("dve", lambda e, nn=nn, q0=q0: e.tensor_tensor(out=T1[:, :nn], in0=T1[:, :nn], in1=CAC[:, q0:], op=ALU.add), reads=["tT1", "tCAC"], writes=["tT1"])
                            P.op("act", lambda e, att=att, nn=nn: e.activation(out=att[:, :nn], in_=T1[:, :nn], func=AF.Exp, scale=-1.0), reads=["tT1"], writes=[nm_(att)])
                            if j >= 0:
                                P.op("dve", lambda e, att=att: e.tensor_tensor(out=att[:, :128], in0=att[:, :128], in1=M_LT, op=ALU.mult), reads=[nm_(att), "CF"], writes=[nm_(att)])
                            P.op("dve", lambda e, nn=nn, q0=q0: e.tensor_tensor(out=CAC[:, q0:], in0=CAC[:, q0:], in1=PS[5][:, :nn], op=ALU.add), reads=["ps5", "tCAC"], writes=["tCAC"])
                            mm(PS[2][:, q0:], V[:, kb, h * 128:(h + 1) * 128], att[:, :nn], False, kb == 0, ["tV", nm_(att)], ["ps2"], inc=True)
                    if kind == "sb":
                        P.op("act", lambda e, h=h: e.activation(out=OA[:, h, :], in_=PS[2][:, :], func=AF.Copy), reads=["ps2"], writes=["tOA"])
                    else:
                        P.op("dve", lambda e: e.reciprocal(out=RD[:], in_=PS[3][:, :]), reads=["ps3"], writes=["tRD"])
                        P.op("dve", lambda e, h=h: e.tensor_tensor(out=OA[:, h, :], in0=PS[2][:, :], in1=RD[:], op=ALU.mult), reads=["ps2", "tRD"], writes=["tOA"])
                group_norm_store(OA, 4, lambda c: col("mn", mrow + c), dst, QB, SQs, tmp, rstd, stg, 512)
        P.barrier()

    attn_phase("mla")
    attn_phase("sb")
    attn_phase("ca")
    ssd_phase(P, k, env, mm, ftload)
    P.barrier()


def ssd_phase(P, k, env, mm, ftload):
    nc = k.nc
    l = k.l
    PS, FT, col, IDF, ONESB, IDB, M_LE = env["PS"], env["FT"], env["col"], env["IDF"], env["ONESB"], env["IDB"], env["M_LE"]
    sb, nm_, rstd_from_ps = env["sb"], env["nm_"], env["rstd_from_ps"]
    DTR, catT, SEL, dt_bias, a_log, ssm_d, ssm_norm8 = env["DTR"], env["catT"], env["SEL"], env["dt_bias"], env["a_log"], env["ssm_d"], env["ssm_norm8"]
    XSs, CSs, CSCs, DTCs = env["XSs"], env["CSs"], env["CSCs"], env["DTCs"]
    def ssd_a():
      with ExitStack() as st:
        XC = sb(st, "sXC", [128, 3 + S], BF16)
        ACC = sb(st, "sACC", [128, S], F32)
        XF = sb(st, "sXF", [128, S], BF16)
        T1 = sb(st, "sT1", [128, S], F32)
        T2 = sb(st, "sT2", [128, S], F32)
        T3 = sb(st, "sT3", [128, S], F32)
        XO = [sb(st, f"sXO{i}", [128, 512], BF16) for i in range(2)]
        SM = sb(st, "sSM", [8, 4], F32)
        CC = sb(st, "sCC", [128, 32, 8], F32)
        P.op("pool", lambda e: e.memset(XC[:, 0:3], 0.0), writes=["sXC"])
        for c in range(8):
            ch = FT_CH["xs"] + c
            P.dma("sp", lambda e, ch=ch: e.dma_start(out=XC[:, 3:3 + S], in_=FT[ch * 128:(ch + 1) * 128, :]), reads=["FT"], writes=["sXC"])
            P.op("dve", lambda e, c=c: e.tensor_scalar(out=ACC[:], in0=XC[:, 3:3 + S], scalar1=col("cw", 3 * 8 + c), scalar2=None, op0=ALU.mult), reads=["sXC", "COLS"], writes=["sACC"])
            for j in (2, 1, 0):
                P.op("dve", lambda e, c=c, j=j: e.scalar_tensor_tensor(out=ACC[:], in0=XC[:, j:j + S], scalar=col("cw", j * 8 + c), in1=ACC[:], op0=ALU.mult, op1=ALU.add), reads=["sXC", "sACC", "COLS"], writes=["sACC"])
            P.op("act", lambda e, c=c: e.activation(out=XF[:], in_=ACC[:], func=AF.Silu, bias=col("cb", c), scale=1.0), reads=["sACC", "COLS"], writes=["sXF"])
            if c >= 4:
                P.dma("sp", lambda e, ch=ch: e.dma_start(out=FT[ch * 128:(ch + 1) * 128, :], in_=XF[:]), reads=["sXF"], writes=["FT"])
            else:
                for blk in range(32):
                    ps = PS[blk % 4]
                    xo = XO[blk % 2]
                    mm(ps[:, :128], XF[:, blk * 128:(blk + 1) * 128], IDB, True, True, ["sXF", "CB"], [nm_(ps)])
                    P.op("act", lambda e, ps=ps, xo=xo: e.activation(out=xo[:, :128], in_=ps[:, :128], func=AF.Copy), reads=[nm_(ps)], writes=[nm_(xo)])
                    P.dma("sp", lambda e, xo=xo, blk=blk, c=c: e.dma_start(out=XSs[blk * 128:(blk + 1) * 128, c * 128:(c + 1) * 128], in_=xo[:, :128]), reads=[nm_(xo)], writes=["XSs"])
        P.dma("sp", lambda e: e.dma_start(out=T1[:8, :], in_=DTR[:, :]), reads=["DTR"], writes=["sT1"])
        P.dma("sp", lambda e: e.dma_start(out=SM[:, 0:1], in_=dt_bias[l]), writes=["sSM"])
        P.dma("sp", lambda e: e.dma_start(out=SM[:, 1:2], in_=a_log[l]), writes=["sSM"])
        P.op("act", lambda e: e.activation(out=SM[:, 2:3], in_=SM[:, 1:2], func=AF.Exp), reads=["sSM"], writes=["sSM"])
        P.op("act", lambda e: e.activation(out=T1[:8, :], in_=T1[:8, :], func=AF.Exp, bias=SM[:, 0:1], scale=1.0), reads=["sT1", "sSM"], writes=["sT1"])
        P.op("act", lambda e: e.activation(out=T1[:8, :], in_=T1[:8, :], func=AF.Ln, bias=1.0), reads=["sT1"], writes=["sT1"])
        P.op("dve", lambda e: e.tensor_scalar(out=T2[:8, :], in0=T1[:8, :], scalar1=SM[:, 2:3], scalar2=-1.0, op0=ALU.mult, op1=ALU.mult), reads=["sT1", "sSM"], writes=["sT2"])
        P.op("pool", lambda e: e.memset(T3[:8, :], 1.0), writes=["sT3"])
        P.op("dve", lambda e: e.tensor_tensor_scan(out=ACC[:8, :], data0=T3[:8, :], data1=T2[:8, :], initial=0.0, op0=ALU.mult, op1=ALU.add), reads=["sT2", "sT3"], writes=["sACC"])
        P.dma("sp", lambda e: e.dma_start(out=CSs[:, :], in_=ACC[:8, :]), reads=["sACC"], writes=["CSs"])
        for (src, key, dstd) in ((ACC, "sACC", CSCs), (T1, "sT1", DTCs)):
            for blk in range(32):
                mm(PS[blk % 4][:, :8], src[:8, blk * 128:(blk + 1) * 128], IDF[:8, :8], True, True, [key, "CF"], [nm_(PS[blk % 4])])
                P.op("dve", lambda e, blk=blk: e.tensor_copy(out=CC[:, blk, :], in_=PS[blk % 4][:, :8]), reads=[nm_(PS[blk % 4])], writes=["sCC"])
            P.dma("sp", lambda e, dstd=dstd: e.dma_start(out=dstd[:, :], in_=CC[:].rearrange("p a b -> p (a b)")), reads=["sCC"], writes=[nm_(dstd)])
    ssd_a()
    P.barrier()
    def ssd_b():
      with ExitStack() as st:
        BTt = sb(st, "uB", [128, 2, S], BF16)
        CTt = sb(st, "uC", [128, 2, S], BF16)
        XS = sb(st, "uXS", [128, 32, 512], BF16)
        CSr = sb(st, "uCSr", [8, S], F32)
        CSC = sb(st, "uCSC", [128, 32, 8], F32)
        DTC = sb(st, "uDTC", [128, 32, 8], F32)
        CSB = sb(st, "uCSB", [128, 8, TB], F32)
        DIF = [sb(st, f"uDIF{i}", [128, TB], F32) for i in range(2)]
        W = [sb(st, f"uW{i}", [128, TB], BF16) for i in range(3)]
        ZT = sb(st, "uZT", [64, 8, TB], BF16)
        SZ = sb(st, "uSZ", [64, TB], F32)
        OAh = sb(st, "uOA", [64, 8, TB], F32)
        SQs = sb(st, "uSQs", [64, 8, TB], BF16)
        stg = sb(st, "ustg", [64, 8, TB], BF16)
        tmp = sb(st, "utmp", [128, TB], F32)
        rstd = sb(st, "urstd", [128, TB], F32)
        DSK = sb(st, "uDSK", [128, 8], F32)
        DI = sb(st, "uDI", [128, 8, 128], F32)
        G8 = sb(st, "uG8", [64, 8], F32)
        G8t = sb(st, "uG8t", [8, 64], F32)
        for g in range(2):
            ftload(BTt, g, FT_CH["bm"] + g)
            ftload(CTt, g, FT_CH["cm"] + g, FT, "act")
        P.dma("sp", lambda e: e.dma_start(out=XS[:], in_=XSs.rearrange("(b p) c -> p b c", p=128)), reads=["XSs"], writes=["uXS"])
        P.dma("sp", lambda e: e.dma_start(out=CSr[:], in_=CSs[:, :]), reads=["CSs"], writes=["uCSr"])
        P.dma("sp", lambda e: e.dma_start(out=CSC[:].rearrange("p a b -> p (a b)"), in_=CSCs[:, :]), reads=["CSCs"], writes=["uCSC"])
        P.dma("sp", lambda e: e.dma_start(out=DTC[:].rearrange("p a b -> p (a b)"), in_=DTCs[:, :]), reads=["DTCs"], writes=["uDTC"])
        P.dma("sp", lambda e: e.dma_start(out=DSK[:], in_=ssm_d[l:l + 1, :].partition_broadcast(128)), writes=["uDSK"])
        P.dma("sp", lambda e: e.dma_start(out=G8t[:], in_=ssm_norm8[l]), writes=["uG8t"])
        mm(PS[7][:64, :8], G8t[:, :], IDF[:8, :8], True, True, ["uG8t", "CF"], ["ps7"])
        P.op("dve", lambda e: e.tensor_copy(out=G8[:], in_=PS[7][:64, :8]), reads=["ps7"], writes=["uG8"])
        for h in range(8):
            P.op("dve", lambda e, h=h: e.tensor_scalar(out=DI[:, h, :], in0=IDF, scalar1=DSK[:, h:h + 1], scalar2=None, op0=ALU.mult), reads=["CF", "uDSK"], writes=["uDI"])
        n = 0
        for QB in range(NTB):
            sl = slice(QB * TB, (QB + 1) * TB)
            for h in range(8):
                mm(PS[7][:, :], SEL[:, h * 128:(h + 1) * 128], CSr[:, sl], True, True, ["SEL", "uCSr"], ["ps7"])
                P.op("act", lambda e, h=h: e.activation(out=CSB[:, h, :], in_=PS[7][:, :], func=AF.Copy), reads=["ps7"], writes=["uCSB"])
                P.dma("act", lambda e, h=h, sl=sl: e.dma_start(out=ZT[:, h, :], in_=FT[FT_CH["z"] * 128 + 64 * h:FT_CH["z"] * 128 + 64 * h + 64, sl]), reads=["FT"], writes=["uZT"])
            for g in range(2):
                nkb = 4 * QB + 4
                for kb in range(nkb):
                    j = kb - 4 * QB
                    q0 = 128 * max(j, 0)
                    nn = TB - q0
                    gp = PS[n % 2]
                    n += 1
                    mm(gp[:, :nn], BTt[:, g, kb * 128:(kb + 1) * 128], CTt[:, g, QB * TB + q0:(QB + 1) * TB], True, True, ["uB", "uC"], [nm_(gp)])
                    for r in range(4):
                        h = 4 * g + r
                        dif = DIF[(n + r) % 2]
                        w = W[(n + r) % 3]
                        if j >= 0:
                            P.op("dve", lambda e, dif=dif, h=h, kb=kb, q0=q0, nn=nn: e.tensor_scalar(out=dif[:, :nn], in0=CSB[:, h, q0:], scalar1=CSC[:, kb, h:h + 1], scalar2=0.0, op0=ALU.subtract, op1=ALU.min), reads=["uCSB", "uCSC"], writes=[nm_(dif)])
                        else:
                            P.op("dve", lambda e, dif=dif, h=h, kb=kb, q0=q0, nn=nn: e.tensor_scalar(out=dif[:, :nn], in0=CSB[:, h, q0:], scalar1=CSC[:, kb, h:h + 1], scalar2=None, op0=ALU.subtract), reads=["uCSB", "uCSC"], writes=[nm_(dif)])
                        P.op("act", lambda e, dif=dif, nn=nn: e.activation(out=dif[:, :nn], in_=dif[:, :nn], func=AF.Exp), reads=[nm_(dif)], writes=[nm_(dif)])
                        P.op("dve", lambda e, dif=dif, w=w, gp=gp, h=h, kb=kb, nn=nn: e.scalar_tensor_tensor(out=w[:, :nn], in0=dif[:, :nn], scalar=DTC[:, kb, h:h + 1], in1=gp[:, :nn], op0=ALU.mult, op1=ALU.mult), reads=[nm_(dif), nm_(gp), "uDTC"], writes=[nm_(w)])
                        if j >= 0:
                            P.op("dve", lambda e, w=w: e.tensor_tensor(out=w[:, :128], in0=w[:, :128], in1=M_LE, op=ALU.mult), reads=[nm_(w), "CF"], writes=[nm_(w)])
                            P.op("dve", lambda e, w=w, h=h: e.tensor_tensor(out=w[:, :128], in0=w[:, :128], in1=DI[:, h, :], op=ALU.add), reads=[nm_(w), "uDI"], writes=[nm_(w)])
                        mm(PS[2 + r][:64, q0:], XS[:, kb, h * 64:(h + 1) * 64], w[:, :nn], kb == 0, kb == nkb - 1, ["uXS", nm_(w)], [f"ps{2 + r}"], inc=True)
                for r in range(4):
                    h = 4 * g + r
                    P.op("act", lambda e, h=h: e.activation(out=SZ[:], in_=ZT[:, h, :], func=AF.Silu), reads=["uZT"], writes=["uSZ"])
                    P.op("dve", lambda e, h=h, r=r: e.tensor_tensor(out=OAh[:, h, :], in0=PS[2 + r][:64, :], in1=SZ[:], op=ALU.mult), reads=[f"ps{2 + r}", "uSZ"], writes=["uOA"])
                    P.op("act", lambda e, h=h: e.activation(out=SQs[:, h, :], in_=OAh[:, h, :], func=AF.Square), reads=["uOA"], writes=["uSQs"])
                for r in range(4):
                    mm(PS[6][:, :], ONESB[0:64, :], SQs[:, 4 * g + r, :], r == 0, r == 3, ["uSQs", "CB"], ["ps6"])
                rstd_from_ps(PS[6], rstd, 256, tmp)
                for r in range(4):
                    h = 4 * g + r
                    P.op("dve", lambda e, h=h: e.scalar_tensor_tensor(out=stg[:, h, :], in0=OAh[:, h, :], scalar=G8[:, h:h + 1], in1=rstd[:64, :], op0=ALU.mult, op1=ALU.mult), reads=["uOA", "urstd", "uG8"], writes=["ustg"])
            P.dma("sp", lambda e, sl=sl: e.dma_start(out=catT[512:1024, sl].rearrange("(h p) t -> p h t", p=64), in_=stg[:]), reads=["ustg"], writes=["catT"])
    ssd_b()


def tail(P, k, env, n_exp):
    nc = k.nc
    l = k.l
    PS, xT, col, IDF, ONESB, ONESF = env["PS"], env["xT"], env["col"], env["IDF"], env["ONESB"], env["ONESF"]
    sb, nm_, norm_mod, load_xT_blk = env["sb"], env["nm_"], env["norm_mod"], env["load_xT_blk"]
    catT, h2T, GTs, w_out, router_w, router_b = env["catT"], env["h2T"], env["GTs"], env["w_out"], env["router_w"], env["router_b"]
    w1, b1, w2, b2, sel32 = env["w1"], env["b1"], env["w2"], env["b2"], env["sel32"]
    def p3():
      with ExitStack() as st:
        CT = sb(st, "oCT", [128, NKC, TB], BF16)
        XT = sb(st, "oXT", [128, NKC, TB], F32)
        WO = [sb(st, f"oWO{i}", [128, NKC, 512], BF16) for i in range(2)]
        wl = w_out[l].rearrange("(kc p) n -> p kc n", p=128)
        nw = 0
        for tb in range(NTB):
            load_xT_blk(CT, tb, src=catT, q="act")
            load_xT_blk(XT, tb)
            for nb in range(4):
                wo = WO[nw % 2]
                nw += 1
                P.dma("pool", lambda e, wo=wo, nb=nb: e.dma_start(out=wo[:], in_=wl[:, :, nb * 512:(nb + 1) * 512]), writes=[nm_(wo)])
                for j in range(4):
                    n = nb * 4 + j
                    ps = PS[n % 4]
                    for kc in range(NKC):
                        P.op("pe", lambda e, ps=ps, wo=wo, kc=kc, j=j: e.matmul(ps[:, :], lhsT=wo[:, kc, j * 128:(j + 1) * 128], rhs=CT[:, kc, :], start=(kc == 0), stop=(kc == NKC - 1)),
                             reads=[nm_(wo), "oCT"], writes=[nm_(ps)], inc=(kc == NKC - 1), acc=(kc > 0))
                    P.op("dve", lambda e, ps=ps, n=n: e.scalar_tensor_tensor(out=XT[:, n, :], in0=ps[:, :], scalar=col("mod", 32 + n), in1=XT[:, n, :], op0=ALU.mult, op1=ALU.add),
                         reads=[nm_(ps), "oXT", "COLS"], writes=["oXT"])
            P.dma("sp", lambda e, tb=tb: e.dma_start(out=xT[:, tb * TB:(tb + 1) * TB].rearrange("(kc p) t -> p kc t", p=128), in_=XT[:]), reads=["oXT"], writes=["xT"])
    p3()
    P.barrier()
    import os
    if os.environ.get("SKIP_P4"):
        return
    if getattr(k, "dbg_out", False):
        dd0 = nc.dram_tensor("d_x1T", [D, S], F32, kind="ExternalOutput").ap()
        P.dma("sp", lambda e: e.dma_start(out=dd0[:, :], in_=xT[:, :]), reads=[], writes=["d_x1T"])
        P.barrier()
    def p4a():
      with ExitStack() as st:
        XT = sb(st, "rXT", [128, NKC, TB], F32)
        HF = sb(st, "rHF", [128, NKC, TB], F32)
        HT = sb(st, "rHT", [128, NKC, TB], BF16)
        SQ = sb(st, "rSQ", [128, NKC, TB], BF16)
        tmp = sb(st, "rtmp", [128, TB], F32)
        rstd = sb(st, "rrstd", [128, TB], F32)
        t2 = sb(st, "rt2", [128, TB], F32)
        RW = sb(st, "rRW", [128, NKC, NEXP], F32)
        RB = sb(st, "rRB", [1, NEXP], F32)
        LG = sb(st, "rLG", [128, NEXP], F32)
        M8 = sb(st, "rM8", [128, 8], F32)
        NM = sb(st, "rNM", [128, 1], F32)
        MK = sb(st, "rMK", [128, NEXP], F32)
        EE = sb(st, "rEE", [128, NEXP], F32)
        SM = sb(st, "rSM", [128, 1], F32)
        GG = sb(st, "rGG", [128, NEXP], F32)
        GTt = sb(st, "rGT", [32, TB], F32)
        RBB = sb(st, "rRBB", [128, NEXP], F32)
        if not os.environ.get("SKIP_RW"):
            P.dma("sp", lambda e: e.dma_start(out=RW[:], in_=router_w[l].rearrange("(kc p) n -> p kc n", p=128)), writes=["rRW"])
        if not os.environ.get("SKIP_RBB"):
            P.dma("sp", lambda e: e.dma_start(out=RBB[:], in_=router_b[l].partition_broadcast(128)), writes=["rRBB"])
        for tb in range(NTB):
            if not os.environ.get("SKIP_XTLOAD"):
                load_xT_blk(XT, tb)
            if not os.environ.get("SKIP_NORM2"):
                norm_mod((SQ, tmp, rstd, t2), XT, HT, lambda kc: col("A2", kc), lambda kc: col("mod", 48 + kc), tb, HF=(None if os.environ.get("NO_HF") else HF))
            if not os.environ.get("SKIP_H2T"):
                P.dma("sp", lambda e, tb=tb: e.dma_start(out=h2T[:, tb * TB:(tb + 1) * TB].rearrange("(kc p) t -> p kc t", p=128), in_=HT[:]), reads=["rHT"], writes=["h2T"])
            if os.environ.get("SKIP_ROUTER"):
                continue
            for tt in range(4):
                ps = PS[tt % 2]
                for kc in range(NKC):
                    P.op("pe", lambda e, ps=ps, kc=kc, tt=tt: e.matmul(ps[:, :NEXP], lhsT=HF[:, kc, tt * 128:(tt + 1) * 128], rhs=RW[:, kc, :], start=(kc == 0), stop=(kc == NKC - 1)),
                         reads=["rHF", "rRW"], writes=[nm_(ps)], inc=(kc == NKC - 1), acc=(kc > 0))
                P.op("dve", lambda e, ps=ps: e.tensor_tensor(out=LG[:], in0=ps[:, :NEXP], in1=RBB[:], op=ALU.add), reads=[nm_(ps), "rRBB"], writes=["rLG"])
                P.op("dve", lambda e: e.max(out=M8[:], in_=LG[:]), reads=["rLG"], writes=["rM8"])
                P.op("dve", lambda e: e.tensor_scalar(out=NM[:], in0=M8[:, 0:1], scalar1=-1.0, scalar2=None, op0=ALU.mult), reads=["rM8"], writes=["rNM"])
                P.op("dve", lambda e: e.tensor_scalar(out=MK[:], in0=LG[:], scalar1=M8[:, 3:4], scalar2=1e30, op0=ALU.subtract, op1=ALU.mult), reads=["rLG", "rM8"], writes=["rMK"])
                P.op("dve", lambda e: e.tensor_scalar(out=MK[:], in0=MK[:], scalar1=1.0, scalar2=0.0, op0=ALU.add, op1=ALU.max), reads=["rMK"], writes=["rMK"])
                P.op("dve", lambda e: e.tensor_scalar(out=MK[:], in0=MK[:], scalar1=1.0, scalar2=None, op0=ALU.min), reads=["rMK"], writes=["rMK"])
                P.op("act", lambda e: e.activation(out=EE[:], in_=LG[:], func=AF.Exp, bias=NM[:, 0:1], scale=1.0), reads=["rLG", "rNM"], writes=["rEE"])
                P.op("dve", lambda e: e.tensor_tensor(out=EE[:], in0=EE[:], in1=MK[:], op=ALU.mult), reads=["rEE", "rMK"], writes=["rEE"])
                P.op("dve", lambda e: e.tensor_reduce(out=SM[:], in_=EE[:], axis=mybir.AxisListType.X, op=ALU.add), reads=["rEE"], writes=["rSM"])
                P.op("dve", lambda e: e.tensor_scalar(out=SM[:], in0=SM[:], scalar1=1e-30, scalar2=None, op0=ALU.max), reads=["rSM"], writes=["rSM"])
                P.op("dve", lambda e: e.reciprocal(out=SM[:], in_=SM[:]), reads=["rSM"], writes=["rSM"])
                P.op("dve", lambda e: e.tensor_scalar(out=GG[:], in0=EE[:], scalar1=SM[:, 0:1], scalar2=None, op0=ALU.mult), reads=["rEE", "rSM"], writes=["rGG"])
                P.op("pe", lambda e, tt=tt: e.matmul(PS[2][:NEXP, tt * 128:(tt + 1) * 128], lhsT=GG[:], rhs=IDF, start=True, stop=True), reads=["rGG", "CF"], writes=["ps2"])
            P.op("act", lambda e: e.activation(out=GTt[:], in_=PS[2][:NEXP, :], func=AF.Copy), reads=["ps2"], writes=["rGT"])
            P.dma("sp", lambda e, tb=tb: e.dma_start(out=GTs[:, tb * TB:(tb + 1) * TB], in_=GTt[:]), reads=["rGT"], writes=["GT"])
        if getattr(k, "dbg_out", False):
            for nm2, t_, shp in (("d_LG", LG, [128, NEXP]), ("d_M8", M8, [128, 8]), ("d_GG", GG, [128, NEXP]), ("d_RW", RW, [128, NKC * NEXP]), ("d_RBB", RBB, [128, NEXP]), ("d_HF", HF, [128, NKC * TB]), ("d_GTt", GTt, [32, TB]), ("d_XT", XT, [128, NKC * TB]), ("d_rstd", rstd, [128, TB]), ("d_tmp", tmp, [128, TB])):
                dd = nc.dram_tensor(nm2, shp, F32, kind="ExternalOutput").ap()
                src = t_[:] if len(t_.shape) == 2 else t_[:].rearrange("p a b -> p (a b)")
                P.dma("sp", lambda e, dd=dd, src=src: e.dma_start(out=dd[:, :], in_=src), reads=[nm_(t_)], writes=[nm2])
    p4a()
    P.barrier()
    if os.environ.get("SKIP_P4B"):
        return
    def p4b():
      with ExitStack() as st:
        HT = sb(st, "mHT", [128, NKC, TB], BF16)
        YA = sb(st, "mYA", [128, NKC, TB], F32)
        XC = [sb(st, f"mXC{i}", [128, TB], F32) for i in range(2)]
        W1 = [sb(st, f"mW1{i}", [128, NKC, 384], BF16) for i in range(3)]
        W2s = [sb(st, f"mW2{i}", [128, 6, D], BF16) for i in range(2)]
        nw2 = [0]
        SGL = sb(st, "mSGL", [128, 6, TB], F32)
        AT = sb(st, "mAT", [128, 6, TB], BF16)
        GBC = sb(st, "mGBC", [128, TB], F32)
        G1 = sb(st, "mG1", [128, TB], F32)
        SG = sb(st, "mSG", [128, TB], F32)
        U1 = sb(st, "mU1", [128, TB], F32)
        GTt = sb(st, "mGT", [32, TB], F32)
        SEL = sb(st, "mSEL", [32, 32 * 128], F32)
        B2 = sb(st, "mB2", [32, D], F32)
        B1C = sb(st, "mB1C", [128, 384], F32)
        B1T = sb(st, "mB1T", [128, 128], F32)
        P.dma("sp", lambda e: e.dma_start(out=SEL[:], in_=sel32[:, :]), writes=["mSEL"])
        P.dma("sp", lambda e: e.dma_start(out=B2[:], in_=b2[l]), writes=["mB2"])
        for i in range(3):
            P.dma("sp", lambda e, i=i: e.dma_start(out=B1T[:], in_=b1[l][i * 128:(i + 1) * 128, :]), writes=["mB1T"])
            P.op("pe", lambda e: e.matmul(PS[7][:, :128], lhsT=B1T[:], rhs=IDF, start=True, stop=True), reads=["mB1T", "CF"], writes=["ps7"])
            P.op("dve", lambda e, i=i: e.tensor_copy(out=B1C[:, i * 128:(i + 1) * 128], in_=PS[7][:, :128]), reads=["ps7"], writes=["mB1C"])
        nw = 0
        npz = 0
        for tb in range(NTB):
            load_xT_blk(HT, tb, src=h2T)
            P.dma("sp", lambda e, tb=tb: e.dma_start(out=GTt[:], in_=GTs[:, tb * TB:(tb + 1) * TB]), reads=["GT"], writes=["mGT"])
            for n in range(NKC):
                ps = PS[npz % 4]
                npz += 1
                P.op("pe", lambda e, ps=ps, n=n: e.matmul(ps[:, :], lhsT=B2[:, n * 128:(n + 1) * 128], rhs=GTt[:], start=True, stop=True), reads=["mB2", "mGT"], writes=[nm_(ps)])
                P.op("act", lambda e, ps=ps, n=n: e.activation(out=YA[:, n, :], in_=ps[:, :], func=AF.Copy), reads=[nm_(ps)], writes=["mYA"])
            for ex in range(n_exp):
                P.op("pe", lambda e, ex=ex: e.matmul(PS[5][:, :], lhsT=SEL[:, ex * 128:(ex + 1) * 128], rhs=GTt[:], start=True, stop=True), reads=["mSEL", "mGT"], writes=["ps5"])
                P.op("act", lambda e: e.activation(out=GBC[:], in_=PS[5][:, :], func=AF.Copy), reads=["ps5"], writes=["mGBC"])
                W2 = W2s[nw2[0] % 2]
                nw2[0] += 1
                P.dma("pool", lambda e, ex=ex, W2=W2: e.dma_start(out=W2[:], in_=w2[l][ex].rearrange("(fc p) n -> p fc n", p=128)), writes=[nm_(W2)])
                w1l = w1[l][ex].rearrange("(kc p) n -> p kc n", p=128)
                for pc in range(4):
                    wt = W1[nw % 3]
                    nw += 1
                    P.dma("pool", lambda e, wt=wt, pc=pc, w1l=w1l: e.dma_start(out=wt[:], in_=w1l[:, :, pc * 384:(pc + 1) * 384]), writes=[nm_(wt)])
                    for f3 in range(3):
                        f = pc * 3 + f3
                        ps = PS[npz % 4]
                        npz += 1
                        for kc in range(NKC):
                            P.op("pe", lambda e, ps=ps, wt=wt, kc=kc, f3=f3: e.matmul(ps[:, :], lhsT=wt[:, kc, f3 * 128:(f3 + 1) * 128], rhs=HT[:, kc, :], start=(kc == 0), stop=(kc == NKC - 1)),
                                 reads=[nm_(wt), "mHT"], writes=[nm_(ps)], inc=(kc == NKC - 1), acc=(kc > 0))
                        bc = B1C[:, ex * 12 + f:ex * 12 + f + 1]
                        if f < 6:
                            P.op("dve", lambda e, ps=ps, bc=bc: e.tensor_scalar(out=G1[:], in0=ps[:, :], scalar1=bc, scalar2=7.0, op0=ALU.add, op1=ALU.min), reads=[nm_(ps), "mB1C"], writes=["mG1"])
                            P.op("act", lambda e: e.activation(out=SG[:], in_=G1[:], func=AF.Sigmoid, scale=1.702), reads=["mG1"], writes=["mSG"])
                            P.op("dve", lambda e, f=f: e.tensor_tensor(out=SGL[:, f, :], in0=G1[:], in1=SG[:], op=ALU.mult), reads=["mG1", "mSG"], writes=["mSGL"])
                        else:
                            j = f - 6
                            P.op("dve", lambda e, ps=ps, bc=bc: e.tensor_scalar(out=U1[:], in0=ps[:, :], scalar1=bc, scalar2=7.0, op0=ALU.add, op1=ALU.min), reads=[nm_(ps), "mB1C"], writes=["mU1"])
                            P.op("dve", lambda e: e.tensor_scalar(out=U1[:], in0=U1[:], scalar1=-7.0, scalar2=1.0, op0=ALU.max, op1=ALU.add), reads=["mU1"], writes=["mU1"])
                            P.op("dve", lambda e, j=j: e.tensor_tensor(out=U1[:], in0=U1[:], in1=SGL[:, j, :], op=ALU.mult), reads=["mU1", "mSGL"], writes=["mU1"])
                            P.op("dve", lambda e, j=j: e.tensor_tensor(out=AT[:, j, :], in0=U1[:], in1=GBC[:], op=ALU.mult), reads=["mU1", "mGBC"], writes=["mAT"])
                for n in range(NKC):
                    ps = PS[npz % 4]
                    npz += 1
                    for fc in range(6):
                        P.op("pe", lambda e, ps=ps, fc=fc, n=n, W2=W2: e.matmul(ps[:, :], lhsT=W2[:, fc, n * 128:(n + 1) * 128], rhs=AT[:, fc, :], start=(fc == 0), stop=(fc == 5)),
                             reads=[nm_(W2), "mAT"], writes=[nm_(ps)], inc=(fc == 5), acc=(fc > 0))
                    P.op("dve", lambda e, ps=ps, n=n: e.tensor_tensor(out=YA[:, n, :], in0=YA[:, n, :], in1=ps[:, :], op=ALU.add), reads=[nm_(ps), "mYA"], writes=["mYA"])
            for n in range(NKC):
                xc = XC[n % 2]
                P.dma("sp", lambda e, xc=xc, n=n, tb=tb: e.dma_start(out=xc[:], in_=xT[n * 128:(n + 1) * 128, tb * TB:(tb + 1) * TB]), reads=["xT"], writes=[nm_(xc)])
                P.op("dve", lambda e, xc=xc, n=n: e.scalar_tensor_tensor(out=xc[:], in0=YA[:, n, :], scalar=col("mod", 80 + n), in1=xc[:], op0=ALU.mult, op1=ALU.add),
                     reads=["mYA", nm_(xc), "COLS"], writes=[nm_(xc)])
                P.dma("sp", lambda e, xc=xc, n=n, tb=tb: e.dma_start(out=xT[n * 128:(n + 1) * 128, tb * TB:(tb + 1) * TB], in_=xc[:]), reads=[nm_(xc)], writes=["xT"])
    p4b()


def final(P, k, env):
    nc = k.nc
    PS, xT, y_out, col, IDF, ONESB = env["PS"], env["xT"], env["y_out"], env["col"], env["IDF"], env["ONESB"]
    sb, nm_, rstd_from_ps = env["sb"], env["nm_"], env["rstd_from_ps"]
    with ExitStack() as st:
        XT = sb(st, "fXT", [128, NKC, TB], F32)
        SQ = sb(st, "fSQ", [128, NKC, TB], BF16)
        tmp = sb(st, "ftmp", [128, TB], F32)
        rstd = sb(st, "frstd", [128, TB], F32)
        OT = [sb(st, f"fOT{i}", [128, D], F32) for i in range(2)]
        nn_ = [0]
        xo_out = env.get("xo_out")

        def emit_T(dst, dkey, tb):
            for tt in range(4):
                ot = OT[nn_[0] % 2]
                nn_[0] += 1
                for g in range(4):
                    ps = PS[g]
                    for j in range(4):
                        kc = g * 4 + j
                        P.op("pe", lambda e, ps=ps, j=j, kc=kc, tt=tt: e.matmul(ps[:, j * 128:(j + 1) * 128], lhsT=XT[:, kc, tt * 128:(tt + 1) * 128], rhs=IDF, start=True, stop=True),
                             reads=["fXT", "CF"], writes=[nm_(ps)], inc=(j == 3))
                    P.op("act", lambda e, ps=ps, ot=ot, g=g: e.activation(out=ot[:, g * 512:(g + 1) * 512], in_=ps[:, :], func=AF.Copy), reads=[nm_(ps)], writes=[nm_(ot)])
                r0 = tb * TB + tt * 128
                P.dma("sp", lambda e, ot=ot, r0=r0, dst=dst: e.dma_start(out=dst[r0:r0 + 128, :], in_=ot[:]), reads=[nm_(ot)], writes=[dkey])

        for tb in range(NTB):
            P.dma("sp", lambda e, tb=tb: e.dma_start(out=XT[:], in_=xT[:, tb * TB:(tb + 1) * TB].rearrange("(kc p) t -> p kc t", p=128)),
                  reads=["xT"], writes=["fXT"])
            if xo_out is not None:
                emit_T(xo_out, "xo", tb)
            P.op("act", lambda e: e.activation(out=SQ[:].rearrange("p a b -> p (a b)"), in_=XT[:].rearrange("p a b -> p (a b)"), func=AF.Square),
                 reads=["fXT"], writes=["fSQ"])
            for kc in range(NKC):
                P.op("pe", lambda e, kc=kc: e.matmul(PS[6][:, :], lhsT=ONESB, rhs=SQ[:, kc, :], start=(kc == 0), stop=(kc == NKC - 1)),
                     reads=["fSQ", "CB"], writes=["ps6"], inc=(kc == NKC - 1), acc=(kc > 0))
            rstd_from_ps(PS[6], rstd, D, tmp)
            for kc in range(NKC):
                P.op("dve", lambda e, kc=kc: e.scalar_tensor_tensor(out=XT[:, kc, :], in0=XT[:, kc, :], scalar=col("fin", kc), in1=rstd[:], op0=ALU.mult, op1=ALU.mult),
                     reads=["fXT", "frstd", "COLS"], writes=["fXT"])
            emit_T(y_out, "y", tb)
    P.barrier()


def _consts():
    p = np.arange(128)[:, None]
    f = np.arange(128)[None, :]
    c = np.zeros((128, 8, 128), np.float32)
    c[:, 0] = (p == f)
    c[:, 1] = 1.0
    c[:, 2] = (p < f)
    c[:, 3] = (p <= f)
    c[:, 4] = ((p // 64) <= (f // 64))
    c[:, 5] = (p > f)
    sel = np.zeros((8, 8 * 128), np.float32)
    for h in range(8):
        sel[h, h * 128:(h + 1) * 128] = 1.0
    sel32 = np.zeros((32, 32 * 128), np.float32)
    for h in range(32):
        sel32[h, h * 128:(h + 1) * 128] = 1.0
    inv = 1.0 / (10000.0 ** (np.arange(0, 64, 2, dtype=np.float32) / 64))
    ang = np.arange(S, dtype=np.float32)[:, None] * inv[None, :]
    cosT = np.cos(ang).T.astype(np.float32)
    sinT = np.sin(ang).T.astype(np.float32)
    rc = np.concatenate([cosT, cosT, cosT, cosT], 0)
    rs = np.concatenate([-sinT, sinT, -sinT, sinT], 0)
    return c, sel, np.ascontiguousarray(rc), np.ascontiguousarray(rs), sel32


def _ca_bias_tables(ca_rel_bias):
    L = ca_rel_bias.shape[0]
    kk = np.arange(640)[:, None]
    qq = np.arange(128)[None, :]
    dist = (512 + qq) - kk
    idx = np.clip(dist, -63, 256) + 63
    qch = (512 + qq) // 64
    kch = kk // 64
    valid = (kch <= qch) & (kch >= qch - 8)
    out = np.empty((L, 4, 640, 128), np.float32)
    for l in range(L):
        for h in range(4):
            t = ca_rel_bias[l, h][idx]
            out[l, h] = np.where(valid, t, np.float32(-30000.0))
    return np.ascontiguousarray(out.reshape(L, 4, 5, 128, 128))


_CACHE = {}


def prep_inputs(inputs, depth, l0=0):
    L = depth
    cst, sel, rc, rs, sel32 = _consts()
    g = lambda n: np.ascontiguousarray(np.asarray(inputs[n], np.float32)[l0:l0 + L])
    shared = {
        "attn_norm": g("attn_norm").reshape(L, 16, 128), "ffn_norm": g("ffn_norm").reshape(L, 16, 128),
        "mod_w": g("mod_w"), "mod_b": g("mod_b").reshape(L, 96, 128), "w_in": g("w_in"),
        "mla_q_norm": g("mla_q_norm").reshape(L, 4, 128), "mla_w_q_up": g("mla_w_q_up"),
        "mla_kv_norm": g("mla_kv_norm").reshape(L, 2, 128), "mla_w_kv_up": g("mla_w_kv_up"),
        "ssm_conv_w": g("ssm_conv_w").reshape(L, 32, 128), "ssm_conv_b": g("ssm_conv_b").reshape(L, 8, 128),
        "ssm_dt_bias": g("ssm_dt_bias").reshape(L, 8, 1), "ssm_a_log": g("ssm_a_log").reshape(L, 8, 1),
        "ssm_d": g("ssm_d"), "ssm_norm": g("ssm_norm").reshape(L, 4, 128), "ssm_norm8": g("ssm_norm").reshape(L, 8, 64),
        "ca_biasT": _ca_bias_tables(g("ca_rel_bias")), "mix_out_norm": g("mix_out_norm").reshape(L, 12, 128),
        "w_out": g("w_out"), "router_w": g("router_w"), "router_b": g("router_b").reshape(L, 1, NEXP),
        "moe_w1": g("moe_w1"), "moe_b1": g("moe_b1").reshape(L, NEXP * 12, 128), "moe_w2": g("moe_w2"), "moe_b2": g("moe_b2"),
        "final_norm": np.asarray(inputs["final_norm"], np.float32).reshape(16, 128),
        "cst": cst, "sel8": sel, "sel32": sel32, "rope_cos": rc, "rope_sin": rs,
    }
    return shared


def run(inputs, depth=4, ncores=NCORES, n_exp=NEXP, dbg_cat=None, dbg_out=False):
    nc, k = build(depth, n_exp, dbg_cat is not None, dbg_out)
    shared = prep_inputs(inputs, depth)
    x = np.asarray(inputs["x"], np.float32)
    c = np.asarray(inputs["c"], np.float32)
    in_maps = []
    for i in range(ncores):
        b = i % 4
        m = dict(shared)
        m["x"] = np.ascontiguousarray(x[b])
        m["c"] = np.ascontiguousarray(c[b].reshape(16, 128))
        if dbg_cat is not None:
            m["catT_in"] = dbg_cat
        in_maps.append(m)
    res = run_bass_kernel_spmd(nc, in_maps, core_ids=list(range(ncores)))
    if dbg_out:
        return [r for r in res.results]
    return [r["y"] for r in res.results]


LAYERS_PER_LAUNCH = 4


def kernel(**inputs):
    depth = 4
    lpl = LAYERS_PER_LAUNCH
    nl = depth // lpl
    nc, k = build(lpl, NEXP, chain=(nl > 1))
    x = np.asarray(inputs["x"], np.float32)
    c = np.asarray(inputs["c"], np.float32)
    xs = [np.ascontiguousarray(x[b]) for b in range(4)]
    outs = None
    for i in range(nl):
        shared = prep_inputs(inputs, lpl, l0=i * lpl)
        in_maps = []
        for b in range(NCORES):
            m = dict(shared)
            m["x"] = xs[b % 4]
            m["c"] = np.ascontiguousarray(c[b % 4].reshape(16, 128))
            in_maps.append(m)
        res = run_bass_kernel_spmd(nc, in_maps, core_ids=list(range(NCORES)))
        outs = res.results
        if nl > 1:
            xs = [np.ascontiguousarray(np.asarray(outs[b]["xo"], np.float32)) for b in range(4)]
    return np.stack([np.asarray(outs[b]["y"], np.float32) for b in range(4)], 0)
```

```python
from contextlib import ExitStack
import os
import numpy as np
import concourse.bass as bass
import concourse.mybir as mybir
from concourse.bass_utils import run_bass_kernel_spmd

F32 = mybir.dt.float32
BF16 = mybir.dt.bfloat16
AF = mybir.ActivationFunctionType
ALU = mybir.AluOpType

SEM_LIMIT = 30000
NOARENA = not bool(os.environ.get("USE_ARENA"))
NDMA_SLOTS = 8

D = 2048
S = 4096
NKC = 16
TB = 512
NTB = S // TB
EPS = 1e-6
NEXP = 32
DEXP = 768
NCORES = 4


class _Res:
    __slots__ = ("lw", "rd")

    def __init__(self):
        self.lw = None
        self.rd = {}


class Prog:
    ENGS = ("pe", "act", "dve", "pool", "sp")

    def __init__(self, nc, stack):
        self.nc = nc
        self.stack = stack
        self.streams = {e: [] for e in self.ENGS}
        self.seen = {e: {} for e in self.ENGS}
        self.clk = {}
        self.res = {}
        self.nsem = 0
        self.ninstr = 0
        self.pending = {}
        self.allclk = []
        for e in ("pe", "act", "dve", "pool"):
            self.clk[e] = self._newclk(e)
        self.dslots = {q: [self._newclk(f"d{q}{i}") for i in range(NDMA_SLOTS)] for q in ("sp", "pool", "act")}
        self.dnext = {q: 0 for q in ("sp", "pool", "act")}

    def _newclk(self, name):
        self.nsem += 1
        c = [self.stack.enter_context(self.nc.semaphore(f"{name}_{self.nsem}")), 0]
        self.allclk.append(c)
        return c

    def _r(self, key):
        r = self.res.get(key)
        if r is None:
            r = self.res[key] = _Res()
        return r

    def _deps(self, eng, reads, writes, acc, is_dma):
        need = {}

        def add(d, kind):
            if d is None:
                return
            clk, tick, deng = d
            if (not is_dma) and deng == eng and kind != "raw":
                return
            if self.seen[eng].get(clk, 0) >= tick:
                return
            if need.get(clk, 0) < tick:
                need[clk] = tick

        for k in reads:
            add(self._r(k).lw, "raw")
        for k in writes:
            r = self._r(k)
            if not (acc and r.lw is not None and r.lw[2] == eng):
                add(r.lw, "waw")
            for d in r.rd.values():
                add(d, "war")
        return need

    def _commit(self, reads, writes, done):
        for k in reads:
            self._r(k).rd[done[0]] = done
        for k in writes:
            r = self._r(k)
            r.lw = done
            r.rd = {}

    def _emit_waits(self, eng, need):
        for clk, tick in need.items():
            self.seen[eng][clk] = tick
            self.streams[eng].append(("w", clk, tick))

    def op(self, eng, fn, reads=(), writes=(), inc=True, acc=False):
        need = self._deps(eng, reads, writes, acc, False)
        self._emit_waits(eng, need)
        c = self.clk[eng]
        if c[1] >= SEM_LIMIT and not self.pending.get(eng, False):
            c = self.clk[eng] = self._newclk(eng)
        self.pending[eng] = not inc
        if inc:
            c[1] += 1
            tick = c[1]
        else:
            tick = c[1] + 1
        done = (c[0], tick, eng)
        self.streams[eng].append(("i", fn, c[0] if inc else None))
        self._commit(reads, writes, done)
        self.ninstr += 1
        return done

    def dma(self, q, fn, reads=(), writes=()):
        need = self._deps(q, reads, writes, False, True)
        i = self.dnext[q]
        self.dnext[q] = (i + 1) % NDMA_SLOTS
        slot = self.dslots[q][i]
        if slot[1] >= SEM_LIMIT:
            slot = self.dslots[q][i] = self._newclk(f"d{q}{i}")
        if slot[1] > 0 and self.seen[q].get(slot[0], 0) < slot[1]:
            if need.get(slot[0], 0) < slot[1]:
                need[slot[0]] = slot[1]
        self._emit_waits(q, need)
        slot[1] += 16
        done = (slot[0], slot[1], "dma")
        self.streams[q].append(("d", fn, slot[0]))
        self._commit(reads, writes, done)
        self.ninstr += 1
        return done

    def barrier(self):
        for e in self.ENGS:
            need = {}
            for c in self.allclk:
                if c[1] > 0 and self.seen[e].get(c[0], 0) < c[1]:
                    need[c[0]] = c[1]
            self._emit_waits(e, need)
        self.res = {}

    def finish(self):
        nc = self.nc
        engmap = {"pe": "tensor", "act": "scalar", "dve": "vector", "pool": "gpsimd", "sp": "sync"}
        with nc.Block() as block:
            for e in self.ENGS:
                items = self.streams[e]

                def body(engobj, items=items):
                    for it in items:
                        if it[0] == "w":
                            engobj.wait_ge(it[1], it[2])
                        elif it[0] == "i":
                            ins = it[1](engobj)
                            if it[2] is not None:
                                ins.then_inc(it[2], 1)
                        else:
                            it[1](engobj).then_inc(it[2], 16)

                getattr(block, engmap[e])(body)


IN_A, IN_B, IN_C, IN_D = 0, 832, 2376, 3912
FT_CH = {}
_c = 0
for _nm, _n in (("qlat", 4), ("kvlat", 2), ("kr", 1), ("krs", 1), ("z", 4), ("xs", 4), ("bm", 2), ("cm", 2),
                ("qc", 4), ("kc", 4), ("qd", 4), ("kd", 4)):
    FT_CH[_nm] = _c
    _c += _n
NFT = _c


class K:
    pass


def build(depth, n_exp=NEXP, dbg_cat=False, dbg_out=False, chain=False):
    nc = bass.Bass("TRN2", target_bir_lowering=False)
    k = K()
    k.nc = nc

    def din(name, shape, dt=F32):
        return nc.dram_tensor(name, list(shape), dt, kind="ExternalInput").ap()

    def dscr(name, shape, dt):
        return nc.dram_tensor(name, list(shape), dt, kind="Internal").ap()

    L = depth
    x_in = din("x", [S, D])
    c_in = din("c", [16, 128])
    attn_norm = din("attn_norm", [L, 16, 128])
    ffn_norm = din("ffn_norm", [L, 16, 128])
    mod_w = din("mod_w", [L, D, 6 * D])
    mod_b = din("mod_b", [L, 96, 128])
    w_in = din("w_in", [L, D, 5448])
    mla_q_norm = din("mla_q_norm", [L, 4, 128])
    wq = din("mla_w_q_up", [L, 512, 768])
    mla_kv_norm = din("mla_kv_norm", [L, 2, 128])
    wkv = din("mla_w_kv_up", [L, 256, 1024])
    conv_w = din("ssm_conv_w", [L, 4 * 8, 128])
    conv_b = din("ssm_conv_b", [L, 8, 128])
    dt_bias = din("ssm_dt_bias", [L, 8, 1])
    a_log = din("ssm_a_log", [L, 8, 1])
    ssm_d = din("ssm_d", [L, 8])
    ssm_norm = din("ssm_norm", [L, 4, 128])
    ssm_norm8 = din("ssm_norm8", [L, 8, 64])
    ca_bias = din("ca_biasT", [L, 4, 5, 128, 128])
    mix_norm = din("mix_out_norm", [L, 12, 128])
    w_out = din("w_out", [L, D, D])
    router_w = din("router_w", [L, D, NEXP])
    router_b = din("router_b", [L, 1, NEXP])
    w1 = din("moe_w1", [L, NEXP, D, 2 * DEXP])
    b1 = din("moe_b1", [L, NEXP * 12, 128])
    w2 = din("moe_w2", [L, NEXP, DEXP, D])
    b2 = din("moe_b2", [L, NEXP, D])
    final_norm = din("final_norm", [16, 128])
    cst = din("cst", [128, 8, 128])
    sel8 = din("sel8", [8, 8 * 128])
    sel32 = din("sel32", [32, 32 * 128])
    rope_cos = din("rope_cos", [128, S])
    rope_sin = din("rope_sin", [128, S])
    y_out = nc.dram_tensor("y", [S, D], F32, kind="ExternalOutput").ap()
    xo_out = nc.dram_tensor("xo", [S, D], F32, kind="ExternalOutput").ap() if chain else None

    xT = dscr("xT", [D, S], F32)
    FT = dscr("FT", [NFT * 128, S], BF16)
    DTR = dscr("DTR", [8, S], F32)
    VCs = dscr("VC", [S, 512], BF16)
    VDs = dscr("VD", [S, 512], BF16)
    catT = din("catT_in", [D, S], BF16) if dbg_cat else dscr("catT", [D, S], BF16)
    k.dbg_cat = dbg_cat
    k.dbg_out = dbg_out
    cat_dump = nc.dram_tensor("cat_dump", [D, S], BF16, kind="ExternalOutput").ap() if (dbg_out and not dbg_cat) else None
    h2T = dscr("h2T", [D, S], BF16)
    GTs = dscr("GT", [NEXP, S], F32)
    FT2 = dscr("FT2", [11 * 128, S], BF16)
    VMs = dscr("VM", [S, 512], BF16)
    XSs = dscr("XSs", [S, 512], BF16)
    CSs = dscr("CSs", [8, S], F32)
    CSCs = dscr("CSCs", [128, 256], F32)
    DTCs = dscr("DTCs", [128, 256], F32)

    with ExitStack() as gst:
        P = Prog(nc, gst)

        ARENA_BYTES = 196608
        ARENA = None if NOARENA else gst.enter_context(nc.sbuf_tensor("ARENA", [128, ARENA_BYTES // 2], BF16))
        aoff = [0]

        class Tile:
            def __init__(self, ap, name, shape):
                self.ap, self.name, self.shape = ap, name, tuple(shape)

            def __getitem__(self, key):
                return self.ap[key]

        sbcnt = [0]

        def sb(st, name, shape, dt):
            if NOARENA:
                sbcnt[0] += 1
                return Tile(st.enter_context(nc.sbuf_tensor(f"{name}__u{sbcnt[0]}", list(shape), dt)), name, shape)
            esz = 4 if dt == F32 else 2
            nel = 1
            for d_ in shape[1:]:
                nel *= d_
            nbytes = (nel * esz + 63) // 64 * 64
            off = aoff[0]
            assert off + nbytes <= ARENA_BYTES, (name, off, nbytes)
            aoff[0] = off + nbytes
            if st is not gst:
                st.callback(lambda off=off: aoff.__setitem__(0, off))
            v = ARENA[0:shape[0], off // 2:off // 2 + nel * esz // 2]
            if dt == F32:
                v = v.bitcast(F32)
            if len(shape) == 3:
                v = v.rearrange("p (a b) -> p a b", a=shape[1])
            return Tile(v, name, shape)

        PS = [gst.enter_context(nc.psum_tensor(f"ps{i}", [128, 512], F32)) for i in range(8)]
        CF = sb(gst, "CF", [128, 8, 128], F32)
        CB = sb(gst, "CB", [128, 8, 128], BF16)
        SEL = sb(gst, "SEL", [8, 8 * 128], F32)
        P.dma("sp", lambda e: e.dma_start(out=CF[:], in_=cst[:, :, :]), writes=["CF"])
        P.dma("sp", lambda e: e.dma_start(out=SEL[:], in_=sel8[:, :]), writes=["SEL"])
        P.op("dve", lambda e: e.tensor_copy(out=CB[:], in_=CF[:]), reads=["CF"], writes=["CB"])
        IDF, ONESF = CF[:, 0, :], CF[:, 1, :]
        IDB, ONESB, TRIB, ZB = CB[:, 0, :], CB[:, 1, :], CB[:, 5, :], CB[:, 6, :]
        M_LT, M_LE, M_MLA = CF[:, 2, :], CF[:, 3, :], CF[:, 4, :]
        COLS = sb(gst, "COLS", [128, 400], F32)
        colmap = {}
        _o = 0
        for nm, n in (("an", 16), ("fn", 16), ("modb", 96), ("mod", 96), ("A1", 16), ("A2", 16), ("qn", 4), ("kvn", 2),
                      ("cw", 32), ("cb", 8), ("sn", 4), ("mn", 12), ("cond", 16), ("fin", 16), ("b1", 12)):
            colmap[nm] = (_o, n)
            _o += n
        assert _o <= 400

        def col(nm, j=0, n=1):
            o = colmap[nm][0] + j
            return COLS[:, o:o + n]

        cnt = [0]

        def uid(s):
            cnt[0] += 1
            return f"{s}{cnt[0]}"

        def colize(st, src_ap, n, nm, j0=0):
            t = sb(st, uid("cz"), [128, 128], F32)
            key = uid("czk")
            P.dma("sp", lambda e: e.dma_start(out=t[:n, :], in_=src_ap), writes=[key])
            P.op("pe", lambda e: e.matmul(PS[7][:, :n], lhsT=t[:n, :], rhs=IDF[:n, :n], start=True, stop=True),
                 reads=[key, "CF"], writes=["ps7"])
            P.op("dve", lambda e: e.tensor_copy(out=col(nm, j0, n), in_=PS[7][:, :n]), reads=["ps7"], writes=["COLS"])

        def rstd_from_ps(ps_t, out_t, dim, tmp_t, p=128):
            P.op("dve", lambda e: e.tensor_scalar(out=tmp_t[:p, :], in0=ps_t[:p, :], scalar1=1.0 / dim, scalar2=EPS, op0=ALU.mult, op1=ALU.add),
                 reads=[nm_(ps_t)], writes=[nm_(tmp_t)])
            P.op("act", lambda e: e.activation(out=tmp_t[:p, :], in_=tmp_t[:p, :], func=AF.Sqrt), reads=[nm_(tmp_t)], writes=[nm_(tmp_t)])
            P.op("dve", lambda e: e.reciprocal(out=out_t[:p, :], in_=tmp_t[:p, :]), reads=[nm_(tmp_t)], writes=[nm_(out_t)])

        def nm_(t):
            if isinstance(t, str):
                return t
            if isinstance(t, Tile):
                return t.name
            n_ = t.tensor.name if hasattr(t, "tensor") else t.name
            assert n_ != "ARENA", "arena AP passed to nm_"
            return n_

        with ExitStack() as st:
            XI = [sb(st, f"XI{i}", [128, D], F32) for i in range(2)]
            XO = [sb(st, f"XO{i}", [128, 4, 128], F32) for i in range(2)]
            n = 0
            for tt in range(S // 128):
                xi = XI[tt % 2]
                P.dma("sp", lambda e, xi=xi, tt=tt: e.dma_start(out=xi[:], in_=x_in[tt * 128:(tt + 1) * 128, :]), writes=[nm_(xi)])
                for g in range(4):
                    ps = PS[n % 4]
                    xo = XO[n % 2]
                    for j in range(4):
                        kc = g * 4 + j
                        P.op("pe", lambda e, ps=ps, xi=xi, j=j, kc=kc: e.matmul(ps[:, j * 128:(j + 1) * 128], lhsT=xi[:, kc * 128:(kc + 1) * 128], rhs=IDF, start=True, stop=True),
                             reads=[nm_(xi), "CF"], writes=[nm_(ps)], inc=(j == 3))
                    P.op("act", lambda e, ps=ps, xo=xo: e.activation(out=xo[:].rearrange("p a b -> p (a b)"), in_=ps[:, :], func=AF.Copy), reads=[nm_(ps)], writes=[nm_(xo)])
                    P.dma("sp", lambda e, xo=xo, g=g, tt=tt: e.dma_start(
                        out=xT[g * 512:(g + 1) * 512, tt * 128:(tt + 1) * 128].rearrange("(a p) t -> p a t", p=128), in_=xo[:]),
                        reads=[nm_(xo)], writes=["xT"])
                    n += 1
            colize(st, c_in[:, :], 16, "cond")
            P.op("act", lambda e: e.activation(out=col("cond", 0, 16), in_=col("cond", 0, 16), func=AF.Silu), reads=["COLS"], writes=["COLS"])
            colize(st, final_norm[:, :], 16, "fin")
        P.barrier()

        def load_xT_blk(XT, tb, src=xT, q="sp"):
            P.dma(q, lambda e: e.dma_start(out=XT[:], in_=src[:, tb * TB:(tb + 1) * TB].rearrange("(kc p) t -> p kc t", p=128)),
                  reads=[nm_(src)], writes=[nm_(XT)])

        def norm_mod(st_tiles, XT, HT, Acol, Scol, tb, HF=None):
            SQ, tmp, rstd, t2 = st_tiles
            P.op("act", lambda e: e.activation(out=SQ[:].rearrange("p a b -> p (a b)"), in_=XT[:].rearrange("p a b -> p (a b)"), func=AF.Square),
                 reads=[nm_(XT)], writes=[nm_(SQ)])
            for kc in range(NKC):
                P.op("pe", lambda e, kc=kc: e.matmul(PS[6][:, :], lhsT=ONESB, rhs=SQ[:, kc, :], start=(kc == 0), stop=(kc == NKC - 1)),
                     reads=[nm_(SQ), "CB"], writes=["ps6"], inc=(kc == NKC - 1), acc=(kc > 0))
            rstd_from_ps(PS[6], rstd, D, tmp)
            for kc in range(NKC):
                P.op("dve", lambda e, kc=kc: e.scalar_tensor_tensor(out=t2[:], in0=XT[:, kc, :], scalar=Acol(kc), in1=rstd[:], op0=ALU.mult, op1=ALU.mult),
                     reads=[nm_(XT), nm_(rstd), "COLS"], writes=[nm_(t2)])
                if HF is None:
                    P.op("dve", lambda e, kc=kc: e.tensor_scalar(out=HT[:, kc, :], in0=t2[:], scalar1=Scol(kc), scalar2=None, op0=ALU.add),
                         reads=[nm_(t2), "COLS"], writes=[nm_(HT)])
                else:
                    P.op("dve", lambda e, kc=kc: e.tensor_scalar(out=HF[:, kc, :], in0=t2[:], scalar1=Scol(kc), scalar2=None, op0=ALU.add),
                         reads=[nm_(t2), "COLS"], writes=[nm_(HF)])
                    P.op("act", lambda e, kc=kc: e.activation(out=HT[:, kc, :], in_=HF[:, kc, :], func=AF.Copy), reads=[nm_(HF)], writes=[nm_(HT)])

        def group_norm_store(OA, nch, gcolf, dst_rows, tb, SQs, tmp, rstd, stg, dim):
            P.op("act", lambda e: e.activation(out=SQs[:, :nch, :].rearrange("p a b -> p (a b)"), in_=OA[:, :nch, :].rearrange("p a b -> p (a b)"), func=AF.Square),
                 reads=[nm_(OA)], writes=[nm_(SQs)])
            for c in range(nch):
                P.op("pe", lambda e, c=c: e.matmul(PS[6][:, :], lhsT=ONESB, rhs=SQs[:, c, :], start=(c == 0), stop=(c == nch - 1)),
                     reads=[nm_(SQs), "CB"], writes=["ps6"], inc=(c == nch - 1), acc=(c > 0))
            rstd_from_ps(PS[6], rstd, dim, tmp)
            for c in range(nch):
                P.op("dve", lambda e, c=c: e.scalar_tensor_tensor(out=stg[:, c, :], in0=OA[:, c, :], scalar=gcolf(c), in1=rstd[:], op0=ALU.mult, op1=ALU.mult),
                     reads=[nm_(OA), nm_(rstd), "COLS"], writes=[nm_(stg)])
            P.dma("sp", lambda e: e.dma_start(out=catT[dst_rows:dst_rows + nch * 128, tb * TB:(tb + 1) * TB].rearrange("(a p) t -> p a t", p=128), in_=stg[:, :nch, :]),
                  reads=[nm_(stg)], writes=["catT"])

        benv = dict(locals())

        def do_layer(l):
            with ExitStack() as st:
                colize(st, attn_norm[l], 16, "an")
                colize(st, ffn_norm[l], 16, "fn")
                colize(st, mod_b[l], 96, "modb")
                colize(st, mla_q_norm[l], 4, "qn")
                colize(st, mla_kv_norm[l], 2, "kvn")
                colize(st, conv_w[l], 32, "cw")
                colize(st, conv_b[l], 8, "cb")
                colize(st, ssm_norm[l], 4, "sn")
                colize(st, mix_norm[l], 12, "mn")
                MW = [sb(st, f"MW{i}", [128, NKC, 512], F32) for i in range(2)]
                for nb in range(24):
                    mw = MW[nb % 2]
                    P.dma("sp" if nb % 2 == 0 else "act", lambda e, mw=mw, nb=nb: e.dma_start(out=mw[:], in_=mod_w[l][:, nb * 512:(nb + 1) * 512].rearrange("(kc p) n -> p kc n", p=128)),
                          writes=[nm_(mw)])
                    for j in range(4):
                        for kc in range(NKC):
                            P.op("pe", lambda e, mw=mw, j=j, kc=kc: e.matmul(PS[7][:, j:j + 1], lhsT=mw[:, kc, j * 128:(j + 1) * 128], rhs=col("cond", kc, 1), start=(kc == 0), stop=(kc == NKC - 1)),
                                 reads=[nm_(mw), "COLS"], writes=["ps7"], inc=(kc == NKC - 1), acc=(kc > 0))
                    P.op("dve", lambda e, nb=nb: e.tensor_tensor(out=col("mod", nb * 4, 4), in0=PS[7][:, 0:4], in1=col("modb", nb * 4, 4), op=ALU.add),
                         reads=["ps7", "COLS"], writes=["COLS"])
                P.op("dve", lambda e: e.scalar_tensor_tensor(out=col("A1", 0, 16), in0=col("mod", 16, 16), scalar=1.0, in1=col("an", 0, 16), op0=ALU.add, op1=ALU.mult),
                     reads=["COLS"], writes=["COLS"])
                P.op("dve", lambda e: e.scalar_tensor_tensor(out=col("A2", 0, 16), in0=col("mod", 64, 16), scalar=1.0, in1=col("fn", 0, 16), op0=ALU.add, op1=ALU.mult),
                     reads=["COLS"], writes=["COLS"])
            P.barrier()

            with ExitStack() as st:
                XT = sb(st, "XT", [128, NKC, TB], F32)
                HT = sb(st, "HT", [128, NKC, TB], BF16)
                SQ = sb(st, "SQ", [128, NKC, TB], BF16)
                tmp = sb(st, "tmp", [128, TB], F32)
                rstd = sb(st, "rstd", [128, TB], F32)
                t2 = sb(st, "t2", [128, TB], F32)
                WB = [sb(st, f"WB{i}", [128, NKC, 512], BF16) for i in range(2)]
                STG = [sb(st, f"STG{i}", [128, TB], BF16) for i in range(3)]
                STF = sb(st, "STF", [128, TB], F32)
                blocks = []
                blocks.append(([(IN_A, 512)], [("ft", FT_CH["qlat"] + i) for i in range(4)]))
                blocks.append(([(IN_A + 512, 256), (IN_A + 768, 64), (IN_A + 768, 64), (IN_A + 800, 32), (IN_A + 768, 32), (IN_A + 800, 32), (IN_A + 768, 32)],
                               [("ft", FT_CH["kvlat"]), ("ft", FT_CH["kvlat"] + 1), ("ft", FT_CH["kr"]), None]))
                blocks[-1] = (blocks[-1][0], [("ft", FT_CH["kvlat"]), ("ft", FT_CH["kvlat"] + 1), ("ft", FT_CH["kr"]), ("ft64", FT_CH["krs"])])
                blocks.append(([(IN_B, 512)], [("ft", FT_CH["z"] + i) for i in range(4)]))
                blocks.append(([(IN_B + 512, 512)], [("ft", FT_CH["xs"] + i) for i in range(4)]))
                blocks.append(([(IN_B + 1024, 512)], [("ft", FT_CH["bm"]), ("ft", FT_CH["bm"] + 1), ("ft", FT_CH["cm"]), ("ft", FT_CH["cm"] + 1)]))
                blocks.append(([(IN_B + 1536, 8)], [("dtr", 0)]))
                blocks.append(([(IN_C, 512)], [("ft", FT_CH["qc"] + i) for i in range(4)]))
                blocks.append(([(IN_C + 512, 512)], [("ft", FT_CH["kc"] + i) for i in range(4)]))
                blocks.append(([(IN_D, 512)], [("ft", FT_CH["qd"] + i) for i in range(4)]))
                blocks.append(([(IN_D + 512, 512)], [("ft", FT_CH["kd"] + i) for i in range(4)]))
                blocks.append(([(IN_C + 1024, 512)], [("v", VCs)]))
                blocks.append(([(IN_D + 1024, 512)], [("v", VDs)]))
                wl = w_in[l].rearrange("(kc p) n -> p kc n", p=128)
                nw = 0
                ne = 0
                for tb in range(NTB):
                    load_xT_blk(XT, tb)
                    norm_mod((SQ, tmp, rstd, t2), XT, HT, lambda kc: col("A1", kc), lambda kc: col("mod", kc), tb)
                    for segs, dests in blocks:
                        wb = WB[nw % 2]
                        nw += 1
                        o = 0
                        for (c0, n_) in segs:
                            P.dma("pool", lambda e, wb=wb, o=o, c0=c0, n_=n_: e.dma_start(out=wb[:, :, o:o + n_], in_=wl[:, :, c0:c0 + n_]), writes=[nm_(wb)])
                            o += n_
                        if dests[0][0] == "v":
                            for tt in range(4):
                                ps = PS[ne % 4]
                                stg = STG[ne % 3]
                                ne += 1
                                for kc in range(NKC):
                                    P.op("pe", lambda e, ps=ps, wb=wb, kc=kc, tt=tt: e.matmul(ps[:, :], lhsT=HT[:, kc, tt * 128:(tt + 1) * 128], rhs=wb[:, kc, :], start=(kc == 0), stop=(kc == NKC - 1)),
                                         reads=[nm_(wb), "HT"], writes=[nm_(ps)], inc=(kc == NKC - 1), acc=(kc > 0))
                                P.op("act", lambda e, ps=ps, stg=stg: e.activation(out=stg[:], in_=ps[:, :], func=AF.Copy), reads=[nm_(ps)], writes=[nm_(stg)])
                                dstv = dests[0][1]
                                r0 = tb * TB + tt * 128
                                P.dma("sp", lambda e, stg=stg, dstv=dstv, r0=r0: e.dma_start(out=dstv[r0:r0 + 128, :], in_=stg[:]), reads=[nm_(stg)], writes=[nm_(dstv)])
                            continue
                        for j, dst in enumerate(dests):
                            m = 128
                            if dst[0] == "dtr":
                                m = 8
                            ps = PS[ne % 4]
                            stg = STG[ne % 3]
                            ne += 1
                            for kc in range(NKC):
                                P.op("pe", lambda e, ps=ps, wb=wb, kc=kc, j=j, m=m: e.matmul(ps[:m, :], lhsT=wb[:, kc, j * 128:j * 128 + m], rhs=HT[:, kc, :], start=(kc == 0), stop=(kc == NKC - 1)),
                                     reads=[nm_(wb), "HT"], writes=[nm_(ps)], inc=(kc == NKC - 1), acc=(kc > 0))
                            if dst[0] == "dtr":
                                P.op("act", lambda e, ps=ps: e.activation(out=STF[:8, :], in_=ps[:8, :], func=AF.Copy), reads=[nm_(ps)], writes=["STF"])
                                P.dma("sp", lambda e, tb=tb: e.dma_start(out=DTR[:, tb * TB:(tb + 1) * TB], in_=STF[:8, :]), reads=["STF"], writes=["DTR"])
                            else:
                                P.op("act", lambda e, ps=ps, stg=stg: e.activation(out=stg[:], in_=ps[:, :], func=AF.Copy), reads=[nm_(ps)], writes=[nm_(stg)])
                                ch = dst[1]
                                if dst[0] == "ft64":
                                    pass
                                P.dma("sp", lambda e, stg=stg, ch=ch, tb=tb: e.dma_start(out=FT[ch * 128:(ch + 1) * 128, tb * TB:(tb + 1) * TB], in_=stg[:]), reads=[nm_(stg)], writes=["FT"])
            P.barrier()
            k.l = l
            if not dbg_cat:
                mixers(P, k, {**benv, **locals()})
            P.barrier()
            if cat_dump is not None and l == 0:
                P.dma("sp", lambda e: e.dma_start(out=cat_dump[:, :], in_=catT[:, :]), reads=["catT"], writes=["cat_dump"])
                P.barrier()
            tail(P, k, {**benv, **locals()}, n_exp)
            P.barrier()


        for l_ in range(L):
            do_layer(l_)

        if dbg_out:
            for nm2, src, shp, dt_ in (("d_h2T", h2T, [D, S], BF16), ("d_GT", GTs, [NEXP, S], F32), ("d_xT", xT, [D, S], F32)):
                dd = nc.dram_tensor(nm2, shp, dt_, kind="ExternalOutput").ap()
                P.dma("sp", lambda e, dd=dd, src=src: e.dma_start(out=dd[:, :], in_=src[:, :]), reads=[], writes=[nm2])
            P.barrier()
        final(P, k, locals())
        P.finish()
    k.P = P
    return nc, k


def mixers(P, k, env):
    nc = k.nc
    l = k.l
    PS, FT, col, IDF, ONESB, ONESF = env["PS"], env["FT"], env["col"], env["IDF"], env["ONESB"], env["ONESF"]
    IDB, TRIB, ZB, M_LT, M_LE, M_MLA = env["IDB"], env["TRIB"], env["ZB"], env["M_LT"], env["M_LE"], env["M_MLA"]
    sb, nm_, rstd_from_ps, group_norm_store = env["sb"], env["nm_"], env["rstd_from_ps"], env["group_norm_store"]
    wq, wkv, VCs, VDs, DTR, ca_bias, catT = env["wq"], env["wkv"], env["VCs"], env["VDs"], env["DTR"], env["ca_bias"], env["catT"]
    rope_cos, rope_sin, FT2, VMs, SEL = env["rope_cos"], env["rope_sin"], env["FT2"], env["VMs"], env["SEL"]
    dt_bias, a_log, ssm_d, CF = env["dt_bias"], env["a_log"], env["ssm_d"], env["CF"]

    def ftload(dst, h, ch, src=FT, q="sp"):
        P.dma(q, lambda e: e.dma_start(out=dst[:, h, :], in_=src[ch * 128:(ch + 1) * 128, :]), reads=[nm_(src)], writes=[nm_(dst)])

    def mm(out, lhsT, rhs, start, stop, reads, writes, inc=None):
        P.op("pe", lambda e: e.matmul(out, lhsT=lhsT, rhs=rhs, start=start, stop=stop), reads=reads, writes=writes, inc=(stop if inc is None else inc), acc=not start)

    def mla_prep():
      with ExitStack() as st:
        LT = sb(st, "aLT", [128, 6, TB], BF16)
        KRt = sb(st, "aKR", [128, 2, TB], BF16)
        COS = sb(st, "aCOS", [128, TB], F32)
        SIN = sb(st, "aSIN", [128, TB], F32)
        SQ = sb(st, "aSQ", [128, 6, TB], BF16)
        NRM = sb(st, "aNRM", [128, 6, TB], BF16)
        tmp = sb(st, "atmp", [128, TB], F32)
        rstd = sb(st, "arstd", [128, TB], F32)
        t1 = sb(st, "at1", [128, TB], F32)
        t2 = sb(st, "at2", [128, TB], F32)
        WQ = sb(st, "aWQ", [128, 4, 1280], BF16)
        WKV = sb(st, "aWKV", [128, 2, 1536], BF16)
        STG = [sb(st, f"aSTG{i}", [128, TB], BF16) for i in range(3)]
        wql = wq[l].rearrange("(kc p) n -> p kc n", p=128)
        wkl = wkv[l].rearrange("(kc p) n -> p kc n", p=128)
        P.dma("pool", lambda e: e.dma_start(out=WQ[:, :, 0:768], in_=wql[:, :, :]), writes=["aWQ"])
        P.dma("pool", lambda e: e.dma_start(out=WKV[:, :, 0:1024], in_=wkl[:, :, :]), writes=["aWKV"])
        for h in range(4):
            P.dma("pool", lambda e, h=h: e.dma_start(out=WQ[:, :, 768 + 64 * h:768 + 64 * h + 64], in_=wql[:, :, 192 * h + 128:192 * h + 192]), writes=["aWQ"])
            P.dma("pool", lambda e, h=h: e.dma_start(out=WQ[:, :, 1024 + 64 * h:1024 + 64 * h + 32], in_=wql[:, :, 192 * h + 160:192 * h + 192]), writes=["aWQ"])
            P.dma("pool", lambda e, h=h: e.dma_start(out=WQ[:, :, 1024 + 64 * h + 32:1024 + 64 * h + 64], in_=wql[:, :, 192 * h + 128:192 * h + 160]), writes=["aWQ"])
            P.dma("pool", lambda e, h=h: e.dma_start(out=WKV[:, :, 1024 + 128 * h:1024 + 128 * h + 128], in_=wkl[:, :, 256 * h + 128:256 * h + 256]), writes=["aWKV"])
        ne = 0
        for tb in range(NTB):
            sl = slice(tb * TB, (tb + 1) * TB)
            for c in range(6):
                P.dma("sp", lambda e, c=c, sl=sl: e.dma_start(out=LT[:, c, :], in_=FT[(FT_CH["qlat"] + c) * 128:(FT_CH["qlat"] + c + 1) * 128, sl]), reads=["FT"], writes=["aLT"])
            for c in range(2):
                P.dma("sp", lambda e, c=c, sl=sl: e.dma_start(out=KRt[:, c, :], in_=FT[(FT_CH["kr"] + c) * 128:(FT_CH["kr"] + c + 1) * 128, sl]), reads=["FT"], writes=["aKR"])
            P.dma("act", lambda e, sl=sl: e.dma_start(out=COS[:], in_=rope_cos[:, sl]), writes=["aCOS"])
            P.dma("act", lambda e, sl=sl: e.dma_start(out=SIN[:], in_=rope_sin[:, sl]), writes=["aSIN"])
            P.op("act", lambda e: e.activation(out=SQ[:].rearrange("p a b -> p (a b)"), in_=LT[:].rearrange("p a b -> p (a b)"), func=AF.Square), reads=["aLT"], writes=["aSQ"])
            for (c0, n, gname, dim) in ((0, 4, "qn", 512), (4, 2, "kvn", 256)):
                for c in range(n):
                    mm(PS[6][:, :], ONESB, SQ[:, c0 + c, :], c == 0, c == n - 1, ["aSQ", "CB"], ["ps6"])
                rstd_from_ps(PS[6], rstd, dim, tmp)
                for c in range(n):
                    P.op("dve", lambda e, c=c, c0=c0, gname=gname: e.scalar_tensor_tensor(out=NRM[:, c0 + c, :], in0=LT[:, c0 + c, :], scalar=col(gname, c), in1=rstd[:], op0=ALU.mult, op1=ALU.mult),
                         reads=["aLT", "arstd", "COLS"], writes=["aNRM"])
            for h in range(8):
                ps = PS[ne % 4]
                stg = STG[ne % 3]
                ne += 1
                if h < 4:
                    for kc in range(4):
                        mm(ps[:, :], WQ[:, kc, 192 * h:192 * h + 128], NRM[:, kc, :], kc == 0, kc == 3, ["aWQ", "aNRM"], [nm_(ps)])
                    row = h
                else:
                    hh = h - 4
                    for kc in range(2):
                        mm(ps[:, :], WKV[:, kc, 256 * hh:256 * hh + 128], NRM[:, 4 + kc, :], kc == 0, kc == 1, ["aWKV", "aNRM"], [nm_(ps)])
                    row = 6 + hh
                P.op("act", lambda e, ps=ps, stg=stg: e.activation(out=stg[:], in_=ps[:, :], func=AF.Copy), reads=[nm_(ps)], writes=[nm_(stg)])
                P.dma("sp", lambda e, stg=stg, row=row, sl=sl: e.dma_start(out=FT2[row * 128:(row + 1) * 128, sl], in_=stg[:]), reads=[nm_(stg)], writes=["FT2"])
            for ab in range(2):
                psa, pss = PS[4], PS[5]
                for kc in range(4):
                    mm(psa[:, :], WQ[:, kc, 768 + 128 * ab:768 + 128 * ab + 128], NRM[:, kc, :], kc == 0, kc == 3, ["aWQ", "aNRM"], ["ps4"])
                for kc in range(4):
                    mm(pss[:, :], WQ[:, kc, 1024 + 128 * ab:1024 + 128 * ab + 128], NRM[:, kc, :], kc == 0, kc == 3, ["aWQ", "aNRM"], ["ps5"])
                stg = STG[ne % 3]
                ne += 1
                P.op("dve", lambda e: e.tensor_tensor(out=t1[:], in0=psa[:, :], in1=COS[:], op=ALU.mult), reads=["ps4", "aCOS"], writes=["at1"])
                P.op("dve", lambda e: e.tensor_tensor(out=t2[:], in0=pss[:, :], in1=SIN[:], op=ALU.mult), reads=["ps5", "aSIN"], writes=["at2"])
                P.op("dve", lambda e, stg=stg: e.tensor_tensor(out=stg[:], in0=t1[:], in1=t2[:], op=ALU.add), reads=["at1", "at2"], writes=[nm_(stg)])
                P.dma("sp", lambda e, stg=stg, ab=ab, sl=sl: e.dma_start(out=FT2[(4 + ab) * 128:(5 + ab) * 128, sl], in_=stg[:]), reads=[nm_(stg)], writes=["FT2"])
            stg = STG[ne % 3]
            ne += 1
            P.op("dve", lambda e: e.tensor_tensor(out=t1[:], in0=KRt[:, 0, :], in1=COS[:], op=ALU.mult), reads=["aKR", "aCOS"], writes=["at1"])
            P.op("dve", lambda e: e.tensor_tensor(out=t2[:], in0=KRt[:, 1, :], in1=SIN[:], op=ALU.mult), reads=["aKR", "aSIN"], writes=["at2"])
            P.op("dve", lambda e, stg=stg: e.tensor_tensor(out=stg[:], in0=t1[:], in1=t2[:], op=ALU.add), reads=["at1", "at2"], writes=[nm_(stg)])
            P.dma("sp", lambda e, stg=stg, sl=sl: e.dma_start(out=FT2[10 * 128:11 * 128, sl], in_=stg[:]), reads=[nm_(stg)], writes=["FT2"])
            for tt in range(4):
                ps = PS[ne % 4]
                stg = STG[ne % 3]
                ne += 1
                for kc in range(2):
                    mm(ps[:, :], NRM[:, 4 + kc, tt * 128:(tt + 1) * 128], WKV[:, kc, 1024:1536], kc == 0, kc == 1, ["aWKV", "aNRM"], [nm_(ps)])
                P.op("act", lambda e, ps=ps, stg=stg: e.activation(out=stg[:], in_=ps[:, :], func=AF.Copy), reads=[nm_(ps)], writes=[nm_(stg)])
                r0 = tb * TB + tt * 128
                P.dma("sp", lambda e, stg=stg, r0=r0: e.dma_start(out=VMs[r0:r0 + 128, :], in_=stg[:]), reads=[nm_(stg)], writes=["VM"])
    mla_prep()
    P.barrier()

    def attn_phase(kind):
        with ExitStack() as st:
            Q = sb(st, "tQ", [128, 4, S], BF16)
            Kt = sb(st, "tK", [128, 4, S], BF16)
            V = sb(st, "tV", [128, 32, 512], BF16)
            OA = sb(st, "tOA", [128, 4, TB], F32)
            SQs = sb(st, "tSQs", [128, 4, TB], BF16)
            tmp = sb(st, "ttmp", [128, TB], F32)
            rstd = sb(st, "trstd", [128, TB], F32)
            stg = sb(st, "tstg", [128, 4, TB], BF16)
            ATT = [sb(st, f"tATT{i}", [128, TB], BF16) for i in range(3)]
            RD = sb(st, "tRD", [128, TB], F32)
            if kind == "mla":
                QR = sb(st, "tQR", [128, 2, S], BF16)
                KRD = sb(st, "tKRD", [128, 1, S], BF16)
                for h in range(4):
                    ftload(Q, h, h, FT2)
                    ftload(Kt, h, 6 + h, FT2, "act")
                ftload(QR, 0, 4, FT2)
                ftload(QR, 1, 5, FT2)
                ftload(KRD, 0, 10, FT2)
                vsrc, scale, mrow, dst = VMs, 192.0 ** -0.5, 0, 0
            elif kind == "sb":
                for h in range(4):
                    ftload(Q, h, FT_CH["qc"] + h)
                    ftload(Kt, h, FT_CH["kc"] + h, FT, "act")
                vsrc, scale, mrow, dst = VCs, 128.0 ** -0.5, 4, 1024
                CAC = sb(st, "tCAC", [128, TB], F32)
                E1 = sb(st, "tE1", [128, TB], F32)
                SP = sb(st, "tSP", [128, TB], F32)
                NLK = sb(st, "tNLK", [128, TB], BF16)
                T1 = sb(st, "tT1", [128, TB], F32)
            else:
                for h in range(4):
                    ftload(Q, h, FT_CH["qd"] + h)
                    ftload(Kt, h, FT_CH["kd"] + h, FT, "act")
                vsrc, scale, mrow, dst = VDs, 128.0 ** -0.5, 8, 1536
                BT = sb(st, "tBT", [128, 20, 128], F32)
                P.dma("sp", lambda e: e.dma_start(out=BT[:], in_=ca_bias[l].rearrange("h j k q -> k (h j) q")), writes=["tBT"])
                P.op("dve", lambda e: e.tensor_scalar(out=BT[:].rearrange("p a b -> p (a b)"), in0=BT[:].rearrange("p a b -> p (a b)"), scalar1=float(128.0 ** 0.5), scalar2=None, op0=ALU.mult), reads=["tBT"], writes=["tBT"])
            P.dma("sp", lambda e: e.dma_start(out=V[:], in_=vsrc.rearrange("(b p) c -> p b c", p=128)), reads=[nm_(vsrc)], writes=["tV"])
            n = 0
            for QB in range(NTB):
                for h in range(4):
                    if kind == "ca":
                        for qi in range(4):
                            qt = QB * 4 + qi
                            blks = [jb for jb in range(5) if qt - 4 + jb >= 0]
                            for idx, jb in enumerate(blks):
                                kb = qt - 4 + jb
                                ps = PS[n % 2]
                                att = ATT[n % 3]
                                n += 1
                                mm(ps[:, :128], Kt[:, h, kb * 128:(kb + 1) * 128], Q[:, h, qt * 128:(qt + 1) * 128], True, False, ["tK", "tQ"], [nm_(ps)])
                                mm(ps[:, :128], IDF, BT[:, h * 5 + jb, :], False, True, ["tBT", "CF"], [nm_(ps)])
                                P.op("act", lambda e, ps=ps, att=att: e.activation(out=att[:, :128], in_=ps[:, :128], func=AF.Exp, scale=float(scale)), reads=[nm_(ps)], writes=[nm_(att)])
                                last = idx == len(blks) - 1
                                mm(PS[2][:, qi * 128:(qi + 1) * 128], V[:, kb, h * 128:(h + 1) * 128], att[:, :128], idx == 0, last, ["tV", nm_(att)], ["ps2"], inc=False)
                                mm(PS[3][:, qi * 128:(qi + 1) * 128], ONESB, att[:, :128], idx == 0, last, ["CB", nm_(att)], ["ps3"], inc=True)
                    elif kind == "mla":
                        nkb = 4 * QB + 4
                        for kb in range(nkb):
                            j = kb - 4 * QB
                            q0 = 128 * max(j, 0)
                            nn = TB - q0
                            qs = slice(QB * TB + q0, (QB + 1) * TB)
                            ps = PS[n % 2]
                            att = ATT[n % 3]
                            n += 1
                            r0 = 64 * (h % 2)
                            mm(ps[:, :nn], Kt[:, h, kb * 128:(kb + 1) * 128], Q[:, h, qs], True, False, ["tK", "tQ"], [nm_(ps)])
                            mm(ps[:, :nn], KRD[r0:r0 + 64, 0, kb * 128:(kb + 1) * 128], QR[r0:r0 + 64, h // 2, qs], False, True, ["tKRD", "tQR"], [nm_(ps)])
                            P.op("act", lambda e, ps=ps, att=att, nn=nn: e.activation(out=att[:, :nn], in_=ps[:, :nn], func=AF.Exp, scale=float(scale)), reads=[nm_(ps)], writes=[nm_(att)])
                            if j >= 0:
                                P.op("dve", lambda e, att=att: e.tensor_tensor(out=att[:, :128], in0=att[:, :128], in1=M_MLA, op=ALU.mult), reads=[nm_(att), "CF"], writes=[nm_(att)])
                            last = kb == nkb - 1
                            mm(PS[2][:, q0:], V[:, kb, h * 128:(h + 1) * 128], att[:, :nn], kb == 0, last, ["tV", nm_(att)], ["ps2"], inc=False)
                            mm(PS[3][:, q0:], ONESB, att[:, :nn], kb == 0, last, ["CB", nm_(att)], ["ps3"], inc=True)
                    else:
                        P.op("pool", lambda e: e.memset(CAC[:], 0.0), writes=["tCAC"])
                        mm(PS[2][:, :], ZB, Q[:, 0, 0:TB], True, False, ["CB", "tQ"], ["ps2"], inc=True)
                        nkb = 4 * QB + 4
                        for kb in range(nkb - 1, -1, -1):
                            j = kb - 4 * QB
                            q0 = 128 * max(j, 0)
                            nn = TB - q0
                            qs = slice(QB * TB + q0, (QB + 1) * TB)
                            ps = PS[n % 2]
                            att = ATT[n % 3]
                            n += 1
                            mm(ps[:, :nn], Kt[:, h, kb * 128:(kb + 1) * 128], Q[:, h, qs], True, True, ["tK", "tQ"], [nm_(ps)])
                            P.op("act", lambda e, ps=ps, nn=nn: e.activation(out=E1[:, :nn], in_=ps[:, :nn], func=AF.Exp, scale=float(-scale)), reads=[nm_(ps)], writes=["tE1"])
                            P.op("act", lambda e, nn=nn: e.activation(out=SP[:, :nn], in_=E1[:, :nn], func=AF.Ln, bias=1.0), reads=["tE1"], writes=["tSP"])
                            P.op("dve", lambda e, ps=ps, nn=nn: e.scalar_tensor_tensor(out=NLK[:, :nn], in0=ps[:, :nn], scalar=float(scale), in1=SP[:, :nn], op0=ALU.mult, op1=ALU.add), reads=[nm_(ps), "tSP"], writes=["tNLK"])
                            if j >= 0:
                                P.op("dve", lambda e: e.tensor_tensor(out=NLK[:, :128], in0=NLK[:, :128], in1=M_LT, op=ALU.mult), reads=["tNLK", "CF"], writes=["tNLK"])
                            mm(PS[4][:, :nn], TRIB, NLK[:, :nn], True, True, ["CB", "tNLK"], ["ps4"])
                            mm(PS[5][:, :nn], ONESB, NLK[:, :nn], True, True, ["CB", "tNLK"], ["ps5"])
                            P.op("dve", lambda e, nn=nn: e.tensor_tensor(out=T1[:, :nn], in0=PS[4][:, :nn], in1=SP[:, :nn], op=ALU.add), reads=["ps4", "tSP"], writes=["tT1"])
                            P.op("dve", lambda e, nn=nn, q0=q0: e.tensor_tensor(out=T1[:, :nn], in0=T1[:, :nn], in1=CAC[:, q0:], op=ALU.add), reads=["tT1", "tCAC"], writes=["tT1"])
                            P.op("act", lambda e, att=att, nn=nn: e.activation(out=att[:, :nn], in_=T1[:, :nn], func=AF.Exp, scale=-1.0), reads=["tT1"], writes=[nm_(att)])
                            if j >= 0:
                                P.op("dve", lambda e, att=att: e.tensor_tensor(out=att[:, :128], in0=att[:, :128], in1=M_LT, op=ALU.mult), reads=[nm_(att), "CF"], writes=[nm_(att)])
                            P.op("dve", lambda e, nn=nn, q0=q0: e.tensor_tensor(out=CAC[:, q0:], in0=CAC[:, q0:], in1=PS[5][:, :nn], op=ALU.add), reads=["ps5", "tCAC"], writes=["tCAC"])
                            mm(PS[2][:, q0:], V[:, kb, h * 128:(h + 1) * 128], att[:, :nn], False, kb == 0, ["tV", nm_(att)], ["ps2"], inc=True)
                    if kind == "sb":
                        P.op("act", lambda e, h=h: e.activation(out=OA[:, h, :], in_=PS[2][:, :], func=AF.Copy), reads=["ps2"], writes=["tOA"])
                    else:
                        P.op("dve", lambda e: e.reciprocal(out=RD[:], in_=PS[3][:, :]), reads=["ps3"], writes=["tRD"])
                        P.op("dve", lambda e, h=h: e.tensor_tensor(out=OA[:, h, :], in0=PS[2][:, :], in1=RD[:], op=ALU.mult), reads=["ps2", "tRD"], writes=["tOA"])
                group_norm_store(OA, 4, lambda c: col("mn", mrow + c), dst, QB, SQs, tmp, rstd, stg, 512)
        P.barrier()

    attn_phase("mla")
    attn_phase("sb")
    attn_phase("ca")
    ssd_phase(P, k, env, mm, ftload)
    P.barrier()


def ssd_phase(P, k, env, mm, ftload):
    nc = k.nc
    l = k.l
    PS, FT, col, IDF, ONESB, IDB, M_LE = env["PS"], env["FT"], env["col"], env["IDF"], env["ONESB"], env["IDB"], env["M_LE"]
    sb, nm_, rstd_from_ps = env["sb"], env["nm_"], env["rstd_from_ps"]
    DTR, catT, SEL, dt_bias, a_log, ssm_d, ssm_norm8 = env["DTR"], env["catT"], env["SEL"], env["dt_bias"], env["a_log"], env["ssm_d"], env["ssm_norm8"]
    XSs, CSs, CSCs, DTCs = env["XSs"], env["CSs"], env["CSCs"], env["DTCs"]
    def ssd_a():
      with ExitStack() as st:
        XC = sb(st, "sXC", [128, 3 + S], BF16)
        ACC = sb(st, "sACC", [128, S], F32)
        XF = sb(st, "sXF", [128, S], BF16)
        T1 = sb(st, "sT1", [128, S], F32)
        T2 = sb(st, "sT2", [128, S], F32)
        T3 = sb(st, "sT3", [128, S], F32)
        XO = [sb(st, f"sXO{i}", [128, 512], BF16) for i in range(2)]
        SM = sb(st, "sSM", [8, 4], F32)
        CC = sb(st, "sCC", [128, 32, 8], F32)
        P.op("pool", lambda e: e.memset(XC[:, 0:3], 0.0), writes=["sXC"])
        for c in range(8):
            ch = FT_CH["xs"] + c
            P.dma("sp", lambda e, ch=ch: e.dma_start(out=XC[:, 3:3 + S], in_=FT[ch * 128:(ch + 1) * 128, :]), reads=["FT"], writes=["sXC"])
            P.op("dve", lambda e, c=c: e.tensor_scalar(out=ACC[:], in0=XC[:, 3:3 + S], scalar1=col("cw", 3 * 8 + c), scalar2=None, op0=ALU.mult), reads=["sXC", "COLS"], writes=["sACC"])
            for j in (2, 1, 0):
                P.op("dve", lambda e, c=c, j=j: e.scalar_tensor_tensor(out=ACC[:], in0=XC[:, j:j + S], scalar=col("cw", j * 8 + c), in1=ACC[:], op0=ALU.mult, op1=ALU.add), reads=["sXC", "sACC", "COLS"], writes=["sACC"])
            P.op("act", lambda e, c=c: e.activation(out=XF[:], in_=ACC[:], func=AF.Silu, bias=col("cb", c), scale=1.0), reads=["sACC", "COLS"], writes=["sXF"])
            if c >= 4:
                P.dma("sp", lambda e, ch=ch: e.dma_start(out=FT[ch * 128:(ch + 1) * 128, :], in_=XF[:]), reads=["sXF"], writes=["FT"])
            else:
                for blk in range(32):
                    ps = PS[blk % 4]
                    xo = XO[blk % 2]
                    mm(ps[:, :128], XF[:, blk * 128:(blk + 1) * 128], IDB, True, True, ["sXF", "CB"], [nm_(ps)])
                    P.op("act", lambda e, ps=ps, xo=xo: e.activation(out=xo[:, :128], in_=ps[:, :128], func=AF.Copy), reads=[nm_(ps)], writes=[nm_(xo)])
                    P.dma("sp", lambda e, xo=xo, blk=blk, c=c: e.dma_start(out=XSs[blk * 128:(blk + 1) * 128, c * 128:(c + 1) * 128], in_=xo[:, :128]), reads=[nm_(xo)], writes=["XSs"])
        P.dma("sp", lambda e: e.dma_start(out=T1[:8, :], in_=DTR[:, :]), reads=["DTR"], writes=["sT1"])
        P.dma("sp", lambda e: e.dma_start(out=SM[:, 0:1], in_=dt_bias[l]), writes=["sSM"])
        P.dma("sp", lambda e: e.dma_start(out=SM[:, 1:2], in_=a_log[l]), writes=["sSM"])
        P.op("act", lambda e: e.activation(out=SM[:, 2:3], in_=SM[:, 1:2], func=AF.Exp), reads=["sSM"], writes=["sSM"])
        P.op("act", lambda e: e.activation(out=T1[:8, :], in_=T1[:8, :], func=AF.Exp, bias=SM[:, 0:1], scale=1.0), reads=["sT1", "sSM"], writes=["sT1"])
        P.op("act", lambda e: e.activation(out=T1[:8, :], in_=T1[:8, :], func=AF.Ln, bias=1.0), reads=["sT1"], writes=["sT1"])
        P.op("dve", lambda e: e.tensor_scalar(out=T2[:8, :], in0=T1[:8, :], scalar1=SM[:, 2:3], scalar2=-1.0, op0=ALU.mult, op1=ALU.mult), reads=["sT1", "sSM"], writes=["sT2"])
        P.op("pool", lambda e: e.memset(T3[:8, :], 1.0), writes=["sT3"])
        P.op("dve", lambda e: e.tensor_tensor_scan(out=ACC[:8, :], data0=T3[:8, :], data1=T2[:8, :], initial=0.0, op0=ALU.mult, op1=ALU.add), reads=["sT2", "sT3"], writes=["sACC"])
        P.dma("sp", lambda e: e.dma_start(out=CSs[:, :], in_=ACC[:8, :]), reads=["sACC"], writes=["CSs"])
        for (src, key, dstd) in ((ACC, "sACC", CSCs), (T1, "sT1", DTCs)):
            for blk in range(32):
                mm(PS[blk % 4][:, :8], src[:8, blk * 128:(blk + 1) * 128], IDF[:8, :8], True, True, [key, "CF"], [nm_(PS[blk % 4])])
                P.op("dve", lambda e, blk=blk: e.tensor_copy(out=CC[:, blk, :], in_=PS[blk % 4][:, :8]), reads=[nm_(PS[blk % 4])], writes=["sCC"])
            P.dma("sp", lambda e, dstd=dstd: e.dma_start(out=dstd[:, :], in_=CC[:].rearrange("p a b -> p (a b)")), reads=["sCC"], writes=[nm_(dstd)])
    ssd_a()
    P.barrier()
    def ssd_b():
      with ExitStack() as st:
        BTt = sb(st, "uB", [128, 2, S], BF16)
        CTt = sb(st, "uC", [128, 2, S], BF16)
        XS = sb(st, "uXS", [128, 32, 512], BF16)
        CSr = sb(st, "uCSr", [8, S], F32)
        CSC = sb(st, "uCSC", [128, 32, 8], F32)
        DTC = sb(st, "uDTC", [128, 32, 8], F32)
        CSB = sb(st, "uCSB", [128, 8, TB], F32)
        DIF = [sb(st, f"uDIF{i}", [128, TB], F32) for i in range(2)]
        W = [sb(st, f"uW{i}", [128, TB], BF16) for i in range(3)]
        ZT = sb(st, "uZT", [64, 8, TB], BF16)
        SZ = sb(st, "uSZ", [64, TB], F32)
        OAh = sb(st, "uOA", [64, 8, TB], F32)
        SQs = sb(st, "uSQs", [64, 8, TB], BF16)
        stg = sb(st, "ustg", [64, 8, TB], BF16)
        tmp = sb(st, "utmp", [128, TB], F32)
        rstd = sb(st, "urstd", [128, TB], F32)
        DSK = sb(st, "uDSK", [128, 8], F32)
        DI = sb(st, "uDI", [128, 8, 128], F32)
        G8 = sb(st, "uG8", [64, 8], F32)
        G8t = sb(st, "uG8t", [8, 64], F32)
        for g in range(2):
            ftload(BTt, g, FT_CH["bm"] + g)
            ftload(CTt, g, FT_CH["cm"] + g, FT, "act")
        P.dma("sp", lambda e: e.dma_start(out=XS[:], in_=XSs.rearrange("(b p) c -> p b c", p=128)), reads=["XSs"], writes=["uXS"])
        P.dma("sp", lambda e: e.dma_start(out=CSr[:], in_=CSs[:, :]), reads=["CSs"], writes=["uCSr"])
        P.dma("sp", lambda e: e.dma_start(out=CSC[:].rearrange("p a b -> p (a b)"), in_=CSCs[:, :]), reads=["CSCs"], writes=["uCSC"])
        P.dma("sp", lambda e: e.dma_start(out=DTC[:].rearrange("p a b -> p (a b)"), in_=DTCs[:, :]), reads=["DTCs"], writes=["uDTC"])
        P.dma("sp", lambda e: e.dma_start(out=DSK[:], in_=ssm_d[l:l + 1, :].partition_broadcast(128)), writes=["uDSK"])
        P.dma("sp", lambda e: e.dma_start(out=G8t[:], in_=ssm_norm8[l]), writes=["uG8t"])
        mm(PS[7][:64, :8], G8t[:, :], IDF[:8, :8], True, True, ["uG8t", "CF"], ["ps7"])
        P.op("dve", lambda e: e.tensor_copy(out=G8[:], in_=PS[7][:64, :8]), reads=["ps7"], writes=["uG8"])
        for h in range(8):
            P.op("dve", lambda e, h=h: e.tensor_scalar(out=DI[:, h, :], in0=IDF, scalar1=DSK[:, h:h + 1], scalar2=None, op0=ALU.mult), reads=["CF", "uDSK"], writes=["uDI"])
        n = 0
        for QB in range(NTB):
            sl = slice(QB * TB, (QB + 1) * TB)
            for h in range(8):
                mm(PS[7][:, :], SEL[:, h * 128:(h + 1) * 128], CSr[:, sl], True, True, ["SEL", "uCSr"], ["ps7"])
                P.op("act", lambda e, h=h: e.activation(out=CSB[:, h, :], in_=PS[7][:, :], func=AF.Copy), reads=["ps7"], writes=["uCSB"])
                P.dma("act", lambda e, h=h, sl=sl: e.dma_start(out=ZT[:, h, :], in_=FT[FT_CH["z"] * 128 + 64 * h:FT_CH["z"] * 128 + 64 * h + 64, sl]), reads=["FT"], writes=["uZT"])
            for g in range(2):
                nkb = 4 * QB + 4
                for kb in range(nkb):
                    j = kb - 4 * QB
                    q0 = 128 * max(j, 0)
                    nn = TB - q0
                    gp = PS[n % 2]
                    n += 1
                    mm(gp[:, :nn], BTt[:, g, kb * 128:(kb + 1) * 128], CTt[:, g, QB * TB + q0:(QB + 1) * TB], True, True, ["uB", "uC"], [nm_(gp)])
                    for r in range(4):
                        h = 4 * g + r
                        dif = DIF[(n + r) % 2]
                        w = W[(n + r) % 3]
                        if j >= 0:
                            P.op("dve", lambda e, dif=dif, h=h, kb=kb, q0=q0, nn=nn: e.tensor_scalar(out=dif[:, :nn], in0=CSB[:, h, q0:], scalar1=CSC[:, kb, h:h + 1], scalar2=0.0, op0=ALU.subtract, op1=ALU.min), reads=["uCSB", "uCSC"], writes=[nm_(dif)])
                        else:
                            P.op("dve", lambda e, dif=dif, h=h, kb=kb, q0=q0, nn=nn: e.tensor_scalar(out=dif[:, :nn], in0=CSB[:, h, q0:], scalar1=CSC[:, kb, h:h + 1], scalar2=None, op0=ALU.subtract), reads=["uCSB", "uCSC"], writes=[nm_(dif)])
                        P.op("act", lambda e, dif=dif, nn=nn: e.activation(out=dif[:, :nn], in_=dif[:, :nn], func=AF.Exp), reads=[nm_(dif)], writes=[nm_(dif)])
                        P.op("dve", lambda e, dif=dif, w=w, gp=gp, h=h, kb=kb, nn=nn: e.scalar_tensor_tensor(out=w[:, :nn], in0=dif[:, :nn], scalar=DTC[:, kb, h:h + 1], in1=gp[:, :nn], op0=ALU.mult, op1=ALU.mult), reads=[nm_(dif), nm_(gp), "uDTC"], writes=[nm_(w)])
                        if j >= 0:
                            P.op("dve", lambda e, w=w: e.tensor_tensor(out=w[:, :128], in0=w[:, :128], in1=M_LE, op=ALU.mult), reads=[nm_(w), "CF"], writes=[nm_(w)])
                            P.op("dve", lambda e, w=w, h=h: e.tensor_tensor(out=w[:, :128], in0=w[:, :128], in1=DI[:, h, :], op=ALU.add), reads=[nm_(w), "uDI"], writes=[nm_(w)])
                        mm(PS[2 + r][:64, q0:], XS[:, kb, h * 64:(h + 1) * 64], w[:, :nn], kb == 0, kb == nkb - 1, ["uXS", nm_(w)], [f"ps{2 + r}"], inc=True)
                for r in range(4):
                    h = 4 * g + r
                    P.op("act", lambda e, h=h: e.activation(out=SZ[:], in_=ZT[:, h, :], func=AF.Silu), reads=["uZT"], writes=["uSZ"])
                    P.op("dve", lambda e, h=h, r=r: e.tensor_tensor(out=OAh[:, h, :], in0=PS[2 + r][:64, :], in1=SZ[:], op=ALU.mult), reads=[f"ps{2 + r}", "uSZ"], writes=["uOA"])
                    P.op("act", lambda e, h=h: e.activation(out=SQs[:, h, :], in_=OAh[:, h, :], func=AF.Square), reads=["uOA"], writes=["uSQs"])
                for r in range(4):
                    mm(PS[6][:, :], ONESB[0:64, :], SQs[:, 4 * g + r, :], r == 0, r == 3, ["uSQs", "CB"], ["ps6"])
                rstd_from_ps(PS[6], rstd, 256, tmp)
                for r in range(4):
                    h = 4 * g + r
                    P.op("dve", lambda e, h=h: e.scalar_tensor_tensor(out=stg[:, h, :], in0=OAh[:, h, :], scalar=G8[:, h:h + 1], in1=rstd[:64, :], op0=ALU.mult, op1=ALU.mult), reads=["uOA", "urstd", "uG8"], writes=["ustg"])
            P.dma("sp", lambda e, sl=sl: e.dma_start(out=catT[512:1024, sl].rearrange("(h p) t -> p h t", p=64), in_=stg[:]), reads=["ustg"], writes=["catT"])
    ssd_b()


def tail(P, k, env, n_exp):
    nc = k.nc
    l = k.l
    PS, xT, col, IDF, ONESB, ONESF = env["PS"], env["xT"], env["col"], env["IDF"], env["ONESB"], env["ONESF"]
    sb, nm_, norm_mod, load_xT_blk = env["sb"], env["nm_"], env["norm_mod"], env["load_xT_blk"]
    catT, h2T, GTs, w_out, router_w, router_b = env["catT"], env["h2T"], env["GTs"], env["w_out"], env["router_w"], env["router_b"]
    w1, b1, w2, b2, sel32 = env["w1"], env["b1"], env["w2"], env["b2"], env["sel32"]
    def p3():
      with ExitStack() as st:
        CT = sb(st, "oCT", [128, NKC, TB], BF16)
        XT = sb(st, "oXT", [128, NKC, TB], F32)
        WO = [sb(st, f"oWO{i}", [128, NKC, 512], BF16) for i in range(2)]
        wl = w_out[l].rearrange("(kc p) n -> p kc n", p=128)
        nw = 0
        for tb in range(NTB):
            load_xT_blk(CT, tb, src=catT, q="act")
            load_xT_blk(XT, tb)
            for nb in range(4):
                wo = WO[nw % 2]
                nw += 1
                P.dma("pool", lambda e, wo=wo, nb=nb: e.dma_start(out=wo[:], in_=wl[:, :, nb * 512:(nb + 1) * 512]), writes=[nm_(wo)])
                for j in range(4):
                    n = nb * 4 + j
                    ps = PS[n % 4]
                    for kc in range(NKC):
                        P.op("pe", lambda e, ps=ps, wo=wo, kc=kc, j=j: e.matmul(ps[:, :], lhsT=wo[:, kc, j * 128:(j + 1) * 128], rhs=CT[:, kc, :], start=(kc == 0), stop=(kc == NKC - 1)),
                             reads=[nm_(wo), "oCT"], writes=[nm_(ps)], inc=(kc == NKC - 1), acc=(kc > 0))
                    P.op("dve", lambda e, ps=ps, n=n: e.scalar_tensor_tensor(out=XT[:, n, :], in0=ps[:, :], scalar=col("mod", 32 + n), in1=XT[:, n, :], op0=ALU.mult, op1=ALU.add),
                         reads=[nm_(ps), "oXT", "COLS"], writes=["oXT"])
            P.dma("sp", lambda e, tb=tb: e.dma_start(out=xT[:, tb * TB:(tb + 1) * TB].rearrange("(kc p) t -> p kc t", p=128), in_=XT[:]), reads=["oXT"], writes=["xT"])
    p3()
    P.barrier()
    import os
    if os.environ.get("SKIP_P4"):
        return
    if getattr(k, "dbg_out", False):
        dd0 = nc.dram_tensor("d_x1T", [D, S], F32, kind="ExternalOutput").ap()
        P.dma("sp", lambda e: e.dma_start(out=dd0[:, :], in_=xT[:, :]), reads=[], writes=["d_x1T"])
        P.barrier()
    def p4a():
      with ExitStack() as st:
        XT = sb(st, "rXT", [128, NKC, TB], F32)
        HF = sb(st, "rHF", [128, NKC, TB], F32)
        HT = sb(st, "rHT", [128, NKC, TB], BF16)
        SQ = sb(st, "rSQ", [128, NKC, TB], BF16)
        tmp = sb(st, "rtmp", [128, TB], F32)
        rstd = sb(st, "rrstd", [128, TB], F32)
        t2 = sb(st, "rt2", [128, TB], F32)
        RW = sb(st, "rRW", [128, NKC, NEXP], F32)
        RB = sb(st, "rRB", [1, NEXP], F32)
        LG = sb(st, "rLG", [128, NEXP], F32)
        M8 = sb(st, "rM8", [128, 8], F32)
        NM = sb(st, "rNM", [128, 1], F32)
        MK = sb(st, "rMK", [128, NEXP], F32)
        EE = sb(st, "rEE", [128, NEXP], F32)
        SM = sb(st, "rSM", [128, 1], F32)
        GG = sb(st, "rGG", [128, NEXP], F32)
        GTt = sb(st, "rGT", [32, TB], F32)
        RBB = sb(st, "rRBB", [128, NEXP], F32)
        if not os.environ.get("SKIP_RW"):
            P.dma("sp", lambda e: e.dma_start(out=RW[:], in_=router_w[l].rearrange("(kc p) n -> p kc n", p=128)), writes=["rRW"])
        if not os.environ.get("SKIP_RBB"):
            P.dma("sp", lambda e: e.dma_start(out=RBB[:], in_=router_b[l].partition_broadcast(128)), writes=["rRBB"])
        for tb in range(NTB):
            if not os.environ.get("SKIP_XTLOAD"):
                load_xT_blk(XT, tb)
            if not os.environ.get("SKIP_NORM2"):
                norm_mod((SQ, tmp, rstd, t2), XT, HT, lambda kc: col("A2", kc), lambda kc: col("mod", 48 + kc), tb, HF=(None if os.environ.get("NO_HF") else HF))
            if not os.environ.get("SKIP_H2T"):
                P.dma("sp", lambda e, tb=tb: e.dma_start(out=h2T[:, tb * TB:(tb + 1) * TB].rearrange("(kc p) t -> p kc t", p=128), in_=HT[:]), reads=["rHT"], writes=["h2T"])
            if os.environ.get("SKIP_ROUTER"):
                continue
            for tt in range(4):
                ps = PS[tt % 2]
                for kc in range(NKC):
                    P.op("pe", lambda e, ps=ps, kc=kc, tt=tt: e.matmul(ps[:, :NEXP], lhsT=HF[:, kc, tt * 128:(tt + 1) * 128], rhs=RW[:, kc, :], start=(kc == 0), stop=(kc == NKC - 1)),
                         reads=["rHF", "rRW"], writes=[nm_(ps)], inc=(kc == NKC - 1), acc=(kc > 0))
                P.op("dve", lambda e, ps=ps: e.tensor_tensor(out=LG[:], in0=ps[:, :NEXP], in1=RBB[:], op=ALU.add), reads=[nm_(ps), "rRBB"], writes=["rLG"])
                P.op("dve", lambda e: e.max(out=M8[:], in_=LG[:]), reads=["rLG"], writes=["rM8"])
                P.op("dve", lambda e: e.tensor_scalar(out=NM[:], in0=M8[:, 0:1], scalar1=-1.0, scalar2=None, op0=ALU.mult), reads=["rM8"], writes=["rNM"])
                P.op("dve", lambda e: e.tensor_scalar(out=MK[:], in0=LG[:], scalar1=M8[:, 3:4], scalar2=1e30, op0=ALU.subtract, op1=ALU.mult), reads=["rLG", "rM8"], writes=["rMK"])
                P.op("dve", lambda e: e.tensor_scalar(out=MK[:], in0=MK[:], scalar1=1.0, scalar2=0.0, op0=ALU.add, op1=ALU.max), reads=["rMK"], writes=["rMK"])
                P.op("dve", lambda e: e.tensor_scalar(out=MK[:], in0=MK[:], scalar1=1.0, scalar2=None, op0=ALU.min), reads=["rMK"], writes=["rMK"])
                P.op("act", lambda e: e.activation(out=EE[:], in_=LG[:], func=AF.Exp, bias=NM[:, 0:1], scale=1.0), reads=["rLG", "rNM"], writes=["rEE"])
                P.op("dve", lambda e: e.tensor_tensor(out=EE[:], in0=EE[:], in1=MK[:], op=ALU.mult), reads=["rEE", "rMK"], writes=["rEE"])
                P.op("dve", lambda e: e.tensor_reduce(out=SM[:], in_=EE[:], axis=mybir.AxisListType.X, op=ALU.add), reads=["rEE"], writes=["rSM"])
                P.op("dve", lambda e: e.tensor_scalar(out=SM[:], in0=SM[:], scalar1=1e-30, scalar2=None, op0=ALU.max), reads=["rSM"], writes=["rSM"])
                P.op("dve", lambda e: e.reciprocal(out=SM[:], in_=SM[:]), reads=["rSM"], writes=["rSM"])
                P.op("dve", lambda e: e.tensor_scalar(out=GG[:], in0=EE[:], scalar1=SM[:, 0:1], scalar2=None, op0=ALU.mult), reads=["rEE", "rSM"], writes=["rGG"])
                P.op("pe", lambda e, tt=tt: e.matmul(PS[2][:NEXP, tt * 128:(tt + 1) * 128], lhsT=GG[:], rhs=IDF, start=True, stop=True), reads=["rGG", "CF"], writes=["ps2"])
            P.op("act", lambda e: e.activation(out=GTt[:], in_=PS[2][:NEXP, :], func=AF.Copy), reads=["ps2"], writes=["rGT"])
            P.dma("sp", lambda e, tb=tb: e.dma_start(out=GTs[:, tb * TB:(tb + 1) * TB], in_=GTt[:]), reads=["rGT"], writes=["GT"])
        if getattr(k, "dbg_out", False):
            for nm2, t_, shp in (("d_LG", LG, [128, NEXP]), ("d_M8", M8, [128, 8]), ("d_GG", GG, [128, NEXP]), ("d_RW", RW, [128, NKC * NEXP]), ("d_RBB", RBB, [128, NEXP]), ("d_HF", HF, [128, NKC * TB]), ("d_GTt", GTt, [32, TB]), ("d_XT", XT, [128, NKC * TB]), ("d_rstd", rstd, [128, TB]), ("d_tmp", tmp, [128, TB])):
                dd = nc.dram_tensor(nm2, shp, F32, kind="ExternalOutput").ap()
                src = t_[:] if len(t_.shape) == 2 else t_[:].rearrange("p a b -> p (a b)")
                P.dma("sp", lambda e, dd=dd, src=src: e.dma_start(out=dd[:, :], in_=src), reads=[nm_(t_)], writes=[nm2])
    p4a()
    P.barrier()
    if os.environ.get("SKIP_P4B"):
        return
    def p4b():
      with ExitStack() as st:
        HT = sb(st, "mHT", [128, NKC, TB], BF16)
        YA = sb(st, "mYA", [128, NKC, TB], F32)
        XC = [sb(st, f"mXC{i}", [128, TB], F32) for i in range(2)]
        W1 = [sb(st, f"mW1{i}", [128, NKC, 384], BF16) for i in range(3)]
        W2s = [sb(st, f"mW2{i}", [128, 6, D], BF16) for i in range(2)]
        nw2 = [0]
        SGL = sb(st, "mSGL", [128, 6, TB], F32)
        AT = sb(st, "mAT", [128, 6, TB], BF16)
        GBC = sb(st, "mGBC", [128, TB], F32)
        G1 = sb(st, "mG1", [128, TB], F32)
        SG = sb(st, "mSG", [128, TB], F32)
        U1 = sb(st, "mU1", [128, TB], F32)
        GTt = sb(st, "mGT", [32, TB], F32)
        SEL = sb(st, "mSEL", [32, 32 * 128], F32)
        B2 = sb(st, "mB2", [32, D], F32)
        B1C = sb(st, "mB1C", [128, 384], F32)
        B1T = sb(st, "mB1T", [128, 128], F32)
        P.dma("sp", lambda e: e.dma_start(out=SEL[:], in_=sel32[:, :]), writes=["mSEL"])
        P.dma("sp", lambda e: e.dma_start(out=B2[:], in_=b2[l]), writes=["mB2"])
        for i in range(3):
            P.dma("sp", lambda e, i=i: e.dma_start(out=B1T[:], in_=b1[l][i * 128:(i + 1) * 128, :]), writes=["mB1T"])
            P.op("pe", lambda e: e.matmul(PS[7][:, :128], lhsT=B1T[:], rhs=IDF, start=True, stop=True), reads=["mB1T", "CF"], writes=["ps7"])
            P.op("dve", lambda e, i=i: e.tensor_copy(out=B1C[:, i * 128:(i + 1) * 128], in_=PS[7][:, :128]), reads=["ps7"], writes=["mB1C"])
        nw = 0
        npz = 0
        ROT = [PS[i] for i in (0, 1, 2, 3, 4, 6, 7)]
        for tb in range(NTB):
            load_xT_blk(HT, tb, src=h2T)
            P.dma("sp", lambda e, tb=tb: e.dma_start(out=GTt[:], in_=GTs[:, tb * TB:(tb + 1) * TB]), reads=["GT"], writes=["mGT"])
            for n in range(NKC):
                ps = ROT[npz % 7]
                npz += 1
                P.op("pe", lambda e, ps=ps, n=n: e.matmul(ps[:, :], lhsT=B2[:, n * 128:(n + 1) * 128], rhs=GTt[:], start=True, stop=True), reads=["mB2", "mGT"], writes=[nm_(ps)])
                P.op("act", lambda e, ps=ps, n=n: e.activation(out=YA[:, n, :], in_=ps[:, :], func=AF.Copy), reads=[nm_(ps)], writes=["mYA"])
            for ex in range(n_exp):
                P.op("pe", lambda e, ex=ex: e.matmul(PS[5][:, :], lhsT=SEL[:, ex * 128:(ex + 1) * 128], rhs=GTt[:], start=True, stop=True), reads=["mSEL", "mGT"], writes=["ps5"])
                P.op("act", lambda e: e.activation(out=GBC[:], in_=PS[5][:, :], func=AF.Copy), reads=["ps5"], writes=["mGBC"])
                W2 = W2s[nw2[0] % 2]
                nw2[0] += 1
                P.dma("pool", lambda e, ex=ex, W2=W2: e.dma_start(out=W2[:], in_=w2[l][ex].rearrange("(fc p) n -> p fc n", p=128)), writes=[nm_(W2)])
                w1l = w1[l][ex].rearrange("(kc p) n -> p kc n", p=128)
                for pc in range(4):
                    wt = W1[nw % 3]
                    nw += 1
                    P.dma("pool", lambda e, wt=wt, pc=pc, w1l=w1l: e.dma_start(out=wt[:], in_=w1l[:, :, pc * 384:(pc + 1) * 384]), writes=[nm_(wt)])
                    for f3 in range(3):
                        f = pc * 3 + f3
                        ps = ROT[npz % 7]
                        npz += 1
                        for kc in range(NKC):
                            P.op("pe", lambda e, ps=ps, wt=wt, kc=kc, f3=f3: e.matmul(ps[:, :], lhsT=wt[:, kc, f3 * 128:(f3 + 1) * 128], rhs=HT[:, kc, :], start=(kc == 0), stop=(kc == NKC - 1)),
                                 reads=[nm_(wt), "mHT"], writes=[nm_(ps)], inc=(kc == NKC - 1), acc=(kc > 0))
                        bc = B1C[:, ex * 12 + f:ex * 12 + f + 1]
                        if f < 6:
                            P.op("dve", lambda e, ps=ps, bc=bc: e.tensor_scalar(out=G1[:], in0=ps[:, :], scalar1=bc, scalar2=7.0, op0=ALU.add, op1=ALU.min), reads=[nm_(ps), "mB1C"], writes=["mG1"])
                            P.op("act", lambda e: e.activation(out=SG[:], in_=G1[:], func=AF.Sigmoid, scale=1.702), reads=["mG1"], writes=["mSG"])
                            P.op("dve", lambda e, f=f: e.tensor_tensor(out=SGL[:, f, :], in0=G1[:], in1=SG[:], op=ALU.mult), reads=["mG1", "mSG"], writes=["mSGL"])
                        else:
                            j = f - 6
                            P.op("dve", lambda e, ps=ps, bc=bc: e.tensor_scalar(out=U1[:], in0=ps[:, :], scalar1=bc, scalar2=7.0, op0=ALU.add, op1=ALU.min), reads=[nm_(ps), "mB1C"], writes=["mU1"])
                            P.op("dve", lambda e: e.tensor_scalar(out=U1[:], in0=U1[:], scalar1=-7.0, scalar2=1.0, op0=ALU.max, op1=ALU.add), reads=["mU1"], writes=["mU1"])
                            P.op("dve", lambda e, j=j: e.tensor_tensor(out=U1[:], in0=U1[:], in1=SGL[:, j, :], op=ALU.mult), reads=["mU1", "mSGL"], writes=["mU1"])
                            P.op("dve", lambda e, j=j: e.tensor_tensor(out=AT[:, j, :], in0=U1[:], in1=GBC[:], op=ALU.mult), reads=["mU1", "mGBC"], writes=["mAT"])
                for n in range(NKC):
                    ps = ROT[npz % 7]
                    npz += 1
                    for fc in range(6):
                        P.op("pe", lambda e, ps=ps, fc=fc, n=n, W2=W2: e.matmul(ps[:, :], lhsT=W2[:, fc, n * 128:(n + 1) * 128], rhs=AT[:, fc, :], start=(fc == 0), stop=(fc == 5)),
                             reads=[nm_(W2), "mAT"], writes=[nm_(ps)], inc=(fc == 5), acc=(fc > 0))
                    P.op("dve", lambda e, ps=ps, n=n: e.tensor_tensor(out=YA[:, n, :], in0=YA[:, n, :], in1=ps[:, :], op=ALU.add), reads=[nm_(ps), "mYA"], writes=["mYA"])
            for n in range(NKC):
                xc = XC[n % 2]
                P.dma("sp", lambda e, xc=xc, n=n, tb=tb: e.dma_start(out=xc[:], in_=xT[n * 128:(n + 1) * 128, tb * TB:(tb + 1) * TB]), reads=["xT"], writes=[nm_(xc)])
                P.op("dve", lambda e, xc=xc, n=n: e.scalar_tensor_tensor(out=xc[:], in0=YA[:, n, :], scalar=col("mod", 80 + n), in1=xc[:], op0=ALU.mult, op1=ALU.add),
                     reads=["mYA", nm_(xc), "COLS"], writes=[nm_(xc)])
                P.dma("sp", lambda e, xc=xc, n=n, tb=tb: e.dma_start(out=xT[n * 128:(n + 1) * 128, tb * TB:(tb + 1) * TB], in_=xc[:]), reads=[nm_(xc)], writes=["xT"])
    p4b()


def final(P, k, env):
    nc = k.nc
    PS, xT, y_out, col, IDF, ONESB = env["PS"], env["xT"], env["y_out"], env["col"], env["IDF"], env["ONESB"]
    sb, nm_, rstd_from_ps = env["sb"], env["nm_"], env["rstd_from_ps"]
    with ExitStack() as st:
        XT = sb(st, "fXT", [128, NKC, TB], F32)
        SQ = sb(st, "fSQ", [128, NKC, TB], BF16)
        tmp = sb(st, "ftmp", [128, TB], F32)
        rstd = sb(st, "frstd", [128, TB], F32)
        OT = [sb(st, f"fOT{i}", [128, D], F32) for i in range(2)]
        nn_ = [0]
        xo_out = env.get("xo_out")

        def emit_T(dst, dkey, tb):
            for tt in range(4):
                ot = OT[nn_[0] % 2]
                nn_[0] += 1
                for g in range(4):
                    ps = PS[g]
                    for j in range(4):
                        kc = g * 4 + j
                        P.op("pe", lambda e, ps=ps, j=j, kc=kc, tt=tt: e.matmul(ps[:, j * 128:(j + 1) * 128], lhsT=XT[:, kc, tt * 128:(tt + 1) * 128], rhs=IDF, start=True, stop=True),
                             reads=["fXT", "CF"], writes=[nm_(ps)], inc=(j == 3))
                    P.op("act", lambda e, ps=ps, ot=ot, g=g: e.activation(out=ot[:, g * 512:(g + 1) * 512], in_=ps[:, :], func=AF.Copy), reads=[nm_(ps)], writes=[nm_(ot)])
                r0 = tb * TB + tt * 128
                P.dma("sp", lambda e, ot=ot, r0=r0, dst=dst: e.dma_start(out=dst[r0:r0 + 128, :], in_=ot[:]), reads=[nm_(ot)], writes=[dkey])

        for tb in range(NTB):
            P.dma("sp", lambda e, tb=tb: e.dma_start(out=XT[:], in_=xT[:, tb * TB:(tb + 1) * TB].rearrange("(kc p) t -> p kc t", p=128)),
                  reads=["xT"], writes=["fXT"])
            if xo_out is not None:
                emit_T(xo_out, "xo", tb)
            P.op("act", lambda e: e.activation(out=SQ[:].rearrange("p a b -> p (a b)"), in_=XT[:].rearrange("p a b -> p (a b)"), func=AF.Square),
                 reads=["fXT"], writes=["fSQ"])
            for kc in range(NKC):
                P.op("pe", lambda e, kc=kc: e.matmul(PS[6][:, :], lhsT=ONESB, rhs=SQ[:, kc, :], start=(kc == 0), stop=(kc == NKC - 1)),
                     reads=["fSQ", "CB"], writes=["ps6"], inc=(kc == NKC - 1), acc=(kc > 0))
            rstd_from_ps(PS[6], rstd, D, tmp)
            for kc in range(NKC):
                P.op("dve", lambda e, kc=kc: e.scalar_tensor_tensor(out=XT[:, kc, :], in0=XT[:, kc, :], scalar=col("fin", kc), in1=rstd[:], op0=ALU.mult, op1=ALU.mult),
                     reads=["fXT", "frstd", "COLS"], writes=["fXT"])
            emit_T(y_out, "y", tb)
    P.barrier()


def _consts():
    p = np.arange(128)[:, None]
    f = np.arange(128)[None, :]
    c = np.zeros((128, 8, 128), np.float32)
    c[:, 0] = (p == f)
    c[:, 1] = 1.0
    c[:, 2] = (p < f)
    c[:, 3] = (p <= f)
    c[:, 4] = ((p // 64) <= (f // 64))
    c[:, 5] = (p > f)
    sel = np.zeros((8, 8 * 128), np.float32)
    for h in range(8):
        sel[h, h * 128:(h + 1) * 128] = 1.0
    sel32 = np.zeros((32, 32 * 128), np.float32)
    for h in range(32):
        sel32[h, h * 128:(h + 1) * 128] = 1.0
    inv = 1.0 / (10000.0 ** (np.arange(0, 64, 2, dtype=np.float32) / 64))
    ang = np.arange(S, dtype=np.float32)[:, None] * inv[None, :]
    cosT = np.cos(ang).T.astype(np.float32)
    sinT = np.sin(ang).T.astype(np.float32)
    rc = np.concatenate([cosT, cosT, cosT, cosT], 0)
    rs = np.concatenate([-sinT, sinT, -sinT, sinT], 0)
    return c, sel, np.ascontiguousarray(rc), np.ascontiguousarray(rs), sel32


def _ca_bias_tables(ca_rel_bias):
    L = ca_rel_bias.shape[0]
    kk = np.arange(640)[:, None]
    qq = np.arange(128)[None, :]
    dist = (512 + qq) - kk
    idx = np.clip(dist, -63, 256) + 63
    qch = (512 + qq) // 64
    kch = kk // 64
    valid = (kch <= qch) & (kch >= qch - 8)
    out = np.empty((L, 4, 640, 128), np.float32)
    for l in range(L):
        for h in range(4):
            t = ca_rel_bias[l, h][idx]
            out[l, h] = np.where(valid, t, np.float32(-30000.0))
    return np.ascontiguousarray(out.reshape(L, 4, 5, 128, 128))


_CACHE = {}


def prep_inputs(inputs, depth, l0=0):
    L = depth
    cst, sel, rc, rs, sel32 = _consts()
    g = lambda n: np.ascontiguousarray(np.asarray(inputs[n], np.float32)[l0:l0 + L])
    shared = {
        "attn_norm": g("attn_norm").reshape(L, 16, 128), "ffn_norm": g("ffn_norm").reshape(L, 16, 128),
        "mod_w": g("mod_w"), "mod_b": g("mod_b").reshape(L, 96, 128), "w_in": g("w_in"),
        "mla_q_norm": g("mla_q_norm").reshape(L, 4, 128), "mla_w_q_up": g("mla_w_q_up"),
        "mla_kv_norm": g("mla_kv_norm").reshape(L, 2, 128), "mla_w_kv_up": g("mla_w_kv_up"),
        "ssm_conv_w": g("ssm_conv_w").reshape(L, 32, 128), "ssm_conv_b": g("ssm_conv_b").reshape(L, 8, 128),
        "ssm_dt_bias": g("ssm_dt_bias").reshape(L, 8, 1), "ssm_a_log": g("ssm_a_log").reshape(L, 8, 1),
        "ssm_d": g("ssm_d"), "ssm_norm": g("ssm_norm").reshape(L, 4, 128), "ssm_norm8": g("ssm_norm").reshape(L, 8, 64),
        "ca_biasT": _ca_bias_tables(g("ca_rel_bias")), "mix_out_norm": g("mix_out_norm").reshape(L, 12, 128),
        "w_out": g("w_out"), "router_w": g("router_w"), "router_b": g("router_b").reshape(L, 1, NEXP),
        "moe_w1": g("moe_w1"), "moe_b1": g("moe_b1").reshape(L, NEXP * 12, 128), "moe_w2": g("moe_w2"), "moe_b2": g("moe_b2"),
        "final_norm": np.asarray(inputs["final_norm"], np.float32).reshape(16, 128),
        "cst": cst, "sel8": sel, "sel32": sel32, "rope_cos": rc, "rope_sin": rs,
    }
    return shared


def run(inputs, depth=4, ncores=NCORES, n_exp=NEXP, dbg_cat=None, dbg_out=False):
    nc, k = build(depth, n_exp, dbg_cat is not None, dbg_out)
    shared = prep_inputs(inputs, depth)
    x = np.asarray(inputs["x"], np.float32)
    c = np.asarray(inputs["c"], np.float32)
    in_maps = []
    for i in range(ncores):
        b = i % 4
        m = dict(shared)
        m["x"] = np.ascontiguousarray(x[b])
        m["c"] = np.ascontiguousarray(c[b].reshape(16, 128))
        if dbg_cat is not None:
            m["catT_in"] = dbg_cat
        in_maps.append(m)
    res = run_bass_kernel_spmd(nc, in_maps, core_ids=list(range(ncores)))
    if dbg_out:
        return [r for r in res.results]
    return [r["y"] for r in res.results]


LAYERS_PER_LAUNCH = 4


def kernel(**inputs):
    depth = 4
    lpl = LAYERS_PER_LAUNCH
    nl = depth // lpl
    nc, k = build(lpl, NEXP, chain=(nl > 1))
    x = np.asarray(inputs["x"], np.float32)
    c = np.asarray(inputs["c"], np.float32)
    xs = [np.ascontiguousarray(x[b]) for b in range(4)]
    outs = None
    for i in range(nl):
        shared = prep_inputs(inputs, lpl, l0=i * lpl)
        in_maps = []
        for b in range(NCORES):
            m = dict(shared)
            m["x"] = xs[b % 4]
            m["c"] = np.ascontiguousarray(c[b % 4].reshape(16, 128))
            in_maps.append(m)
        res = run_bass_kernel_spmd(nc, in_maps, core_ids=list(range(NCORES)))
        outs = res.results
        if nl > 1:
            xs = [np.ascontiguousarray(np.asarray(outs[b]["xo"], np.float32)) for b in range(4)]
    return np.stack([np.asarray(outs[b]["y"], np.float32) for b in range(4)], 0)
```
